# Optimizing a Trainium2 kernel written in Bass

```python
import jax, jax.numpy as jnp
from jax import lax
import numpy as np

D_MODEL = 1024
BATCH = 8
SEQ = 4096
DEPTH = 4

ROPE_THETA = 10000.0
EPS = 1e-6
BLOCK = 128

FOX_HEADS = 8
FOX_DIM = D_MODEL // 16
GLA_HEADS = 4
GLA_DK = D_MODEL // 16
GLA_DV = D_MODEL // 8
GLA_RANK = 16
GLA_TAU = 16.0
GLA_CHUNK = 64
RET_HEADS = 4
RET_DK = D_MODEL // 16
RET_DV = D_MODEL // 8
RET_CHUNK = 128
DIL_HEADS = 8
DIL_DIM = D_MODEL // 16
DIL_PATTERNS = ((128, 1), (512, 4), (2048, 16))

FOX_W = FOX_HEADS * FOX_DIM
GLA_KW = GLA_HEADS * GLA_DK
GLA_VW = GLA_HEADS * GLA_DV
EVEN_MIX = FOX_W + GLA_VW
EVEN_SIZES = (FOX_W, FOX_W, FOX_W, FOX_HEADS, GLA_KW, GLA_KW, GLA_VW, GLA_RANK, EVEN_MIX)
EVEN_IN = sum(EVEN_SIZES)

RET_KW = RET_HEADS * RET_DK
RET_VW = RET_HEADS * RET_DV
DIL_W = DIL_HEADS * DIL_DIM
ODD_MIX = RET_VW + DIL_W
ODD_SIZES = (RET_KW, RET_KW, RET_VW, DIL_W, DIL_W, DIL_W, ODD_MIX)
ODD_IN = sum(ODD_SIZES)

kernel_name = "hybrid_fox_gla_retnet_dilated"

F32 = jnp.float32


def _offsets(sizes):
    return np.cumsum(np.array(sizes))[:-1].tolist()


def rmsnorm(x, g):
    xf = x.astype(F32)
    y = xf * lax.rsqrt(jnp.mean(xf * xf, axis=-1, keepdims=True) + EPS)
    return (y * g.astype(F32)).astype(x.dtype)


def split_heads(t, h):
    B, S, _ = t.shape
    return t.reshape(B, S, h, -1).transpose(0, 2, 1, 3)


def merge_heads(t):
    B, H, S, d = t.shape
    return t.transpose(0, 2, 1, 3).reshape(B, S, H * d)


def rope(x):
    S, dh = x.shape[-2], x.shape[-1]
    inv = jnp.power(ROPE_THETA, -jnp.arange(0, dh, 2, dtype=F32) / dh)
    ang = jnp.arange(S, dtype=F32)[:, None] * inv[None, :]
    cos, sin = jnp.cos(ang), jnp.sin(ang)
    xf = x.astype(F32)
    x1, x2 = xf[..., : dh // 2], xf[..., dh // 2:]
    return jnp.concatenate([x1 * cos - x2 * sin, x2 * cos + x1 * sin], axis=-1).astype(x.dtype)


def head_norm(o, center):
    of = o.astype(F32)
    if center:
        of = of - jnp.mean(of, axis=-1, keepdims=True)
    return of * lax.rsqrt(jnp.mean(of * of, axis=-1, keepdims=True) + EPS)


def forgetting_attention(q, k, v, logf):
    B, H, S, dh = q.shape
    c = jnp.cumsum(logf, axis=-1)
    nb = S // BLOCK
    qb = q.reshape(B, H, nb, BLOCK, dh).transpose(2, 0, 1, 3, 4)
    cb = c.reshape(B, H, nb, BLOCK).transpose(2, 0, 1, 3)
    kpos = jnp.arange(S)
    scale = dh ** -0.5

    def one_block(args):
        i, qi, ci = args
        s = jnp.einsum('bhqd,bhkd->bhqk', qi, k, preferred_element_type=F32) * scale
        s = s + ci[..., :, None] - c[..., None, :]
        qpos = i * BLOCK + jnp.arange(BLOCK)
        s = jnp.where(kpos[None, :] <= qpos[:, None], s, -jnp.inf)
        p = jax.nn.softmax(s, axis=-1)
        return jnp.einsum('bhqk,bhkd->bhqd', p.astype(v.dtype), v)

    o = lax.map(one_block, (jnp.arange(nb), qb, cb))
    return o.transpose(1, 2, 0, 3, 4).reshape(B, H, S, dh)


def gla_chunked(q, k, v, g):
    B, H, S, dk = q.shape
    dv = v.shape[-1]
    C = GLA_CHUNK
    n = S // C
    r = lambda t: t.astype(F32).reshape(B, H, n, C, t.shape[-1])
    qf, kf, vf, gf = r(q) * dk ** -0.5, r(k), r(v), r(g)
    b = jnp.cumsum(gf, axis=3)
    b_last = b[:, :, :, -1:, :]
    q_t = qf * jnp.exp(b)
    k_t = kf * jnp.exp(-b)
    mask = jnp.tril(jnp.ones((C, C), dtype=bool))
    att = jnp.where(mask, jnp.einsum('bhnqd,bhnkd->bhnqk', q_t, k_t), 0.0)
    o_intra = jnp.einsum('bhnqk,bhnkv->bhnqv', att, vf)
    kv = jnp.einsum('bhnkd,bhnkv->bhndv', kf * jnp.exp(b_last - b), vf)
    decay = jnp.exp(b_last[:, :, :, 0, :])

    def step(state, inp):
        kv_n, dec_n = inp
        return state * dec_n[..., None] + kv_n, state

    _, prev = lax.scan(step, jnp.zeros((B, H, dk, dv), F32),
                       (kv.transpose(2, 0, 1, 3, 4), decay.transpose(2, 0, 1, 3)))
    prev = prev.transpose(1, 2, 0, 3, 4)
    o_inter = jnp.einsum('bhnqd,bhndv->bhnqv', q_t, prev)
    return (o_intra + o_inter).reshape(B, H, S, dv).astype(v.dtype)


def retention_chunked(q, k, v):
    B, H, S, dk = q.shape
    dv = v.shape[-1]
    C = RET_CHUNK
    n = S // C
    log_g = jnp.log(1.0 - jnp.power(2.0, -5.0 - jnp.arange(H, dtype=F32)))
    idx = jnp.arange(C, dtype=F32)
    diff = idx[:, None] - idx[None, :]
    dmat = jnp.where(diff >= 0, jnp.exp(jnp.maximum(diff, 0.0)[None] * log_g[:, None, None]), 0.0)
    xi = jnp.exp((idx + 1.0)[None, :] * log_g[:, None])
    zeta = jnp.exp((C - 1.0 - idx)[None, :] * log_g[:, None])
    gC = jnp.exp(C * log_g)
    r = lambda t: t.astype(F32).reshape(B, H, n, C, t.shape[-1])
    qf, kf, vf = r(q) * dk ** -0.5, r(k), r(v)
    att = jnp.einsum('bhnqd,bhnkd->bhnqk', qf, kf) * dmat[None, :, None]
    o_intra = jnp.einsum('bhnqk,bhnkv->bhnqv', att, vf)
    kv = jnp.einsum('bhnkd,bhnkv->bhndv', kf * zeta[None, :, None, :, None], vf)

    def step(state, kv_n):
        return state * gC[None, :, None, None] + kv_n, state

    _, prev = lax.scan(step, jnp.zeros((B, H, dk, dv), F32), kv.transpose(2, 0, 1, 3, 4))
    prev = prev.transpose(1, 2, 0, 3, 4)
    o_inter = jnp.einsum('bhnqd,bhndv->bhnqv', qf * xi[None, :, None, :, None], prev)
    return (o_intra + o_inter).reshape(B, H, S, dv).astype(v.dtype)


def _dilated_branch(q, k, v, window, dil):
    B, H, S, dh = q.shape
    span = window // dil
    unit = dil * BLOCK
    Sp = -(-S // unit) * unit
    m = Sp // dil
    nb = m // BLOCK

    def to_strided(t):
        t = jnp.pad(t, ((0, 0), (0, 0), (0, Sp - S), (0, 0)))
        return t.reshape(B, H, m, dil, dh).transpose(0, 1, 3, 2, 4).reshape(B, H, dil, nb, BLOCK, dh)

    def with_prev(t):
        prev = jnp.pad(t, ((0, 0), (0, 0), (0, 0), (1, 0), (0, 0), (0, 0)))[:, :, :, :-1]
        return jnp.concatenate([prev, t], axis=4)

    qs = to_strided(q)
    kb = with_prev(to_strided(k))
    vb = with_prev(to_strided(v))
    s = jnp.einsum('bhrnqd,bhrnkd->bhrnqk', qs, kb, preferred_element_type=F32) * dh ** -0.5
    qi = jnp.arange(BLOCK)
    ki = jnp.arange(2 * BLOCK) - BLOCK
    rel = qi[:, None] - ki[None, :]
    valid = (rel >= 0) & (rel <= span) & ((jnp.arange(nb)[:, None, None] * BLOCK + ki[None, None, :]) >= 0)
    s = jnp.where(valid, s, -jnp.inf)
    mx = jnp.max(s, axis=-1, keepdims=True)
    p = jnp.exp(s - mx)
    den = jnp.sum(p, axis=-1, keepdims=True)
    o = jnp.einsum('bhrnqk,bhrnkd->bhrnqd', p, vb.astype(F32)) / den
    lse = (mx + jnp.log(den))[..., 0]

    def from_strided(t):
        tail = t.shape[5:]
        t = t.reshape((B, H, dil, m) + tail)
        t = jnp.moveaxis(t, 2, 3).reshape((B, H, Sp) + tail)
        return t[:, :, :S]

    return from_strided(o), from_strided(lse)


def dilated_attention(q, k, v):
    outs, lses = [], []
    for window, dil in DIL_PATTERNS:
        o, lse = _dilated_branch(q, k, v, window, dil)
        outs.append(o)
        lses.append(lse)
    w = jax.nn.softmax(jnp.stack(lses, axis=0), axis=0)
    o = jnp.sum(w[..., None] * jnp.stack(outs, axis=0), axis=0)
    return o.astype(v.dtype)


def even_layer(h, norm_g, w_in, b_f, w_lr, b_lr, gla_g, w_out):
    u = rmsnorm(h, norm_g)
    z = u @ w_in
    fq, fk, fv, ff, gq, gk, gv, glr, gate = jnp.split(z, _offsets(EVEN_SIZES), axis=-1)
    logf = jax.nn.log_sigmoid((ff + b_f).astype(F32)).transpose(0, 2, 1)
    o_a = forgetting_attention(split_heads(fq, FOX_HEADS), split_heads(fk, FOX_HEADS),
                               split_heads(fv, FOX_HEADS), logf)
    glog = jax.nn.log_sigmoid((glr @ w_lr + b_lr).astype(F32)) / GLA_TAU
    o_b = gla_chunked(split_heads(gq, GLA_HEADS), split_heads(gk, GLA_HEADS),
                      split_heads(gv, GLA_HEADS), split_heads(glog, GLA_HEADS))
    o_b = merge_heads(head_norm(o_b, center=False)) * gla_g.astype(F32)
    mix = jnp.concatenate([merge_heads(o_a).astype(F32), o_b], axis=-1).astype(h.dtype)
    return h + (mix * jax.nn.silu(gate)) @ w_out


def odd_layer(h, norm_g, w_in, gn_w, gn_b, w_out):
    u = rmsnorm(h, norm_g)
    z = u @ w_in
    rq, rk, rv, dq, dk, dv, gate = jnp.split(z, _offsets(ODD_SIZES), axis=-1)
    o_c = retention_chunked(rope(split_heads(rq, RET_HEADS)), rope(split_heads(rk, RET_HEADS)),
                            split_heads(rv, RET_HEADS))
    o_c = merge_heads(head_norm(o_c, center=True)) * gn_w.astype(F32) + gn_b.astype(F32)
    o_d = dilated_attention(rope(split_heads(dq, DIL_HEADS)), rope(split_heads(dk, DIL_HEADS)),
                            split_heads(dv, DIL_HEADS))
    mix = jnp.concatenate([o_c, merge_heads(o_d).astype(F32)], axis=-1).astype(h.dtype)
    return h + (mix * jax.nn.silu(gate)) @ w_out


def setup_inputs(seed: int = 0) -> dict:
    key = jax.random.key(seed)
    ks = jax.random.split(key, 16)
    NE = (DEPTH + 1) // 2
    NO = DEPTH // 2
    nrm = lambda k, shape: jax.random.normal(k, shape, F32)
    return {
        "x": nrm(ks[0], (BATCH, SEQ, D_MODEL)),
        "norm_even": 1.0 + 0.02 * nrm(ks[1], (NE, D_MODEL)),
        "w_in_even": nrm(ks[2], (NE, D_MODEL, EVEN_IN)) * D_MODEL ** -0.5,
        "b_f_even": 3.0 + 0.5 * nrm(ks[3], (NE, FOX_HEADS)),
        "w_lr_even": nrm(ks[4], (NE, GLA_RANK, GLA_KW)) * GLA_RANK ** -0.5,
        "b_lr_even": 0.02 * nrm(ks[5], (NE, GLA_KW)),
        "gla_norm_even": 1.0 + 0.02 * nrm(ks[6], (NE, GLA_VW)),
        "w_out_even": nrm(ks[7], (NE, EVEN_MIX, D_MODEL)) * EVEN_MIX ** -0.5,
        "norm_odd": 1.0 + 0.02 * nrm(ks[8], (NO, D_MODEL)),
        "w_in_odd": nrm(ks[9], (NO, D_MODEL, ODD_IN)) * D_MODEL ** -0.5,
        "ret_gn_w_odd": 1.0 + 0.02 * nrm(ks[10], (NO, RET_VW)),
        "ret_gn_b_odd": 0.02 * nrm(ks[11], (NO, RET_VW)),
        "w_out_odd": nrm(ks[12], (NO, ODD_MIX, D_MODEL)) * ODD_MIX ** -0.5,
        "final_norm": 1.0 + 0.02 * nrm(ks[13], (D_MODEL,)),
    }


def reference(x, norm_even, w_in_even, b_f_even, w_lr_even, b_lr_even, gla_norm_even, w_out_even,
              norm_odd, w_in_odd, ret_gn_w_odd, ret_gn_b_odd, w_out_odd, final_norm):
    h = x
    for i in range(DEPTH):
        j = i // 2
        if i % 2 == 0:
            h = even_layer(h, norm_even[j], w_in_even[j], b_f_even[j], w_lr_even[j],
                           b_lr_even[j], gla_norm_even[j], w_out_even[j])
        else:
            h = odd_layer(h, norm_odd[j], w_in_odd[j], ret_gn_w_odd[j], ret_gn_b_odd[j],
                          w_out_odd[j])
    return rmsnorm(h, final_norm)
```

```python
import numpy as np
import ml_dtypes
from contextlib import ExitStack
import concourse.bass as bass
import concourse.mybir as mybir
from concourse.bass_utils import run_bass_kernel_spmd

F32 = mybir.dt.float32
BF16 = mybir.dt.bfloat16
AF = mybir.ActivationFunctionType
ALU = mybir.AluOpType
NPBF = ml_dtypes.bfloat16

T = 4096
D = 1024
NB = 8
EVEN_IN = 3608
ODD_IN = 3584
EPS = 1e-6
NEG = -30000.0


class Prog:
    STREAMS = ("pe", "act", "dve", "pool", "sp")
    SECT = {"pe": "tensor", "act": "scalar", "dve": "vector", "pool": "gpsimd", "sp": "sync"}
    CAP = 20000

    def __init__(self, nc, es):
        self.nc = nc
        self.es = es
        self.sems = {s: [] for s in self.STREAMS}
        self.cnt = {s: 0 for s in self.STREAMS}
        self.dsem = {}
        self.waited = {s: {} for s in self.STREAMS}
        self.reset()

    def reset(self):
        self.ops = []
        self.lw = {}
        self.rd = {}

    def _add(self, stream, fn, reads, writes, dma=None, ndma=0):
        idx = len(self.ops)
        deps = {}
        for k in reads:
            w = self.lw.get(k)
            if w is not None:
                deps[w] = True
        for k in writes:
            w = self.lw.get(k)
            if w is not None:
                deps.setdefault(w, False)
            for r in self.rd.get(k, ()):
                deps.setdefault(r, False)
        for k in reads:
            self.rd.setdefault(k, []).append(idx)
        for k in writes:
            self.lw[k] = idx
            self.rd[k] = []
        self.ops.append(dict(s=stream, fn=fn, deps=deps, dma=dma, ndma=ndma, need=False, c=0))
        return idx

    def op(self, stream, fn, reads=(), writes=()):
        return self._add(stream, fn, tuple(reads), tuple(writes))

    def dma(self, stream, fn, key, n, reads=(), writes=()):
        return self._add(stream, fn, tuple(reads), tuple(writes), dma=key, ndma=n)

    def _sem(self, stream, i):
        lst = self.sems[stream]
        while len(lst) <= i:
            lst.append(self.es.enter_context(self.nc.semaphore(f"s_{stream}_{len(lst)}")))
        return lst[i]

    def finalize_and_emit(self, block):
        ops = self.ops
        for o in ops:
            w = []
            for d, raw in o["deps"].items():
                p = ops[d]
                if p["dma"] is not None:
                    w.append(d)
                elif o["dma"] is not None:
                    w.append(d)
                elif p["s"] == o["s"]:
                    if o["s"] != "pe" and raw:
                        w.append(d)
                else:
                    w.append(d)
            o["w"] = w
            for d in w:
                ops[d]["need"] = True
        for o in ops:
            if o["dma"] is not None:
                if o["dma"] not in self.dsem:
                    self.dsem[o["dma"]] = [self.es.enter_context(self.nc.semaphore("d_" + o["dma"])), 0]
                self.dsem[o["dma"]][1] += 16 * o["ndma"]
                o["c"] = self.dsem[o["dma"]][1]
            elif o["need"]:
                self.cnt[o["s"]] += 1
                o["c"] = self.cnt[o["s"]]
        final_d = {k: v[1] for k, v in self.dsem.items()}

        def target(p):
            if p["dma"] is not None:
                return ("D" + p["dma"], self.dsem[p["dma"]][0], p["c"])
            c = p["c"]
            i = (c - 1) // self.CAP
            return (p["s"] + str(i), self._sem(p["s"], i), (c - 1) % self.CAP + 1)

        for s in self.STREAMS:
            ops_s = [o for o in ops if o["s"] == s]
            if not ops_s and s != "sp":
                continue

            def body(eng, ops_s=ops_s, s=s):
                wt = self.waited[s]
                for o in ops_s:
                    tg = {}
                    for d in o["w"]:
                        name, sem, val = target(ops[d])
                        if wt.get(name, 0) >= val:
                            continue
                        if name not in tg or tg[name][1] < val:
                            tg[name] = (sem, val)
                    for name, (sem, val) in tg.items():
                        eng.wait_ge(sem, val)
                        wt[name] = val
                    r = o["fn"](eng)
                    if o["dma"] is not None:
                        sem = self.dsem[o["dma"]][0]
                        assert len(r) == o["ndma"], (len(r), o["ndma"])
                        for ins in r:
                            ins.then_inc(sem, 16)
                    elif o["need"]:
                        c = o["c"]
                        r.then_inc(self._sem(s, (c - 1) // self.CAP), 1)
                if s == "sp":
                    for k, v in final_d.items():
                        if wt.get("D" + k, 0) < v:
                            eng.wait_ge(self.dsem[k][0], v)
                            wt["D" + k] = v

            getattr(block, self.SECT[s])(body)
        self.reset()


def _consts():
    c = {}
    p = np.arange(128)
    c["ident"] = np.eye(128, dtype=np.float32).astype(NPBF)
    c["ones"] = np.ones((128, 128), np.float32).astype(NPBF)
    c["ones_d128"] = np.full((128, 128), 1.0 / 128, np.float32).astype(NPBF)
    c["nm_cur"] = np.where(p[:, None] > p[None, :], NEG, 0.0).astype(np.float32).astype(NPBF)
    c["nm_prev"] = np.where(p[:, None] < p[None, :], NEG, 0.0).astype(np.float32).astype(NPBF)
    c["tri"] = np.where(p[:, None] <= p[None, :], 1.0, 0.0).astype(np.float32)
    inv = np.power(np.float32(10000.0), -np.arange(0, 64, 2, dtype=np.float32) / np.float32(64)).astype(np.float32)
    ang = (np.arange(T, dtype=np.float32)[None, :] * inv[p % 32][:, None]).astype(np.float32)
    c["cos"] = np.cos(ang.astype(np.float64)).astype(np.float32)
    c["sin"] = np.sin(ang.astype(np.float64)).astype(np.float32)
    perm = np.zeros((128, 128), np.float32)
    for m in range(128):
        if m % 64 < 32:
            perm[m + 32, m] = -1.0
        else:
            perm[m - 32, m] = 1.0
    c["perm"] = perm.astype(NPBF)
    lg = np.log(1.0 - np.power(2.0, -5.0 - np.arange(4, dtype=np.float64)))
    idx = np.arange(128, dtype=np.float64)
    dt = np.zeros((128, 4, 128), np.float32)
    for h in range(4):
        diff = idx[None, :] - idx[:, None]
        dt[:, h, :] = np.where(diff >= 0, np.exp(np.maximum(diff, 0) * lg[h]), 0.0)
    c["ret_dt"] = dt.reshape(128, 512)
    xi = np.zeros((128, 2, 512), np.float32)
    zt = np.zeros((128, 2, 128), np.float32)
    for hp in range(2):
        for e in range(2):
            h = 2 * hp + e
            xi[64 * e:64 * e + 64, hp, :] = np.tile(np.exp((idx + 1.0) * lg[h]), 4)[None, :]
            zt[:, hp, 64 * e:64 * e + 64] = np.exp((127.0 - idx) * lg[h])[:, None]
    c["ret_xi"] = xi.reshape(128, 1024)
    c["ret_zt"] = zt.reshape(128, 256)
    c["ret_gc"] = [float(np.exp(128.0 * lg[h])) for h in range(4)]
    rm = np.ones((128, 512), np.float32)
    rm[:, ::128] = 0.0
    c["resetm"] = rm
    return c


CONST_NAMES = ["ident", "ones", "ones_d128", "nm_cur", "nm_prev", "tri", "cos", "sin", "perm",
               "ret_dt", "ret_xi", "ret_zt", "resetm"]


class Builder:
    def __init__(self, n_layers=4, debug_out=()):
        self.n_layers = n_layers
        self.debug_out = tuple(debug_out)
        self.consts = _consts()
        self.nc = bass.Bass("TRN2", target_bir_lowering=False)
        self.es = ExitStack()
        self.P = Prog(self.nc, self.es)
        self.dram = {}

    def din(self, name, shape, dt):
        self.dram[name] = self.nc.dram_tensor(name, list(shape), dt, kind="ExternalInput").ap()
        return self.dram[name]

    def dscr(self, name, shape, dt):
        kind = "ExternalOutput" if name in self.debug_out else "Internal"
        self.dram[name] = self.nc.dram_tensor(name, list(shape), dt, kind=kind).ap()
        return self.dram[name]

    def declare(self):
        nc = self.nc
        self.din("xT", [D, T], F32)
        for j in range(2):
            self.din(f"w_in_e{j}", [D, EVEN_IN], F32)
            self.din(f"w_in_o{j}", [D, ODD_IN], F32)
            self.din(f"w_out_e{j}", [D, D], F32)
            self.din(f"w_out_o{j}", [D, D], F32)
            self.din(f"w_lr{j}", [16, 256], F32)
        self.din("normg", [128, 5 * 8], F32)
        self.din("b_f", [8, 2], F32)
        self.din("b_lr", [128, 4], F32)
        self.din("gla_g", [128, 8], F32)
        self.din("gn_w", [128, 8], F32)
        self.din("gn_b", [128, 8], F32)
        for n in CONST_NAMES:
            a = self.consts[n]
            self.din("c_" + n, a.shape, BF16 if a.dtype == NPBF else F32)
        self.dram["yT"] = nc.dram_tensor("yT", [D, T], F32, kind="ExternalOutput").ap()
        self.dscr("h0", [D, T], F32)
        self.dscr("h1", [D, T], F32)
        self.dscr("qA", [512, T], BF16)
        self.dscr("kA", [512, T], BF16)
        self.dscr("qB", [256, T], BF16)
        self.dscr("kB", [256, T], BF16)
        self.dscr("ffT", [8, T], F32)
        self.dscr("glrT", [16, T], BF16)
        self.dscr("sgT", [D, T], BF16)
        self.dscr("vA", [T, 1024], BF16)
        self.dscr("vB", [T, 512], BF16)
        self.dscr("mixT", [D, T], BF16)
        self.dscr("caq", [8, 6, T], BF16)
        self.dscr("cak", [8, 6, T], BF16)

    def phase(self, fn):
        nc = self.nc
        self.phase_no = getattr(self, "phase_no", -1) + 1
        with ExitStack() as pes:
            self.pes = pes
            self.tiles = {}
            fn()
            with nc.Block() as block:
                self.P.finalize_and_emit(block)

    def sb(self, name, shape, dt):
        t = self.pes.enter_context(self.nc.sbuf_tensor(f"p{self.phase_no}_{name}", list(shape), dt))
        return t

    def ps(self, name, shape=(128, 512), dt=F32):
        return self.pes.enter_context(self.nc.psum_tensor(f"p{self.phase_no}_{name}", list(shape), dt))

    def load_const(self, name, shape, dt, src=None):
        t = self.sb("k_" + name, shape, dt)
        src = self.dram["c_" + name] if src is None else src
        self.P.dma("sp", lambda e, t=t, src=src: [e.dma_start(out=t[:], in_=src)], "k_" + name, 1,
                   writes=["k_" + name])
        return t

    def phase_ca(self, L):
        P = self.P
        dr = self.dram
        last = (L == self.n_layers)
        odd = (L % 2 == 1)
        j = L // 2
        hsrc = dr["xT"] if L <= 1 else dr[f"h{(L - 1) % 2}"]
        hdst = dr[f"h{L % 2}"]
        NIN = ODD_IN if odd else EVEN_IN

        ones = self.load_const("ones", [128, 128], BF16)
        normg = self.load_const("normg", [128, 40], F32, src=dr["normg"])
        epst = self.sb("epst", [128, 1], F32)
        P.op("dve", lambda e: e.memset(epst[:], EPS), writes=["epst"])
        if odd and not last:
            cos = self.load_const("cos", [128, T], F32)
            sin = self.load_const("sin", [128, T], F32)
            perm = self.load_const("perm", [128, 128], BF16)

        wst = [self.sb(f"wst{i}", [128, 1024], F32) for i in range(2)]
        nst = [0]

        def load_w(dst, src, ncols, nm):
            for ic in range(8):
                for c0 in range(0, ncols, 1024):
                    cw = min(1024, ncols - c0)
                    i = nst[0] % 2
                    nst[0] += 1
                    st = wst[i]
                    P.dma("sp", lambda e, st=st, ic=ic, c0=c0, cw=cw: [e.dma_start(
                        out=st[:, 0:cw], in_=src[ic * 128:(ic + 1) * 128, c0:c0 + cw])],
                        f"wst{i}", 1, writes=[f"wst{i}"])
                    eng = "pool" if (nst[0] % 2) else "dve"
                    P.op(eng, lambda e, st=st, ic=ic, c0=c0, cw=cw: e.tensor_copy(
                        out=dst[:, ic, c0:c0 + cw], in_=st[:, 0:cw]),
                        reads=[f"wst{i}"], writes=[(nm, ic)])

        if L > 0:
            wout = self.sb("wout", [128, 8, D], BF16)
            load_w(wout, dr[f"w_out_{'e' if (L - 1) % 2 == 0 else 'o'}{(L - 1) // 2}"], D, "wout")
        if not last:
            win = self.sb("win", [128, 8, NIN], BF16)
            load_w(win, dr[f"w_in_{'o' if odd else 'e'}{j}"], NIN, "win")

        hT = [self.sb(f"hT{i}", [128, 8, 512], F32) for i in range(2)]
        mg = [self.sb(f"mg{i}", [128, 8, 512], BF16) for i in range(1)] * 2 if L > 0 else None
        sq = self.sb("sq", [128, 8, 512], BF16)
        lnt = self.sb("lnt", [128, 512], F32)
        rstd = self.sb("rstd", [128, 512], F32)
        uT = [self.sb(f"uT{i}", [128, 8, 512], BF16) for i in range(1)] * 2 if not last else None
        NST = 4
        stg = [self.sb(f"stg{i}", [128, 512], BF16) for i in range(NST)]
        stf = self.sb("stf", [128, 512], F32)
        if odd and not last:
            zb = [self.sb(f"zb{i}", [128, 512], BF16) for i in range(2)]
            t1 = [self.sb(f"t1_{i}", [128, 512], F32) for i in range(2)]
            t2 = [self.sb(f"t2_{i}", [128, 512], F32) for i in range(2)]
        if last:
            yst = [self.sb(f"yst{i}", [128, 512], F32) for i in range(2)]
        else:
            stv = [self.sb(f"stv{i}", [128, 8, 128], BF16) for i in range(2)]
            for i in range(2):
                P.op("pool", lambda e, i=i: e.memset(stv[i][:], 1.0), writes=[f"stv{i}"])
        ps_ss = self.ps("ps_ss")
        ps_o = [self.ps(f"ps_o{i}") for i in range(2)]
        ps_z = [self.ps(f"ps_z{i}") for i in range(3)]
        ps_sw = [self.ps(f"ps_sw{i}") for i in range(2)] if (odd and not last) else None

        hv = hsrc.rearrange("(c p) t -> p c t", p=128)
        hdv = hdst.rearrange("(c p) t -> p c t", p=128)
        mv = dr["mixT"].rearrange("(c p) t -> p c t", p=128)
        yv = dr["yT"].rearrange("(c p) t -> p c t", p=128)

        if not last:
            if not odd:
                fm = []
                for c in range(4):
                    fm.append((c * 128, 128, "plain", 0.125, dr["qA"][c * 128:(c + 1) * 128, :]))
                for c in range(4):
                    fm.append((512 + c * 128, 128, "plain", 1.0, dr["kA"][c * 128:(c + 1) * 128, :]))
                fm.append((1536, 8, "f32", 1.0, dr["ffT"][0:8, :]))
                for c in range(2):
                    fm.append((1544 + c * 128, 128, "plain", 1.0, dr["qB"][c * 128:(c + 1) * 128, :]))
                for c in range(2):
                    fm.append((1800 + c * 128, 128, "plain", 1.0, dr["kB"][c * 128:(c + 1) * 128, :]))
                fm.append((2568, 16, "plain", 1.0, dr["glrT"][0:16, :]))
                for c in range(8):
                    fm.append((2584 + c * 128, 128, "silu", 1.0, dr["sgT"][c * 128:(c + 1) * 128, :]))
                tm = [(1024, dr["vA"]), (2056, dr["vB"])]
            else:
                fm = []
                for c in range(2):
                    fm.append((c * 128, 128, "rope", 0.125, dr["qB"][c * 128:(c + 1) * 128, :]))
                for c in range(2):
                    fm.append((256 + c * 128, 128, "rope", 1.0, dr["kB"][c * 128:(c + 1) * 128, :]))
                for c in range(4):
                    fm.append((1024 + c * 128, 128, "rope", 0.125, dr["qA"][c * 128:(c + 1) * 128, :]))
                for c in range(4):
                    fm.append((1536 + c * 128, 128, "rope", 1.0, dr["kA"][c * 128:(c + 1) * 128, :]))
                for c in range(8):
                    fm.append((2560 + c * 128, 128, "silu", 1.0, dr["sgT"][c * 128:(c + 1) * 128, :]))
                tm = [(512, dr["vB"]), (2048, dr["vA"])]

        cnt = dict(z=0, st=0, o=0, rp=0, sv=0)
        for tb in range(NB):
            ts = slice(tb * 512, (tb + 1) * 512)
            h = hT[tb % 2]
            hk = f"hT{tb % 2}"
            P.dma("sp", lambda e, h=h, ts=ts: [e.dma_start(out=h[:, 0:4, :], in_=hv[:, 0:4, ts]),
                                               e.dma_start(out=h[:, 4:8, :], in_=hv[:, 4:8, ts])],
                  hk, 2, writes=[hk])
            if L > 0:
                m = mg[tb % 2]
                mk = "mg0"
                P.dma("sp", lambda e, m=m, ts=ts: [e.dma_start(out=m[:], in_=mv[:, :, ts])], mk, 1,
                      writes=[mk])
                for oc in range(8):
                    po = ps_o[cnt["o"] % 2]
                    pk = f"ps_o{cnt['o'] % 2}"
                    cnt["o"] += 1
                    for mc in range(8):
                        P.op("pe", lambda e, po=po, mc=mc, oc=oc, m=m: e.matmul(
                            po[:], lhsT=wout[:, mc, oc * 128:(oc + 1) * 128], rhs=m[:, mc, :],
                            start=(mc == 0), stop=(mc == 7)),
                            reads=[mk, ("wout", mc)], writes=[pk])
                    P.op("dve", lambda e, po=po, oc=oc, h=h: e.tensor_tensor(
                        out=h[:, oc, :], in0=h[:, oc, :], in1=po[:], op=ALU.add),
                        reads=[pk, hk], writes=[hk])
                if not last:
                    P.dma("pool", lambda e, h=h, ts=ts: [e.dma_start(out=hdv[:, :, ts], in_=h[:])],
                          hk + "s", 1, reads=[hk], writes=[("hdst", tb)])
            P.op("act", lambda e, h=h: e.activation(out=sq[:], in_=h[:], func=AF.Square),
                 reads=[hk], writes=["sq"])
            for c in range(8):
                P.op("pe", lambda e, c=c: e.matmul(ps_ss[:], lhsT=ones[:], rhs=sq[:, c, :],
                                                   start=(c == 0), stop=(c == 7)),
                     reads=["sq", "k_ones"], writes=["ps_ss"])
            P.op("act", lambda e: e.activation(out=lnt[:], in_=ps_ss[:], func=AF.Ln,
                                               scale=1.0 / D, bias=epst[:, 0:1]),
                 reads=["ps_ss", "epst"], writes=["lnt"])
            P.op("act", lambda e: e.activation(out=rstd[:], in_=lnt[:], func=AF.Exp, scale=-0.5),
                 reads=["lnt"], writes=["rstd"])
            if last:
                for c in range(8):
                    y = yst[c % 2]
                    yk = f"yst{c % 2}"
                    P.op("dve", lambda e, y=y, c=c, h=h: e.scalar_tensor_tensor(
                        out=y[:], in0=h[:, c, :], scalar=normg[:, L * 8 + c:L * 8 + c + 1], in1=rstd[:],
                        op0=ALU.mult, op1=ALU.mult),
                        reads=[hk, "rstd", "k_normg"], writes=[yk])
                    P.dma("pool", lambda e, y=y, c=c, ts=ts: [e.dma_start(out=yv[:, c, ts], in_=y[:])],
                          yk + "s", 1, reads=[yk], writes=[("y", tb, c)])
                continue
            u = uT[tb % 2]
            uk = "uT0"
            for c in range(8):
                P.op("dve", lambda e, u=u, c=c, h=h: e.scalar_tensor_tensor(
                    out=u[:, c, :], in0=h[:, c, :], scalar=normg[:, L * 8 + c:L * 8 + c + 1], in1=rstd[:],
                    op0=ALU.mult, op1=ALU.mult),
                    reads=[hk, "rstd", "k_normg"], writes=[(uk, c)])
            ukeys = [(uk, c) for c in range(8)]
            for (c0, M, kind, scale, dst) in fm:
                pz = ps_z[cnt["z"] % 3]
                zk = f"ps_z{cnt['z'] % 3}"
                cnt["z"] += 1
                for ic in range(8):
                    P.op("pe", lambda e, pz=pz, ic=ic, c0=c0, M=M, u=u: e.matmul(
                        pz[0:M, :], lhsT=win[:, ic, c0:c0 + M], rhs=u[:, ic, :],
                        start=(ic == 0), stop=(ic == 7)),
                        reads=[(uk, ic), ("win", ic)], writes=[zk])
                if kind == "f32":
                    P.op("act", lambda e, pz=pz, M=M: e.activation(out=stf[0:M, :], in_=pz[0:M, :], func=AF.Copy),
                         reads=[zk], writes=["stf"])
                    P.dma("pool", lambda e, M=M, dst=dst, ts=ts: [e.dma_start(out=dst[:, ts], in_=stf[0:M, :])],
                          "stfs", 1, reads=["stf"], writes=[("fm", c0, tb)])
                    continue
                if kind in ("plain", "silu"):
                    s = stg[cnt["st"] % NST]
                    sk = f"stg{cnt['st'] % NST}"
                    cnt["st"] += 1
                    if kind == "plain":
                        P.op("act", lambda e, pz=pz, M=M, s=s, scale=scale: e.activation(
                            out=s[0:M, :], in_=pz[0:M, :], func=AF.Copy, scale=scale),
                            reads=[zk], writes=[sk])
                    else:
                        P.op("act", lambda e, pz=pz, M=M, s=s: e.activation(
                            out=s[0:M, :], in_=pz[0:M, :], func=AF.Silu),
                            reads=[zk], writes=[sk])
                    P.dma("pool", lambda e, M=M, dst=dst, ts=ts, s=s: [e.dma_start(out=dst[:, ts], in_=s[0:M, :])],
                          sk + "s", 1, reads=[sk], writes=[("fm", c0, tb)])
                    continue
                r = cnt["rp"] % 2
                cnt["rp"] += 1
                P.op("act", lambda e, pz=pz, r=r, scale=scale: e.activation(
                    out=zb[r][:], in_=pz[:], func=AF.Copy, scale=scale),
                    reads=[zk], writes=[f"zb{r}"])
                P.op("pe", lambda e, r=r: e.matmul(ps_sw[r][:], lhsT=perm[:], rhs=zb[r][:], start=True, stop=True),
                     reads=[f"zb{r}", "k_perm"], writes=[f"ps_sw{r}"])
                P.op("pool", lambda e, r=r, ts=ts: e.tensor_tensor(out=t1[r][:], in0=zb[r][:], in1=cos[:, ts], op=ALU.mult),
                     reads=[f"zb{r}", "k_cos"], writes=[f"t1_{r}"])
                P.op("dve", lambda e, r=r, ts=ts: e.tensor_tensor(out=t2[r][:], in0=ps_sw[r][:], in1=sin[:, ts], op=ALU.mult),
                     reads=[f"ps_sw{r}", "k_sin"], writes=[f"t2_{r}"])
                s = stg[cnt["st"] % NST]
                sk = f"stg{cnt['st'] % NST}"
                cnt["st"] += 1
                P.op("pool", lambda e, r=r, s=s: e.tensor_tensor(out=s[:], in0=t1[r][:], in1=t2[r][:], op=ALU.add),
                     reads=[f"t1_{r}", f"t2_{r}"], writes=[sk])
                P.dma("pool", lambda e, dst=dst, ts=ts, s=s: [e.dma_start(out=dst[:, ts], in_=s[:])],
                      sk + "s", 1, reads=[sk], writes=[("fm", c0, tb)])
            for jt in range(4):
                tile = tb * 4 + jt
                for (c0, dst) in tm:
                    pz = ps_z[cnt["z"] % 3]
                    zk = f"ps_z{cnt['z'] % 3}"
                    cnt["z"] += 1
                    for ic in range(8):
                        P.op("pe", lambda e, pz=pz, ic=ic, c0=c0, u=u, jt=jt: e.matmul(
                            pz[:], lhsT=u[:, ic, jt * 128:(jt + 1) * 128], rhs=win[:, ic, c0:c0 + 512],
                            start=(ic == 0), stop=(ic == 7)),
                            reads=[(uk, ic), ("win", ic)], writes=[zk])
                    if dst is dr["vA"]:
                        vi = cnt["sv"] % 2
                        cnt["sv"] += 1
                        P.op("dve", lambda e, pz=pz, vi=vi: e.tensor_copy(
                            out=stv[vi][:, :, 0:64], in_=pz[:].rearrange("p (h d) -> p h d", d=64)),
                            reads=[zk], writes=[f"stv{vi}"])
                        P.dma("pool", lambda e, dst=dst, tile=tile, vi=vi: [e.dma_start(
                            out=dst[tile * 128:(tile + 1) * 128, :],
                            in_=stv[vi][:].rearrange("p h d -> p (h d)"))],
                            f"stv{vi}s", 1, reads=[f"stv{vi}"], writes=[("tm", c0, tile)])
                        continue
                    s = stg[cnt["st"] % NST]
                    sk = f"stg{cnt['st'] % NST}"
                    cnt["st"] += 1
                    P.op("dve", lambda e, pz=pz, s=s: e.tensor_copy(out=s[:], in_=pz[:]),
                         reads=[zk], writes=[sk])
                    P.dma("pool", lambda e, dst=dst, tile=tile, s=s: [e.dma_start(
                        out=dst[tile * 128:(tile + 1) * 128, :], in_=s[:])],
                        sk + "s", 1, reads=[sk], writes=[("tm", c0, tile)])

    def build(self, nph=None):
        self.declare()
        phases = []
        for L in range(self.n_layers + 1):
            phases.append(lambda L=L: self.phase_ca(L))
            if L == self.n_layers:
                break
            if L % 2 == 0:
                phases.append(lambda L=L: self.phase_even_c(L))
                phases.append(lambda L=L: self.phase_fox(L))
                phases.append(lambda L=L: self.phase_lin(L, "gla"))
            else:
                phases.append(lambda L=L: self.phase_lin(L, "ret"))
                phases.append(lambda L=L: self.phase_dil(L))
        sel = phases[:nph] if not isinstance(nph, (list, tuple)) else [phases[i] for i in nph]
        for ph in sel:
            self.phase(ph)
        return self.nc


    def mm(self, out, lhsT, rhs, start, stop, reads, writes):
        self.P.op("pe", lambda e: e.matmul(out, lhsT=lhsT, rhs=rhs, start=start, stop=stop), reads, writes)

    def act(self, out, in_, func, reads, writes, scale=None, bias=None):
        kw = {}
        if scale is not None:
            kw["scale"] = scale
        if bias is not None:
            kw["bias"] = bias
        self.P.op("act", lambda e: e.activation(out=out, in_=in_, func=func, **kw), reads, writes)

    def tt(self, eng, out, in0, in1, op, reads, writes):
        self.P.op(eng, lambda e: e.tensor_tensor(out=out, in0=in0, in1=in1, op=op), reads, writes)

    def tsc(self, eng, out, in0, scalar, op, reads, writes):
        self.P.op(eng, lambda e: e.tensor_scalar(out=out, in0=in0, scalar1=scalar, scalar2=None, op0=op), reads, writes)

    def stt(self, out, in0, scalar, in1, op0, op1, reads, writes):
        self.P.op("dve", lambda e: e.scalar_tensor_tensor(out=out, in0=in0, scalar=scalar, in1=in1, op0=op0, op1=op1),
                  reads, writes)

    def cp(self, eng, out, in_, reads, writes):
        if eng == "act":
            self.P.op(eng, lambda e: e.copy(out=out, in_=in_), reads, writes)
        else:
            self.P.op(eng, lambda e: e.tensor_copy(out=out, in_=in_), reads, writes)

    def ld(self, key, out, in_, writes=None, reads=(), eng="sp"):
        self.P.dma(eng, lambda e: [e.dma_start(out=out, in_=in_)], key, 1, reads=reads,
                   writes=[key] if writes is None else writes)

    def st(self, key, out, in_, reads, writes, eng="pool"):
        self.P.dma(eng, lambda e: [e.dma_start(out=out, in_=in_)], key, 1, reads=reads, writes=writes)

    def phase_even_c(self, L):
        P = self.P
        dr = self.dram
        j = L // 2
        A = self.sb("cA", [8, T], F32)
        o1 = self.sb("c1", [8, T], F32)
        Pc = self.sb("cP", [8, T], F32)
        r1 = self.sb("cr", [8, T], F32)
        cq = self.sb("cq", [8, 6, T], BF16)
        ck = self.sb("ck", [8, 6, T], BF16)
        bf = self.sb("bf", [8, 2], F32)
        negb = self.sb("negb", [8, 1], F32)
        one1 = self.sb("one1", [8, 1], F32)
        self.ld("cA", A[:], dr["ffT"][:, :])
        self.ld("bf", bf[:], dr["b_f"][:, :])
        self.tsc("dve", negb[:], bf[:, j:j + 1], -1.0, ALU.mult, ["bf"], ["negb"])
        P.op("dve", lambda e: e.memset(one1[:], 1.0), writes=["one1"])
        P.op("pool", lambda e: e.memset(o1[:], 1.0), writes=["c1"])
        P.op("pool", lambda e: e.memset(cq[:, 3:6, :], 1.0), writes=["cq1"])
        P.op("pool", lambda e: e.memset(ck[:, 0:3, :], 1.0), writes=["ck1"])
        self.act(A[:], A[:], AF.Exp, ["cA", "negb"], ["cA"], scale=-1.0, bias=negb[:, 0:1])
        self.act(A[:], A[:], AF.Ln, ["cA", "one1"], ["cA"], bias=one1[:, 0:1])
        P.op("dve", lambda e: e.tensor_tensor_scan(out=Pc[:], data0=o1[:], data1=A[:], initial=0.0,
                                                   op0=ALU.mult, op1=ALU.add), ["c1", "cA"], ["cP"])
        self.cp("dve", ck[:, 3, :], Pc[:], ["cP"], ["ck3"])
        self.tt("dve", r1[:], Pc[:], ck[:, 3, :], ALU.subtract, ["cP", "ck3"], ["cr"])
        self.cp("dve", ck[:, 4, :], r1[:], ["cr"], ["ck4"])
        self.tt("dve", A[:], r1[:], ck[:, 4, :], ALU.subtract, ["cr", "ck4", "cA"], ["cA"])
        self.cp("dve", ck[:, 5, :], A[:], ["cA"], ["ck5"])
        self.tsc("dve", cq[:, 0:3, :], ck[:, 3:6, :], -1.0, ALU.mult, ["ck3", "ck4", "ck5"], ["cq0"])
        self.st("cqs", dr["caq"][:, :, :], cq[:], ["cq0", "cq1"], ["caq"])
        self.st("cks", dr["cak"][:, :, :], ck[:], ["ck1", "ck3", "ck4", "ck5"], ["cak"])

    def attn_epilogue(self, oacc, ok, sg_ap, sgk, dst, uid):
        i = self._ep % 2
        self._ep += 1
        rl, tn, ms = self.ep_rl[i], self.ep_tn[i], self.ep_ms[i]
        self.P.op("dve", lambda e: e.reciprocal(out=rl[0:64, :], in_=oacc[64:128, :]), [ok], [f"ep_rl{i}"])
        self.tt("dve", tn[0:64, :], oacc[0:64, :], rl[0:64, :], ALU.mult, [ok, f"ep_rl{i}"], [f"ep_tn{i}"])
        self.tt("pool", ms[0:64, :], tn[0:64, :], sg_ap, ALU.mult, [f"ep_tn{i}", sgk], [f"ep_ms{i}"])
        self.st(f"ep_ms{i}s", dst, ms[0:64, :], [f"ep_ms{i}"], [("mix", uid)])

    def ep_alloc(self):
        self._ep = 0
        self.ep_rl = [self.sb(f"ep_rl{i}", [64, 512], F32) for i in range(2)]
        self.ep_tn = [self.sb(f"ep_tn{i}", [64, 512], F32) for i in range(2)]
        self.ep_ms = [self.sb(f"ep_ms{i}", [64, 512], BF16) for i in range(2)]

    def phase_fox(self, L):
        P = self.P
        dr = self.dram
        ident = self.load_const("ident", [128, 128], BF16)
        nmc = self.load_const("nm_cur", [128, 128], BF16)
        V = self.sb("V", [128, 32, 1024], BF16)
        for q4 in range(4):
            self.ld(f"V{q4}", V[:, q4 * 8:(q4 + 1) * 8, :],
                    dr["vA"][q4 * 1024:(q4 + 1) * 1024, :].rearrange("(n p) c -> p n c", p=128))
        vkeys = [f"V{q4}" for q4 in range(4)]
        qa = [self.sb(f"qa{i}", [128, T], BF16) for i in range(2)]
        ka = [self.sb(f"ka{i}", [128, T], BF16) for i in range(2)]
        sg = [self.sb(f"sg{i}", [64, T], BF16) for i in range(2)]
        NPT = 4
        pT = [self.sb(f"pT{i}", [128, 512], BF16) for i in range(NPT)]
        self.ep_alloc()
        ps_s = [self.ps(f"ps_s{i}") for i in range(4)]
        ps_o = [self.ps(f"ps_o{i}") for i in range(2)]
        ns = 0
        no = 0
        for h in range(8):
            b = h % 2
            self.ld(f"qa{b}", qa[b][0:64, :], dr["qA"][h * 64:(h + 1) * 64, :], writes=[f"qa{b}", f"qa{b}x"])
            self.ld(f"qa{b}c", qa[b][64:70, :], dr["caq"][h, :, :], writes=[f"qa{b}x"], reads=["caq"])
            self.ld(f"ka{b}", ka[b][0:64, :], dr["kA"][h * 64:(h + 1) * 64, :], writes=[f"ka{b}", f"ka{b}x"])
            self.ld(f"ka{b}c", ka[b][64:70, :], dr["cak"][h, :, :], writes=[f"ka{b}x"], reads=["cak"])
            self.ld(f"sg{b}", sg[b][:], dr["sgT"][h * 64:(h + 1) * 64, :])
            qk_reads = [f"qa{b}", f"qa{b}x", f"ka{b}", f"ka{b}x"]
            for qb in range(8):
                oacc = ps_o[no % 2]
                ok = f"ps_o{no % 2}"
                no += 1
                nkt = 4 * qb + 4
                units = []
                for kt in range(nkt):
                    jd = kt - 4 * qb
                    c0 = 128 * jd if jd > 0 else 0
                    units.append((kt, jd, c0))

                def s_stage(u, ns):
                    kt, jd, c0 = u
                    sp_ = ps_s[ns % 4]
                    sk = f"ps_s{ns % 4}"
                    pt_ = pT[ns % NPT]
                    pk = f"pT{ns % NPT}"
                    self.mm(sp_[:, c0:512], ka[b][0:70, kt * 128:(kt + 1) * 128],
                            qa[b][0:70, qb * 512 + c0:(qb + 1) * 512], True, jd < 0, qk_reads, [sk])
                    if jd >= 0:
                        self.mm(sp_[:, c0:c0 + 128], ident[:], nmc[:], False, True,
                                ["k_ident", "k_nm_cur"], [sk])
                    self.act(pt_[:, c0:512], sp_[:, c0:512], AF.Exp, [sk], [pk])
                    return pt_, pk

                LA = 2
                staged = []
                for ui, u in enumerate(units):
                    staged.append(s_stage(u, ns))
                    ns += 1
                    if ui >= LA:
                        kt, jd, c0 = units[ui - LA]
                        pt_, pk = staged[ui - LA]
                        self.mm(oacc[:, c0:512], V[:, kt, h * 128:(h + 1) * 128], pt_[:, c0:512],
                                kt == 0, kt == nkt - 1, [pk, vkeys[kt // 8]], [ok])
                for ui in range(max(0, len(units) - LA), len(units)):
                    kt, jd, c0 = units[ui]
                    pt_, pk = staged[ui]
                    self.mm(oacc[:, c0:512], V[:, kt, h * 128:(h + 1) * 128], pt_[:, c0:512],
                            kt == 0, kt == nkt - 1, [pk, vkeys[kt // 8]], [ok])
                self.attn_epilogue(oacc, ok, sg[b][:, qb * 512:(qb + 1) * 512], f"sg{b}",
                                   dr["mixT"][h * 64:(h + 1) * 64, qb * 512:(qb + 1) * 512], ("fox", h, qb))

    def phase_lin(self, L, kind):
        P = self.P
        dr = self.dram
        j = L // 2
        gla = kind == "gla"
        ident = self.load_const("ident", [128, 128], BF16)
        onesd = self.load_const("ones_d128", [128, 128], BF16)
        epst = self.sb("epst", [128, 1], F32)
        P.op("dve", lambda e: e.memset(epst[:], EPS), writes=["epst"])
        if gla:
            tri = self.load_const("tri", [128, 128], F32)
            resetm = self.load_const("resetm", [128, 512], F32)
            blr = self.load_const("b_lr", [128, 4], F32, src=dr["b_lr"])
            gpar = self.load_const("gla_g", [128, 8], F32, src=dr["gla_g"])
            wl32 = self.load_const("wl32", [16, 256], F32, src=dr[f"w_lr{j}"])
            wl = self.sb("wl", [16, 256], BF16)
            self.cp("dve", wl[:], wl32[:], ["k_wl32"], ["wl"])
            glr = self.load_const("glr", [16, T], BF16, src=dr["glrT"])
            one1 = self.sb("one1", [128, 1], F32)
            P.op("dve", lambda e: e.memset(one1[:], 1.0), writes=["one1"])
            negb = self.sb("negb", [128, 2], F32)
            self.tsc("dve", negb[:], blr[:, j * 2:j * 2 + 2], -1.0, ALU.mult, ["k_b_lr"], ["negb"])
            sp = self.sb("sp", [128, T], F32)
            Bc = self.sb("Bc", [128, T], F32)
            Epos = self.sb("Epos", [128, T], F32)
        else:
            dtm = self.load_const("ret_dt", [128, 512], F32)
            xi = self.load_const("ret_xi", [128, 1024], F32)
            zt = self.load_const("ret_zt", [128, 256], F32)
            gw = self.load_const("gn_w", [128, 8], F32, src=dr["gn_w"])
            gb = self.load_const("gn_b", [128, 8], F32, src=dr["gn_b"])
            gcs = self.consts["ret_gc"]
        q2 = self.sb("q2", [128, T], BF16)
        k2 = self.sb("k2", [128, T], BF16)
        qt = self.sb("qt", [128, T], BF16)
        kt2 = self.sb("kt2", [128, T], BF16) if gla else k2
        ktok = self.sb("ktok", [128, 32, 128], BF16)
        Vg = self.sb("Vg", [128, 32, 512], BF16)
        for q4 in range(4):
            self.ld(f"Vg{q4}", Vg[:, q4 * 8:(q4 + 1) * 8, :],
                    dr["vB"][q4 * 1024:(q4 + 1) * 1024, :].rearrange("(n p) c -> p n c", p=128))
        sgt = [self.sb(f"sgt{e}", [128, T], BF16) for e in range(2)]
        U = self.sb("U", [128, 128], F32)
        stb = [self.sb(f"stb{i}", [128, 128], BF16) for i in range(2)]
        NA = 4
        Asb = [self.sb(f"Asb{i}", [128, 128], BF16) for i in range(NA)]
        sq = self.sb("sq", [128, 512], BF16)
        lnv = self.sb("lnv", [128, 512], F32)
        rstd = self.sb("rstd", [128, 512], F32)
        on = self.sb("on", [128, 512], F32)
        mst = [self.sb(f"mst{i}", [128, 512], BF16) for i in range(2)]
        if not gla:
            ob = self.sb("ob", [128, 512], BF16)
            mean = self.sb("mean", [128, 512], F32)
            var = self.sb("var", [128, 512], F32)
        ps_g = self.ps("ps_g")
        ps_g2 = self.ps("ps_g2") if not gla else None
        ps_t = self.ps("ps_t", (128, 1024), BF16)
        ps_a = self.ps("ps_a")
        ps_kv = self.ps("ps_kv")
        NOB = 2 if gla else 1
        ps_o = [[self.ps(f"ps_o{e}_{i}") for i in range(NOB)] for e in range(2)]
        nms = 0
        for hp in range(2):
            rows = slice(hp * 128, (hp + 1) * 128)
            self.ld("q2", q2[:], dr["qB"][rows, :])
            self.ld("k2", k2[:], dr["kB"][rows, :])
            for e in range(2):
                h = 2 * hp + e
                mrow = (512 if gla else 0) + h * 128
                self.ld(f"sgt{e}", sgt[e][:], dr["sgT"][mrow:mrow + 128, :])
            for tb in range(NB):
                ts = slice(tb * 512, (tb + 1) * 512)
                if gla:
                    self.mm(ps_g[:], wl[0:16, rows], glr[0:16, ts], True, True, ["wl", "k_glr"], ["ps_g"])
                    self.act(sp[:, ts], ps_g[:], AF.Exp, ["ps_g", "negb"], [("sp", tb)], scale=-1.0,
                             bias=negb[:, hp:hp + 1])
                    self.act(sp[:, ts], sp[:, ts], AF.Ln, [("sp", tb), "one1"], [("sp", tb)], bias=one1[:, 0:1])
                    P.op("dve", lambda e, ts=ts: e.tensor_tensor_scan(
                        out=Bc[:, ts], data0=resetm[:], data1=sp[:, ts], initial=0.0, op0=ALU.mult, op1=ALU.add),
                        [("sp", tb), "k_resetm"], [("Bc", tb)])
                    self.act(Epos[:, ts], Bc[:, ts], AF.Exp, [("Bc", tb)], [("Epos", tb)], scale=-1.0 / 16)
                    self.act(sp[:, ts], Bc[:, ts], AF.Exp, [("Bc", tb), ("sp", tb)], [("sp", tb)], scale=1.0 / 16)
                    self.stt(qt[:, ts], q2[:, ts], 0.125, Epos[:, ts], ALU.mult, ALU.mult,
                             ["q2", ("Epos", tb)], [("qt", tb)])
                    self.tt("pool", kt2[:, ts], k2[:, ts], sp[:, ts], ALU.mult, ["k2", ("sp", tb)], [("kt2", tb)])
                    ktk = ("kt2", tb)
                else:
                    self.tt("dve", qt[:, ts], q2[:, ts], xi[:, hp * 512:(hp + 1) * 512], ALU.mult,
                            ["q2", "k_ret_xi"], [("qt", tb)])
                    ktk = "k2"
                for c in range(4):
                    n = tb * 4 + c
                    cs = slice(n * 128, (n + 1) * 128)
                    pslot = n % 4
                    self.P.op("pe", lambda e, cs=cs, pslot=pslot: e.transpose(
                        ps_t[:, pslot * 128:(pslot + 1) * 128], kt2[:, cs], ident[:]),
                        [ktk, "k_ident"], ["ps_t"])
                    if gla:
                        self.cp("act", ktok[:, n, :], ps_t[:, pslot * 128:(pslot + 1) * 128],
                                ["ps_t"], [("ktok", n)])
                    else:
                        self.tt("dve", ktok[:, n, :], ps_t[:, pslot * 128:(pslot + 1) * 128],
                                zt[:, hp * 128:(hp + 1) * 128], ALU.mult, ["ps_t", "k_ret_zt"],
                                [("ktok", n)])
            na = 0
            for n in range(32):
                tb = n // 4
                cs = slice(n * 128, (n + 1) * 128)
                ocol = slice((n % 4) * 128, (n % 4 + 1) * 128)
                for e in range(2):
                    h = 2 * hp + e
                    r = slice(64 * e, 64 * e + 64)
                    po = ps_o[e][tb % NOB]
                    pok = f"ps_o{e}_{tb % NOB}"
                    asl = slice((na % 4) * 128, (na % 4 + 1) * 128)
                    ak = "ps_a"
                    A_ = Asb[na % NA]
                    Ak = f"Asb{na % NA}"
                    na += 1
                    qk_r = [("qt", tb), ktk, "q2"]
                    if gla:
                        self.mm(ps_a[:, asl], kt2[r, cs], qt[r, cs], True, True, qk_r, [ak])
                        self.tt("dve", A_[:], ps_a[:, asl], tri[:], ALU.mult, [ak, "k_tri"], [Ak])
                    else:
                        self.mm(ps_a[:, asl], k2[r, cs], q2[r, cs], True, True, qk_r, [ak])
                        self.tt("dve", A_[:], ps_a[:, asl], dtm[:, h * 128:(h + 1) * 128], ALU.mult,
                                [ak, "k_ret_dt"], [Ak])
                    vk = f"Vg{n // 8}"
                    self.mm(po[:, ocol], Vg[:, n, h * 128:(h + 1) * 128], A_[:], True, n == 0, [Ak, vk], [pok])
                    if n > 0:
                        self.mm(po[:, ocol], stb[n % 2][r, :], qt[r, cs], False, True,
                                [(f"stb{n % 2}", e), ("qt", tb)], [pok])
                    kvsl = slice((na % 4) * 128, (na % 4 + 1) * 128)
                    kvk = "ps_kv"
                    self.mm(ps_kv[0:64, kvsl], ktok[:, n, r], Vg[:, n, h * 128:(h + 1) * 128], True, True,
                            [("ktok", n), vk], [kvk])
                    uk = ("U", e)
                    if n == 0:
                        self.cp("dve", U[r, :], ps_kv[0:64, kvsl], [kvk], [uk])
                    elif gla:
                        dcol = 128 * (n - 1) + 127
                        self.stt(U[r, :], U[r, :], Epos[r, dcol:dcol + 1], ps_kv[0:64, kvsl], ALU.mult, ALU.add,
                                 [uk, kvk, ("Epos", (n - 1) // 4)], [uk])
                    else:
                        self.stt(U[r, :], U[r, :], gcs[h], ps_kv[0:64, kvsl], ALU.mult, ALU.add, [uk, kvk], [uk])
                    if n < 31:
                        sk = (f"stb{(n + 1) % 2}", e)
                        if gla:
                            dcol = 128 * n + 127
                            self.tsc("pool", stb[(n + 1) % 2][r, :], U[r, :], Epos[r, dcol:dcol + 1], ALU.mult,
                                     [uk, ("Epos", tb)], [sk])
                        else:
                            self.cp("pool", stb[(n + 1) % 2][r, :], U[r, :], [uk], [sk])
                    if n % 4 == 3:
                        ts = slice(tb * 512, (tb + 1) * 512)
                        ms = mst[nms % 2]
                        msk = f"mst{nms % 2}"
                        nms += 1
                        dst = dr["mixT"][(512 if gla else 0) + h * 128:(512 if gla else 0) + (h + 1) * 128, ts]
                        self.act(sq[:], po[:], AF.Square, [pok], ["sq"])
                        self.mm(ps_g[:], onesd[:], sq[:], True, True, ["sq", "k_ones_d128"], ["ps_g"])
                        if gla:
                            self.act(lnv[:], ps_g[:], AF.Ln, ["ps_g", "epst"], ["lnv"], bias=epst[:, 0:1])
                            self.act(rstd[:], lnv[:], AF.Exp, ["lnv"], ["rstd"], scale=-0.5)
                            self.tt("dve", on[:], po[:], rstd[:], ALU.mult, [pok, "rstd"], ["on"])
                            self.stt(ms[:], on[:], gpar[:, j * 4 + h:j * 4 + h + 1], sgt[e][:, ts], ALU.mult, ALU.mult,
                                     ["on", "k_gla_g", f"sgt{e}"], [msk])
                        else:
                            self.cp("act", ob[:], po[:], [pok], ["ob"])
                            self.mm(ps_g2[:], onesd[:], ob[:], True, True, ["ob", "k_ones_d128"], ["ps_g2"])
                            self.cp("act", mean[:], ps_g2[:], ["ps_g2"], ["mean"])
                            self.tt("pool", var[:], mean[:], mean[:], ALU.mult, ["mean"], ["var"])
                            self.tt("dve", var[:], ps_g[:], var[:], ALU.subtract, ["ps_g", "var"], ["var"])
                            self.act(lnv[:], var[:], AF.Ln, ["var", "epst"], ["lnv"], bias=epst[:, 0:1])
                            self.act(rstd[:], lnv[:], AF.Exp, ["lnv"], ["rstd"], scale=-0.5)
                            self.tt("dve", on[:], po[:], mean[:], ALU.subtract, [pok, "mean"], ["on"])
                            self.tt("pool", on[:], on[:], rstd[:], ALU.mult, ["on", "rstd"], ["on"])
                            self.P.op("dve", lambda e_, h=h: e_.tensor_scalar(
                                out=on[:], in0=on[:], scalar1=gw[:, j * 4 + h:j * 4 + h + 1],
                                scalar2=gb[:, j * 4 + h:j * 4 + h + 1], op0=ALU.mult, op1=ALU.add),
                                ["on", "k_gn_w", "k_gn_b"], ["on"])
                            self.tt("pool", ms[:], on[:], sgt[e][:, ts], ALU.mult, ["on", f"sgt{e}"], [msk])
                        self.st(msk + "s", dst, ms[:], [msk], [("mix", kind, h, tb)])

    def phase_dil(self, L):
        P = self.P
        dr = self.dram
        ident = self.load_const("ident", [128, 128], BF16)
        nmc = self.load_const("nm_cur", [128, 128], BF16)
        nmp = self.load_const("nm_prev", [128, 128], BF16)
        q2 = self.sb("q2", [128, T], BF16)
        k2 = self.sb("k2", [128, T], BF16)
        V1 = self.sb("V1", [128, 32, 256], BF16)
        V4 = self.sb("V4", [128, 32, 256], BF16)
        V16 = self.sb("V16", [128, 32, 256], BF16)
        sg = [self.sb(f"sg{i}", [64, T], BF16) for i in range(2)]
        NPT = 3
        pT = [self.sb(f"pT{i}", [128, 512], BF16) for i in range(NPT)]
        self.ep_alloc()
        ps_s = [self.ps(f"ps_s{i}") for i in range(3)]
        ps_o = [self.ps(f"ps_o{i}") for i in range(4)]
        ns = 0
        for hp in range(4):
            rows = slice(hp * 128, (hp + 1) * 128)
            vc = slice(hp * 256, (hp + 1) * 256)
            self.ld("q2", q2[:], dr["qA"][rows, :])
            self.ld("k2", k2[:], dr["kA"][rows, :])
            self.ld("V1", V1[:], dr["vA"][:, vc].rearrange("(n p) c -> p n c", p=128))
            v4 = dr["vA"][:, vc].rearrange("(n i r) c -> i r n c", i=128, r=4)
            P.dma("sp", lambda e, v4=v4: [e.dma_start(out=V4[:, r * 8:(r + 1) * 8, :], in_=v4[:, r, :, :])
                                          for r in range(4)], "V4", 4, writes=["V4"])
            v16 = dr["vA"][:, vc].rearrange("(n i r) c -> i r n c", i=128, r=16)
            P.dma("sp", lambda e, v16=v16: [e.dma_start(out=V16[:, r * 2:(r + 1) * 2, :], in_=v16[:, r, :, :])
                                            for r in range(16)], "V16", 16, writes=["V16"])
            for e in range(2):
                h = 2 * hp + e
                self.ld(f"sg{e}", sg[e][:], dr["sgT"][512 + h * 64:512 + (h + 1) * 64, :])
            for e in range(2):
                h = 2 * hp + e
                r_ = slice(64 * e, 64 * e + 64)
                vh = slice(e * 128, (e + 1) * 128)
                for hf in range(2):
                    units = []
                    for gb in range(16 * hf, 16 * hf + 16):
                        bnk = (gb - 16 * hf) // 4
                        oc = slice((gb % 4) * 128, (gb % 4 + 1) * 128)
                        qc = slice(gb * 128, (gb + 1) * 128)
                        units.append((qc, qc, "cur", V1[:, gb, vh], "V1", [(bnk, oc, slice(0, 128))]))
                        if gb > 0:
                            units.append((slice((gb - 1) * 128, gb * 128), qc, "prev", V1[:, gb - 1, vh], "V1",
                                          [(bnk, oc, slice(0, 128))]))
                    for bnk in range(4):
                        n = 4 * hf + bnk
                        for r in range(4):
                            qc = slice(512 * n + r, 512 * (n + 1), 4)
                            oc = slice(r, 512, 4)
                            units.append((qc, qc, "cur", V4[:, r * 8 + n, vh], "V4", [(bnk, oc, slice(0, 128))]))
                            if n > 0:
                                units.append((slice(512 * (n - 1) + r, 512 * n, 4), qc, "prev",
                                              V4[:, r * 8 + n - 1, vh], "V4", [(bnk, oc, slice(0, 128))]))
                    for r in range(16):
                        qc = slice(2048 * hf + r, 2048 * (hf + 1), 16)
                        outs = [(bnk, slice(r, 512, 16), slice(32 * bnk, 32 * bnk + 32)) for bnk in range(4)]
                        units.append((qc, qc, "cur", V16[:, r * 2 + hf, vh], "V16", outs))
                        if hf > 0:
                            units.append((slice(r, 2048, 16), qc, "prev", V16[:, r * 2, vh], "V16", outs))
                    seq = []
                    for ui, u in enumerate(units):
                        for (bnk, oc, pc) in u[5]:
                            seq.append((ui, bnk))
                    first = {}
                    lastm = {}
                    for si, (ui, bnk) in enumerate(seq):
                        first.setdefault(bnk, si)
                        lastm[bnk] = si
                    si = 0
                    for g0 in range(0, len(units), 4):
                        grp = units[g0:g0 + 4]
                        sp_ = ps_s[ns % 3]
                        sk = f"ps_s{ns % 3}"
                        pt_ = pT[ns % NPT]
                        pk = f"pT{ns % NPT}"
                        ns += 1
                        for gi, (kc, qc, mask, vt, vkey, outs) in enumerate(grp):
                            cs = slice(gi * 128, (gi + 1) * 128)
                            self.mm(sp_[:, cs], k2[r_, kc], q2[r_, qc], True, False, ["q2", "k2"], [sk])
                            self.mm(sp_[:, cs], ident[:], (nmc if mask == "cur" else nmp)[:], False, True,
                                    ["k_ident", "k_nm_cur", "k_nm_prev"], [sk])
                        w = 128 * len(grp)
                        self.act(pt_[:, 0:w], sp_[:, 0:w], AF.Exp, [sk], [pk])
                        for gi, (kc, qc, mask, vt, vkey, outs) in enumerate(grp):
                            for (bnk, oc, pc) in outs:
                                pcs = slice(gi * 128 + pc.start, gi * 128 + pc.stop)
                                self.mm(ps_o[bnk][:, oc], vt, pt_[:, pcs], si == first[bnk], si == lastm[bnk],
                                        [pk, vkey], [f"ps_o{bnk}"])
                                si += 1
                    for bnk in range(4):
                        ts = slice(2048 * hf + 512 * bnk, 2048 * hf + 512 * (bnk + 1))
                        self.attn_epilogue(ps_o[bnk], f"ps_o{bnk}", sg[e][:, ts], f"sg{e}",
                                           dr["mixT"][512 + h * 64:512 + (h + 1) * 64, ts], ("dil", h, hf, bnk))


def make_in_maps(inputs, consts):
    x = np.asarray(inputs["x"], np.float32)
    common = {}
    for j in range(2):
        common[f"w_in_e{j}"] = np.ascontiguousarray(inputs["w_in_even"][j], np.float32)
        common[f"w_in_o{j}"] = np.ascontiguousarray(inputs["w_in_odd"][j], np.float32)
        common[f"w_out_e{j}"] = np.ascontiguousarray(inputs["w_out_even"][j], np.float32)
        common[f"w_out_o{j}"] = np.ascontiguousarray(inputs["w_out_odd"][j], np.float32)
        common[f"w_lr{j}"] = np.ascontiguousarray(inputs["w_lr_even"][j], np.float32)
    gains = [inputs["norm_even"][0], inputs["norm_odd"][0], inputs["norm_even"][1], inputs["norm_odd"][1],
             inputs["final_norm"]]
    ng = np.stack([np.asarray(g, np.float32).reshape(8, 128).T for g in gains], axis=1)
    common["normg"] = np.ascontiguousarray(ng.reshape(128, 40))
    common["b_f"] = np.ascontiguousarray(np.asarray(inputs["b_f_even"], np.float32).T)
    blr = np.asarray(inputs["b_lr_even"], np.float32).reshape(2, 2, 128)
    common["b_lr"] = np.ascontiguousarray(blr.transpose(2, 0, 1).reshape(128, 4))
    for nm, key in (("gla_g", "gla_norm_even"), ("gn_w", "ret_gn_w_odd"), ("gn_b", "ret_gn_b_odd")):
        a = np.asarray(inputs[key], np.float32).reshape(2, 4, 128)
        common[nm] = np.ascontiguousarray(a.transpose(2, 0, 1).reshape(128, 8))
    for n in CONST_NAMES:
        common["c_" + n] = consts[n]
    maps = []
    for b in range(8):
        m = dict(common)
        m["xT"] = np.ascontiguousarray(x[b].T)
        maps.append(m)
    return maps


_CACHE = {}


def kernel(**inputs):
    if "b" not in _CACHE:
        b = Builder()
        b.build()
        _CACHE["b"] = b
    b = _CACHE["b"]
    maps = make_in_maps(inputs, b.consts)
    res = run_bass_kernel_spmd(b.nc, maps, core_ids=list(range(8)))
    out = np.stack([np.ascontiguousarray(r["yT"].T) for r in res.results], axis=0)
    return out.astype(np.float32)
```

```python
import numpy as np
import ml_dtypes
from contextlib import ExitStack
import concourse.bass as bass
import concourse.mybir as mybir
from concourse.bass_utils import run_bass_kernel_spmd

F32 = mybir.dt.float32
BF16 = mybir.dt.bfloat16
AF = mybir.ActivationFunctionType
ALU = mybir.AluOpType
NPBF = ml_dtypes.bfloat16

T = 4096
D = 1024
NB = 8
EVEN_IN = 3608
ODD_IN = 3584
EPS = 1e-6
NEG = -30000.0


class Prog:
    STREAMS = ("pe", "act", "dve", "pool", "sp")
    SECT = {"pe": "tensor", "act": "scalar", "dve": "vector", "pool": "gpsimd", "sp": "sync"}
    CAP = 20000

    def __init__(self, nc, es):
        self.nc = nc
        self.es = es
        self.sems = {s: [] for s in self.STREAMS}
        self.cnt = {s: 0 for s in self.STREAMS}
        self.dsem = {}
        self.waited = {s: {} for s in self.STREAMS}
        self.reset()

    def reset(self):
        self.ops = []
        self.lw = {}
        self.rd = {}

    def _add(self, stream, fn, reads, writes, dma=None, ndma=0):
        idx = len(self.ops)
        deps = {}
        for k in reads:
            w = self.lw.get(k)
            if w is not None:
                deps[w] = True
        for k in writes:
            w = self.lw.get(k)
            if w is not None:
                deps.setdefault(w, False)
            for r in self.rd.get(k, ()):
                deps.setdefault(r, False)
        for k in reads:
            self.rd.setdefault(k, []).append(idx)
        for k in writes:
            self.lw[k] = idx
            self.rd[k] = []
        self.ops.append(dict(s=stream, fn=fn, deps=deps, dma=dma, ndma=ndma, need=False, c=0))
        return idx

    def op(self, stream, fn, reads=(), writes=()):
        return self._add(stream, fn, tuple(reads), tuple(writes))

    def dma(self, stream, fn, key, n, reads=(), writes=()):
        return self._add(stream, fn, tuple(reads), tuple(writes), dma=key, ndma=n)

    def _sem(self, stream, i):
        lst = self.sems[stream]
        while len(lst) <= i:
            lst.append(self.es.enter_context(self.nc.semaphore(f"s_{stream}_{len(lst)}")))
        return lst[i]

    def finalize_and_emit(self, block):
        ops = self.ops
        for o in ops:
            w = []
            for d, raw in o["deps"].items():
                p = ops[d]
                if p["dma"] is not None:
                    w.append(d)
                elif o["dma"] is not None:
                    w.append(d)
                elif p["s"] == o["s"]:
                    if o["s"] != "pe" and raw:
                        w.append(d)
                else:
                    w.append(d)
            o["w"] = w
            for d in w:
                ops[d]["need"] = True
        for o in ops:
            if o["dma"] is not None:
                if o["dma"] not in self.dsem:
                    self.dsem[o["dma"]] = [self.es.enter_context(self.nc.semaphore("d_" + o["dma"])), 0]
                self.dsem[o["dma"]][1] += 16 * o["ndma"]
                o["c"] = self.dsem[o["dma"]][1]
            elif o["need"]:
                self.cnt[o["s"]] += 1
                o["c"] = self.cnt[o["s"]]
        final_d = {k: v[1] for k, v in self.dsem.items()}

        def target(p):
            if p["dma"] is not None:
                return ("D" + p["dma"], self.dsem[p["dma"]][0], p["c"])
            c = p["c"]
            i = (c - 1) // self.CAP
            return (p["s"] + str(i), self._sem(p["s"], i), (c - 1) % self.CAP + 1)

        for s in self.STREAMS:
            ops_s = [o for o in ops if o["s"] == s]
            if not ops_s and s != "sp":
                continue

            def body(eng, ops_s=ops_s, s=s):
                wt = self.waited[s]
                for o in ops_s:
                    tg = {}
                    for d in o["w"]:
                        name, sem, val = target(ops[d])
                        if wt.get(name, 0) >= val:
                            continue
                        if name not in tg or tg[name][1] < val:
                            tg[name] = (sem, val)
                    for name, (sem, val) in tg.items():
                        eng.wait_ge(sem, val)
                        wt[name] = val
                    r = o["fn"](eng)
                    if o["dma"] is not None:
                        sem = self.dsem[o["dma"]][0]
                        assert len(r) == o["ndma"], (len(r), o["ndma"])
                        for ins in r:
                            ins.then_inc(sem, 16)
                    elif o["need"]:
                        c = o["c"]
                        r.then_inc(self._sem(s, (c - 1) // self.CAP), 1)
                if s == "sp":
                    for k, v in final_d.items():
                        if wt.get("D" + k, 0) < v:
                            eng.wait_ge(self.dsem[k][0], v)
                            wt["D" + k] = v

            getattr(block, self.SECT[s])(body)
        self.reset()


def _consts():
    c = {}
    p = np.arange(128)
    c["ident"] = np.eye(128, dtype=np.float32).astype(NPBF)
    c["ones"] = np.ones((128, 128), np.float32).astype(NPBF)
    c["ones_d128"] = np.full((128, 128), 1.0 / 128, np.float32).astype(NPBF)
    c["nm_cur"] = np.where(p[:, None] > p[None, :], NEG, 0.0).astype(np.float32).astype(NPBF)
    c["nm_prev"] = np.where(p[:, None] < p[None, :], NEG, 0.0).astype(np.float32).astype(NPBF)
    c["nm_cur4"] = np.tile(c["nm_cur"], (1, 4))
    c["nm_prev4"] = np.tile(c["nm_prev"], (1, 4))
    c["tri"] = np.where(p[:, None] <= p[None, :], 1.0, 0.0).astype(np.float32)
    inv = np.power(np.float32(10000.0), -np.arange(0, 64, 2, dtype=np.float32) / np.float32(64)).astype(np.float32)
    ang = (np.arange(T, dtype=np.float32)[None, :] * inv[p % 32][:, None]).astype(np.float32)
    c["cos"] = np.cos(ang.astype(np.float64)).astype(np.float32)
    c["sin"] = np.sin(ang.astype(np.float64)).astype(np.float32)
    perm = np.zeros((128, 128), np.float32)
    for m in range(128):
        if m % 64 < 32:
            perm[m + 32, m] = -1.0
        else:
            perm[m - 32, m] = 1.0
    c["perm"] = perm.astype(NPBF)
    lg = np.log(1.0 - np.power(2.0, -5.0 - np.arange(4, dtype=np.float64)))
    idx = np.arange(128, dtype=np.float64)
    dt = np.zeros((128, 4, 128), np.float32)
    for h in range(4):
        diff = idx[None, :] - idx[:, None]
        dt[:, h, :] = np.where(diff >= 0, np.exp(np.maximum(diff, 0) * lg[h]), 0.0)
    c["ret_dt"] = dt.reshape(128, 512)
    xi = np.zeros((128, 2, 512), np.float32)
    zt = np.zeros((128, 2, 128), np.float32)
    for hp in range(2):
        for e in range(2):
            h = 2 * hp + e
            xi[64 * e:64 * e + 64, hp, :] = np.tile(np.exp((idx + 1.0) * lg[h]), 4)[None, :]
            zt[:, hp, 64 * e:64 * e + 64] = np.exp((127.0 - idx) * lg[h])[:, None]
    c["ret_xi"] = xi.reshape(128, 1024)
    c["ret_zt"] = zt.reshape(128, 256)
    c["ret_gc"] = [float(np.exp(128.0 * lg[h])) for h in range(4)]
    rm = np.ones((128, 512), np.float32)
    rm[:, ::128] = 0.0
    c["resetm"] = rm
    return c


CONST_NAMES = ["ident", "ones", "ones_d128", "nm_cur", "nm_prev", "nm_cur4", "nm_prev4", "tri", "cos", "sin", "perm",
               "ret_dt", "ret_xi", "ret_zt", "resetm"]


class Builder:
    def __init__(self, n_layers=4, debug_out=()):
        self.n_layers = n_layers
        self.debug_out = tuple(debug_out)
        self.consts = _consts()
        self.nc = bass.Bass("TRN2", target_bir_lowering=False)
        self.es = ExitStack()
        self.P = Prog(self.nc, self.es)
        self.dram = {}

    def din(self, name, shape, dt):
        self.dram[name] = self.nc.dram_tensor(name, list(shape), dt, kind="ExternalInput").ap()
        return self.dram[name]

    def dscr(self, name, shape, dt):
        kind = "ExternalOutput" if name in self.debug_out else "Internal"
        self.dram[name] = self.nc.dram_tensor(name, list(shape), dt, kind=kind).ap()
        return self.dram[name]

    def declare(self):
        nc = self.nc
        self.din("xT", [D, T], F32)
        for j in range(2):
            self.din(f"w_in_e{j}", [D, EVEN_IN], F32)
            self.din(f"w_in_o{j}", [D, ODD_IN], F32)
            self.din(f"w_out_e{j}", [D, D], F32)
            self.din(f"w_out_o{j}", [D, D], F32)
            self.din(f"w_lr{j}", [16, 256], F32)
        self.din("normg", [128, 5 * 8], F32)
        self.din("b_f", [8, 2], F32)
        self.din("b_lr", [128, 4], F32)
        self.din("gla_g", [128, 8], F32)
        self.din("gn_w", [128, 8], F32)
        self.din("gn_b", [128, 8], F32)
        for n in CONST_NAMES:
            a = self.consts[n]
            self.din("c_" + n, a.shape, BF16 if a.dtype == NPBF else F32)
        self.dram["yT"] = nc.dram_tensor("yT", [D, T], F32, kind="ExternalOutput").ap()
        self.dscr("h0", [D, T], F32)
        self.dscr("h1", [D, T], F32)
        self.dscr("qA", [512, T], BF16)
        self.dscr("kA", [512, T], BF16)
        self.dscr("qB", [256, T], BF16)
        self.dscr("kB", [256, T], BF16)
        self.dscr("ffT", [8, T], F32)
        self.dscr("glrT", [16, T], BF16)
        self.dscr("sgT", [D, T], BF16)
        self.dscr("vA", [T, 1024], BF16)
        self.dscr("vB", [T, 512], BF16)
        self.dscr("mixT", [D, T], BF16)
        self.dscr("caq", [8, 6, T], BF16)
        self.dscr("cak", [8, 6, T], BF16)

    def phase(self, fn):
        nc = self.nc
        self.phase_no = getattr(self, "phase_no", -1) + 1
        with ExitStack() as pes:
            self.pes = pes
            self.tiles = {}
            fn()
            with nc.Block() as block:
                self.P.finalize_and_emit(block)

    def sb(self, name, shape, dt):
        t = self.pes.enter_context(self.nc.sbuf_tensor(f"p{self.phase_no}_{name}", list(shape), dt))
        return t

    def ps(self, name, shape=(128, 512), dt=F32):
        return self.pes.enter_context(self.nc.psum_tensor(f"p{self.phase_no}_{name}", list(shape), dt))

    def load_const(self, name, shape, dt, src=None):
        t = self.sb("k_" + name, shape, dt)
        src = self.dram["c_" + name] if src is None else src
        self.P.dma("sp", lambda e, t=t, src=src: [e.dma_start(out=t[:], in_=src)], "k_" + name, 1,
                   writes=["k_" + name])
        return t

    def phase_ca(self, L):
        P = self.P
        dr = self.dram
        last = (L == self.n_layers)
        odd = (L % 2 == 1)
        j = L // 2
        hsrc = dr["xT"] if L <= 1 else dr[f"h{(L - 1) % 2}"]
        hdst = dr[f"h{L % 2}"]
        NIN = ODD_IN if odd else EVEN_IN

        ones = self.load_const("ones", [128, 128], BF16)
        normg = self.load_const("normg", [128, 40], F32, src=dr["normg"])
        epst = self.sb("epst", [128, 1], F32)
        P.op("dve", lambda e: e.memset(epst[:], EPS), writes=["epst"])
        if odd and not last:
            cosb = [self.sb(f"cosb{i}", [128, 512], F32) for i in range(2)]
            sinb = [self.sb(f"sinb{i}", [128, 512], F32) for i in range(2)]
            perm = self.load_const("perm", [128, 128], BF16)

        wst = [self.sb(f"wst{i}", [128, 1024], F32) for i in range(2)]
        nst = [0]

        def load_w(dst, src, ncols, nm):
            for c0 in range(0, ncols, 1024):
                for ic in range(8):
                    cw = min(1024, ncols - c0)
                    i = nst[0] % 2
                    nst[0] += 1
                    st = wst[i]
                    P.dma("sp", lambda e, st=st, ic=ic, c0=c0, cw=cw: [e.dma_start(
                        out=st[:, 0:cw], in_=src[ic * 128:(ic + 1) * 128, c0:c0 + cw])],
                        f"wst{i}", 1, writes=[f"wst{i}"])
                    eng = "pool" if (nst[0] % 2) else "dve"
                    P.op(eng, lambda e, st=st, ic=ic, c0=c0, cw=cw: e.tensor_copy(
                        out=dst[:, ic, c0:c0 + cw], in_=st[:, 0:cw]),
                        reads=[f"wst{i}"], writes=[(nm, ic, c0 // 1024)])

        if L > 0:
            wout = self.sb("wout", [128, 8, D], BF16)
            load_w(wout, dr[f"w_out_{'e' if (L - 1) % 2 == 0 else 'o'}{(L - 1) // 2}"], D, "wout")
        if not last:
            win = self.sb("win", [128, 8, NIN], BF16)
            load_w(win, dr[f"w_in_{'o' if odd else 'e'}{j}"], NIN, "win")

        hT = [self.sb(f"hT{i}", [128, 8, 512], F32) for i in range(2)]
        mg = [self.sb(f"mg{i}", [128, 8, 512], BF16) for i in range(1)] * 2 if L > 0 else None
        sq = self.sb("sq", [128, 8, 512], BF16)
        lnt = self.sb("lnt", [128, 512], F32)
        rstd = self.sb("rstd", [128, 512], F32)
        uT = [self.sb(f"uT{i}", [128, 8, 512], BF16) for i in range(2)] if not last else None
        NST = 4
        stg = [self.sb(f"stg{i}", [128, 512], BF16) for i in range(NST)]
        stf = self.sb("stf", [128, 512], F32)
        if odd and not last:
            zb = [self.sb(f"zb{i}", [128, 512], BF16) for i in range(2)]
            t1 = [self.sb(f"t1_{i}", [128, 512], F32) for i in range(2)]
            t2 = [self.sb(f"t2_{i}", [128, 512], F32) for i in range(2)]
        if last:
            yst = [self.sb(f"yst{i}", [128, 512], F32) for i in range(2)]
        else:
            stv = [self.sb(f"stv{i}", [128, 8, 128], BF16) for i in range(2)]
            for i in range(2):
                P.op("pool", lambda e, i=i: e.memset(stv[i][:], 1.0), writes=[f"stv{i}"])
        ps_ss = self.ps("ps_ss")
        ps_o = [self.ps(f"ps_o{i}") for i in range(2)]
        ps_z = [self.ps(f"ps_z{i}") for i in range(3)]
        ps_sw = [self.ps(f"ps_sw{i}") for i in range(2)] if (odd and not last) else None

        hv = hsrc.rearrange("(c p) t -> p c t", p=128)
        hdv = hdst.rearrange("(c p) t -> p c t", p=128)
        mv = dr["mixT"].rearrange("(c p) t -> p c t", p=128)
        yv = dr["yT"].rearrange("(c p) t -> p c t", p=128)

        if not last:
            if not odd:
                fm = []
                for c in range(4):
                    fm.append((c * 128, 128, "plain", 0.125, dr["qA"][c * 128:(c + 1) * 128, :]))
                for c in range(4):
                    fm.append((512 + c * 128, 128, "plain", 1.0, dr["kA"][c * 128:(c + 1) * 128, :]))
                fm.append((1536, 8, "f32", 1.0, dr["ffT"][0:8, :]))
                for c in range(2):
                    fm.append((1544 + c * 128, 128, "plain", 1.0, dr["qB"][c * 128:(c + 1) * 128, :]))
                for c in range(2):
                    fm.append((1800 + c * 128, 128, "plain", 1.0, dr["kB"][c * 128:(c + 1) * 128, :]))
                fm.append((2568, 16, "plain", 1.0, dr["glrT"][0:16, :]))
                for c in range(8):
                    fm.append((2584 + c * 128, 128, "silu", 1.0, dr["sgT"][c * 128:(c + 1) * 128, :]))
                tm = [(1024, dr["vA"]), (2056, dr["vB"])]
            else:
                fm = []
                for c in range(2):
                    fm.append((c * 128, 128, "rope", 0.125, dr["qB"][c * 128:(c + 1) * 128, :]))
                for c in range(2):
                    fm.append((256 + c * 128, 128, "rope", 1.0, dr["kB"][c * 128:(c + 1) * 128, :]))
                for c in range(4):
                    fm.append((1024 + c * 128, 128, "rope", 0.125, dr["qA"][c * 128:(c + 1) * 128, :]))
                for c in range(4):
                    fm.append((1536 + c * 128, 128, "rope", 1.0, dr["kA"][c * 128:(c + 1) * 128, :]))
                for c in range(8):
                    fm.append((2560 + c * 128, 128, "silu", 1.0, dr["sgT"][c * 128:(c + 1) * 128, :]))
                tm = [(512, dr["vB"]), (2048, dr["vA"])]

        cnt = dict(z=0, st=0, o=0, rp=0, sv=0)
        for tb in range(NB):
            ts = slice(tb * 512, (tb + 1) * 512)
            h = hT[tb % 2]
            hk = f"hT{tb % 2}"
            P.dma("sp", lambda e, h=h, ts=ts: [e.dma_start(out=h[:, 0:4, :], in_=hv[:, 0:4, ts]),
                                               e.dma_start(out=h[:, 4:8, :], in_=hv[:, 4:8, ts])],
                  hk, 2, writes=[hk])
            if L > 0:
                m = mg[tb % 2]
                mk = "mg0"
                P.dma("sp", lambda e, m=m, ts=ts: [e.dma_start(out=m[:], in_=mv[:, :, ts])], mk, 1,
                      writes=[mk])
                for oc in range(8):
                    po = ps_o[cnt["o"] % 2]
                    pk = f"ps_o{cnt['o'] % 2}"
                    cnt["o"] += 1
                    for mc in range(8):
                        P.op("pe", lambda e, po=po, mc=mc, oc=oc, m=m: e.matmul(
                            po[:], lhsT=wout[:, mc, oc * 128:(oc + 1) * 128], rhs=m[:, mc, :],
                            start=(mc == 0), stop=(mc == 7)),
                            reads=[mk, ("wout", mc, 0)], writes=[pk])
                    P.op("dve", lambda e, po=po, oc=oc, h=h: e.tensor_tensor(
                        out=h[:, oc, :], in0=h[:, oc, :], in1=po[:], op=ALU.add),
                        reads=[pk, hk], writes=[hk])
                if not last:
                    P.dma("pool", lambda e, h=h, ts=ts: [e.dma_start(out=hdv[:, :, ts], in_=h[:])],
                          hk + "s", 1, reads=[hk], writes=[("hdst", tb)])
            P.op("act", lambda e, h=h: e.activation(out=sq[:], in_=h[:], func=AF.Square),
                 reads=[hk], writes=["sq"])
            for c in range(8):
                P.op("pe", lambda e, c=c: e.matmul(ps_ss[:], lhsT=ones[:], rhs=sq[:, c, :],
                                                   start=(c == 0), stop=(c == 7)),
                     reads=["sq", "k_ones"], writes=["ps_ss"])
            P.op("act", lambda e: e.activation(out=lnt[:], in_=ps_ss[:], func=AF.Ln,
                                               scale=1.0 / D, bias=epst[:, 0:1]),
                 reads=["ps_ss", "epst"], writes=["lnt"])
            P.op("act", lambda e: e.activation(out=rstd[:], in_=lnt[:], func=AF.Exp, scale=-0.5),
                 reads=["lnt"], writes=["rstd"])
            if last:
                for c in range(8):
                    y = yst[c % 2]
                    yk = f"yst{c % 2}"
                    P.op("dve", lambda e, y=y, c=c, h=h: e.scalar_tensor_tensor(
                        out=y[:], in0=h[:, c, :], scalar=normg[:, L * 8 + c:L * 8 + c + 1], in1=rstd[:],
                        op0=ALU.mult, op1=ALU.mult),
                        reads=[hk, "rstd", "k_normg"], writes=[yk])
                    P.dma("pool", lambda e, y=y, c=c, ts=ts: [e.dma_start(out=yv[:, c, ts], in_=y[:])],
                          yk + "s", 1, reads=[yk], writes=[("y", tb, c)])
                continue
            u = uT[tb % 2]
            uk = f"uT{tb % 2}"
            for c in range(8):
                P.op("dve", lambda e, u=u, c=c, h=h: e.scalar_tensor_tensor(
                    out=u[:, c, :], in0=h[:, c, :], scalar=normg[:, L * 8 + c:L * 8 + c + 1], in1=rstd[:],
                    op0=ALU.mult, op1=ALU.mult),
                    reads=[hk, "rstd", "k_normg"], writes=[(uk, c)])
            ukeys = [(uk, c) for c in range(8)]
            if odd:
                cb = tb % 2
                self.ld(f"cosb{cb}", cosb[cb][:], dr["c_cos"][:, ts])
                self.ld(f"sinb{cb}", sinb[cb][:], dr["c_sin"][:, ts])
            for (c0, M, kind, scale, dst) in fm:
                pz = ps_z[cnt["z"] % 3]
                zk = f"ps_z{cnt['z'] % 3}"
                cnt["z"] += 1
                for ic in range(8):
                    P.op("pe", lambda e, pz=pz, ic=ic, c0=c0, M=M, u=u: e.matmul(
                        pz[0:M, :], lhsT=win[:, ic, c0:c0 + M], rhs=u[:, ic, :],
                        start=(ic == 0), stop=(ic == 7)),
                        reads=[(uk, ic)] + [("win", ic, g) for g in range(c0 // 1024, (c0 + M - 1) // 1024 + 1)],
                        writes=[zk])
                if kind == "f32":
                    P.op("act", lambda e, pz=pz, M=M: e.activation(out=stf[0:M, :], in_=pz[0:M, :], func=AF.Copy),
                         reads=[zk], writes=["stf"])
                    P.dma("pool", lambda e, M=M, dst=dst, ts=ts: [e.dma_start(out=dst[:, ts], in_=stf[0:M, :])],
                          "stfs", 1, reads=["stf"], writes=[("fm", c0, tb)])
                    continue
                if kind in ("plain", "silu"):
                    s = stg[cnt["st"] % NST]
                    sk = f"stg{cnt['st'] % NST}"
                    cnt["st"] += 1
                    if kind == "plain":
                        P.op("act", lambda e, pz=pz, M=M, s=s, scale=scale: e.activation(
                            out=s[0:M, :], in_=pz[0:M, :], func=AF.Copy, scale=scale),
                            reads=[zk], writes=[sk])
                    else:
                        P.op("act", lambda e, pz=pz, M=M, s=s: e.activation(
                            out=s[0:M, :], in_=pz[0:M, :], func=AF.Silu),
                            reads=[zk], writes=[sk])
                    P.dma("pool", lambda e, M=M, dst=dst, ts=ts, s=s: [e.dma_start(out=dst[:, ts], in_=s[0:M, :])],
                          sk + "s", 1, reads=[sk], writes=[("fm", c0, tb)])
                    continue
                r = cnt["rp"] % 2
                cnt["rp"] += 1
                P.op("act", lambda e, pz=pz, r=r, scale=scale: e.activation(
                    out=zb[r][:], in_=pz[:], func=AF.Copy, scale=scale),
                    reads=[zk], writes=[f"zb{r}"])
                P.op("pe", lambda e, r=r: e.matmul(ps_sw[r][:], lhsT=perm[:], rhs=zb[r][:], start=True, stop=True),
                     reads=[f"zb{r}", "k_perm"], writes=[f"ps_sw{r}"])
                P.op("pool", lambda e, r=r, cb=cb: e.tensor_tensor(out=t1[r][:], in0=zb[r][:], in1=cosb[cb][:], op=ALU.mult),
                     reads=[f"zb{r}", f"cosb{cb}"], writes=[f"t1_{r}"])
                P.op("dve", lambda e, r=r, cb=cb: e.tensor_tensor(out=t2[r][:], in0=ps_sw[r][:], in1=sinb[cb][:], op=ALU.mult),
                     reads=[f"ps_sw{r}", f"sinb{cb}"], writes=[f"t2_{r}"])
                s = stg[cnt["st"] % NST]
                sk = f"stg{cnt['st'] % NST}"
                cnt["st"] += 1
                P.op("pool", lambda e, r=r, s=s: e.tensor_tensor(out=s[:], in0=t1[r][:], in1=t2[r][:], op=ALU.add),
                     reads=[f"t1_{r}", f"t2_{r}"], writes=[sk])
                P.dma("pool", lambda e, dst=dst, ts=ts, s=s: [e.dma_start(out=dst[:, ts], in_=s[:])],
                      sk + "s", 1, reads=[sk], writes=[("fm", c0, tb)])
            for jt in range(4):
                tile = tb * 4 + jt
                for (c0, dst) in tm:
                    pz = ps_z[cnt["z"] % 3]
                    zk = f"ps_z{cnt['z'] % 3}"
                    cnt["z"] += 1
                    for ic in range(8):
                        P.op("pe", lambda e, pz=pz, ic=ic, c0=c0, u=u, jt=jt: e.matmul(
                            pz[:], lhsT=u[:, ic, jt * 128:(jt + 1) * 128], rhs=win[:, ic, c0:c0 + 512],
                            start=(ic == 0), stop=(ic == 7)),
                            reads=[(uk, ic)] + [("win", ic, g) for g in range(c0 // 1024, (c0 + 511) // 1024 + 1)],
                            writes=[zk])
                    if dst is dr["vA"]:
                        vi = cnt["sv"] % 2
                        cnt["sv"] += 1
                        P.op("dve", lambda e, pz=pz, vi=vi: e.tensor_copy(
                            out=stv[vi][:, :, 0:64], in_=pz[:].rearrange("p (h d) -> p h d", d=64)),
                            reads=[zk], writes=[f"stv{vi}"])
                        P.dma("pool", lambda e, dst=dst, tile=tile, vi=vi: [e.dma_start(
                            out=dst[tile * 128:(tile + 1) * 128, :],
                            in_=stv[vi][:].rearrange("p h d -> p (h d)"))],
                            f"stv{vi}s", 1, reads=[f"stv{vi}"], writes=[("tm", c0, tile)])
                        continue
                    s = stg[cnt["st"] % NST]
                    sk = f"stg{cnt['st'] % NST}"
                    cnt["st"] += 1
                    P.op("dve", lambda e, pz=pz, s=s: e.tensor_copy(out=s[:], in_=pz[:]),
                         reads=[zk], writes=[sk])
                    P.dma("pool", lambda e, dst=dst, tile=tile, s=s: [e.dma_start(
                        out=dst[tile * 128:(tile + 1) * 128, :], in_=s[:])],
                        sk + "s", 1, reads=[sk], writes=[("tm", c0, tile)])

    def build(self, nph=None):
        self.declare()
        phases = []
        for L in range(self.n_layers + 1):
            phases.append(lambda L=L: self.phase_ca(L))
            if L == self.n_layers:
                break
            if L % 2 == 0:
                phases.append(lambda L=L: self.phase_even_c(L))
                phases.append(lambda L=L: self.phase_fox(L))
                phases.append(lambda L=L: self.phase_lin(L, "gla"))
            else:
                phases.append(lambda L=L: self.phase_lin(L, "ret"))
                phases.append(lambda L=L: self.phase_dil(L))
        sel = phases[:nph] if not isinstance(nph, (list, tuple)) else [phases[i] for i in nph]
        for ph in sel:
            self.phase(ph)
        return self.nc


    def mm(self, out, lhsT, rhs, start, stop, reads, writes):
        self.P.op("pe", lambda e: e.matmul(out, lhsT=lhsT, rhs=rhs, start=start, stop=stop), reads, writes)

    def act(self, out, in_, func, reads, writes, scale=None, bias=None):
        kw = {}
        if scale is not None:
            kw["scale"] = scale
        if bias is not None:
            kw["bias"] = bias
        self.P.op("act", lambda e: e.activation(out=out, in_=in_, func=func, **kw), reads, writes)

    def tt(self, eng, out, in0, in1, op, reads, writes):
        self.P.op(eng, lambda e: e.tensor_tensor(out=out, in0=in0, in1=in1, op=op), reads, writes)

    def tsc(self, eng, out, in0, scalar, op, reads, writes):
        self.P.op(eng, lambda e: e.tensor_scalar(out=out, in0=in0, scalar1=scalar, scalar2=None, op0=op), reads, writes)

    def stt(self, out, in0, scalar, in1, op0, op1, reads, writes):
        self.P.op("dve", lambda e: e.scalar_tensor_tensor(out=out, in0=in0, scalar=scalar, in1=in1, op0=op0, op1=op1),
                  reads, writes)

    def cp(self, eng, out, in_, reads, writes):
        if eng == "act":
            self.P.op(eng, lambda e: e.copy(out=out, in_=in_), reads, writes)
        else:
            self.P.op(eng, lambda e: e.tensor_copy(out=out, in_=in_), reads, writes)

    def ld(self, key, out, in_, writes=None, reads=(), eng="sp"):
        self.P.dma(eng, lambda e: [e.dma_start(out=out, in_=in_)], key, 1, reads=reads,
                   writes=[key] if writes is None else writes)

    def st(self, key, out, in_, reads, writes, eng="pool"):
        self.P.dma(eng, lambda e: [e.dma_start(out=out, in_=in_)], key, 1, reads=reads, writes=writes)

    def phase_even_c(self, L):
        P = self.P
        dr = self.dram
        j = L // 2
        A = self.sb("cA", [8, T], F32)
        o1 = self.sb("c1", [8, T], F32)
        Pc = self.sb("cP", [8, T], F32)
        r1 = self.sb("cr", [8, T], F32)
        cq = self.sb("cq", [8, 6, T], BF16)
        ck = self.sb("ck", [8, 6, T], BF16)
        bf = self.sb("bf", [8, 2], F32)
        negb = self.sb("negb", [8, 1], F32)
        one1 = self.sb("one1", [8, 1], F32)
        self.ld("cA", A[:], dr["ffT"][:, :])
        self.ld("bf", bf[:], dr["b_f"][:, :])
        self.tsc("dve", negb[:], bf[:, j:j + 1], -1.0, ALU.mult, ["bf"], ["negb"])
        P.op("dve", lambda e: e.memset(one1[:], 1.0), writes=["one1"])
        P.op("pool", lambda e: e.memset(o1[:], 1.0), writes=["c1"])
        P.op("pool", lambda e: e.memset(cq[:, 3:6, :], 1.0), writes=["cq1"])
        P.op("pool", lambda e: e.memset(ck[:, 0:3, :], 1.0), writes=["ck1"])
        self.act(A[:], A[:], AF.Exp, ["cA", "negb"], ["cA"], scale=-1.0, bias=negb[:, 0:1])
        self.act(A[:], A[:], AF.Ln, ["cA", "one1"], ["cA"], bias=one1[:, 0:1])
        P.op("dve", lambda e: e.tensor_tensor_scan(out=Pc[:], data0=o1[:], data1=A[:], initial=0.0,
                                                   op0=ALU.mult, op1=ALU.add), ["c1", "cA"], ["cP"])
        self.cp("dve", ck[:, 3, :], Pc[:], ["cP"], ["ck3"])
        self.tt("dve", r1[:], Pc[:], ck[:, 3, :], ALU.subtract, ["cP", "ck3"], ["cr"])
        self.cp("dve", ck[:, 4, :], r1[:], ["cr"], ["ck4"])
        self.tt("dve", A[:], r1[:], ck[:, 4, :], ALU.subtract, ["cr", "ck4", "cA"], ["cA"])
        self.cp("dve", ck[:, 5, :], A[:], ["cA"], ["ck5"])
        self.tsc("dve", cq[:, 0:3, :], ck[:, 3:6, :], -1.0, ALU.mult, ["ck3", "ck4", "ck5"], ["cq0"])
        self.st("cqs", dr["caq"][:, :, :], cq[:], ["cq0", "cq1"], ["caq"])
        self.st("cks", dr["cak"][:, :, :], ck[:], ["ck1", "ck3", "ck4", "ck5"], ["cak"])

    def attn_epilogue(self, oacc, ok, sg_ap, sgk, dst, uid):
        i = self._ep % 2
        self._ep += 1
        rl, tn, ms = self.ep_rl[i], self.ep_tn[i], self.ep_ms[i]
        self.P.op("dve", lambda e: e.reciprocal(out=rl[0:64, :], in_=oacc[64:128, :]), [ok], [f"ep_rl{i}"])
        self.tt("dve", tn[0:64, :], oacc[0:64, :], rl[0:64, :], ALU.mult, [ok, f"ep_rl{i}"], [f"ep_tn{i}"])
        self.tt("pool", ms[0:64, :], tn[0:64, :], sg_ap, ALU.mult, [f"ep_tn{i}", sgk], [f"ep_ms{i}"])
        self.st(f"ep_ms{i}s", dst, ms[0:64, :], [f"ep_ms{i}"], [("mix", uid)])

    def ep_alloc(self):
        self._ep = 0
        self.ep_rl = [self.sb(f"ep_rl{i}", [64, 512], F32) for i in range(2)]
        self.ep_tn = [self.sb(f"ep_tn{i}", [64, 512], F32) for i in range(2)]
        self.ep_ms = [self.sb(f"ep_ms{i}", [64, 512], BF16) for i in range(2)]

    def phase_fox(self, L):
        P = self.P
        dr = self.dram
        ident = self.load_const("ident", [128, 128], BF16)
        nmc = self.load_const("nm_cur", [128, 128], BF16)
        V = self.sb("V", [128, 32, 1024], BF16)
        for q4 in range(4):
            self.ld(f"V{q4}", V[:, q4 * 8:(q4 + 1) * 8, :],
                    dr["vA"][q4 * 1024:(q4 + 1) * 1024, :].rearrange("(n p) c -> p n c", p=128))
        vkeys = [f"V{q4}" for q4 in range(4)]
        qa = [self.sb(f"qa{i}", [128, T], BF16) for i in range(2)]
        ka = [self.sb(f"ka{i}", [128, T], BF16) for i in range(2)]
        sg = [self.sb(f"sg{i}", [64, T], BF16) for i in range(2)]
        NPT = 4
        pT = [self.sb(f"pT{i}", [128, 512], BF16) for i in range(NPT)]
        self.ep_alloc()
        ps_s = [self.ps(f"ps_s{i}") for i in range(4)]
        ps_o = [self.ps(f"ps_o{i}") for i in range(2)]
        ns = 0
        no = 0
        for h in range(8):
            b = h % 2
            self.ld(f"qa{b}", qa[b][0:64, :], dr["qA"][h * 64:(h + 1) * 64, :], writes=[f"qa{b}", f"qa{b}x"])
            self.ld(f"qa{b}c", qa[b][64:70, :], dr["caq"][h, :, :], writes=[f"qa{b}x"], reads=["caq"])
            self.ld(f"ka{b}", ka[b][0:64, :], dr["kA"][h * 64:(h + 1) * 64, :], writes=[f"ka{b}", f"ka{b}x"])
            self.ld(f"ka{b}c", ka[b][64:70, :], dr["cak"][h, :, :], writes=[f"ka{b}x"], reads=["cak"])
            self.ld(f"sg{b}", sg[b][:], dr["sgT"][h * 64:(h + 1) * 64, :])
            qk_reads = [f"qa{b}", f"qa{b}x", f"ka{b}", f"ka{b}x"]
            for qb in range(8):
                oacc = ps_o[no % 2]
                ok = f"ps_o{no % 2}"
                no += 1
                nkt = 4 * qb + 4
                units = []
                for kt in range(nkt):
                    jd = kt - 4 * qb
                    c0 = 128 * jd if jd > 0 else 0
                    units.append((kt, jd, c0))

                def s_stage(u, ns):
                    kt, jd, c0 = u
                    sp_ = ps_s[ns % 4]
                    sk = f"ps_s{ns % 4}"
                    pt_ = pT[ns % NPT]
                    pk = f"pT{ns % NPT}"
                    self.mm(sp_[:, c0:512], ka[b][0:70, kt * 128:(kt + 1) * 128],
                            qa[b][0:70, qb * 512 + c0:(qb + 1) * 512], True, jd < 0, qk_reads, [sk])
                    if jd >= 0:
                        self.mm(sp_[:, c0:c0 + 128], ident[:], nmc[:], False, True,
                                ["k_ident", "k_nm_cur"], [sk])
                    self.act(pt_[:, c0:512], sp_[:, c0:512], AF.Exp, [sk], [pk])
                    return pt_, pk

                LA = 2
                staged = []
                for ui, u in enumerate(units):
                    staged.append(s_stage(u, ns))
                    ns += 1
                    if ui >= LA:
                        kt, jd, c0 = units[ui - LA]
                        pt_, pk = staged[ui - LA]
                        self.mm(oacc[:, c0:512], V[:, kt, h * 128:(h + 1) * 128], pt_[:, c0:512],
                                kt == 0, kt == nkt - 1, [pk, vkeys[kt // 8]], [ok])
                for ui in range(max(0, len(units) - LA), len(units)):
                    kt, jd, c0 = units[ui]
                    pt_, pk = staged[ui]
                    self.mm(oacc[:, c0:512], V[:, kt, h * 128:(h + 1) * 128], pt_[:, c0:512],
                            kt == 0, kt == nkt - 1, [pk, vkeys[kt // 8]], [ok])
                self.attn_epilogue(oacc, ok, sg[b][:, qb * 512:(qb + 1) * 512], f"sg{b}",
                                   dr["mixT"][h * 64:(h + 1) * 64, qb * 512:(qb + 1) * 512], ("fox", h, qb))

    def phase_lin(self, L, kind):
        P = self.P
        dr = self.dram
        j = L // 2
        gla = kind == "gla"
        ident = self.load_const("ident", [128, 128], BF16)
        onesd = self.load_const("ones_d128", [128, 128], BF16)
        epst = self.sb("epst", [128, 1], F32)
        P.op("dve", lambda e: e.memset(epst[:], EPS), writes=["epst"])
        if gla:
            tri = self.load_const("tri", [128, 128], F32)
            resetm = self.load_const("resetm", [128, 512], F32)
            blr = self.load_const("b_lr", [128, 4], F32, src=dr["b_lr"])
            gpar = self.load_const("gla_g", [128, 8], F32, src=dr["gla_g"])
            wl32 = self.load_const("wl32", [16, 256], F32, src=dr[f"w_lr{j}"])
            wl = self.sb("wl", [16, 256], BF16)
            self.cp("dve", wl[:], wl32[:], ["k_wl32"], ["wl"])
            glr = self.load_const("glr", [16, T], BF16, src=dr["glrT"])
            one1 = self.sb("one1", [128, 1], F32)
            P.op("dve", lambda e: e.memset(one1[:], 1.0), writes=["one1"])
            negb = self.sb("negb", [128, 2], F32)
            self.tsc("dve", negb[:], blr[:, j * 2:j * 2 + 2], -1.0, ALU.mult, ["k_b_lr"], ["negb"])
            sp = self.sb("sp", [128, T], F32)
            Bc = self.sb("Bc", [128, T], F32)
            Epos = self.sb("Epos", [128, T], F32)
        else:
            dtm = self.load_const("ret_dt", [128, 512], F32)
            xi = self.load_const("ret_xi", [128, 1024], F32)
            zt = self.load_const("ret_zt", [128, 256], F32)
            gw = self.load_const("gn_w", [128, 8], F32, src=dr["gn_w"])
            gb = self.load_const("gn_b", [128, 8], F32, src=dr["gn_b"])
            gcs = self.consts["ret_gc"]
        q2 = self.sb("q2", [128, T], BF16)
        k2 = self.sb("k2", [128, T], BF16)
        qt = self.sb("qt", [128, T], BF16)
        kt2 = self.sb("kt2", [128, T], BF16) if gla else k2
        ktok = self.sb("ktok", [128, 32, 128], BF16)
        Vg = self.sb("Vg", [128, 32, 512], BF16)
        for q4 in range(4):
            self.ld(f"Vg{q4}", Vg[:, q4 * 8:(q4 + 1) * 8, :],
                    dr["vB"][q4 * 1024:(q4 + 1) * 1024, :].rearrange("(n p) c -> p n c", p=128))
        sgt = [self.sb(f"sgt{e}", [128, T], BF16) for e in range(2)]
        U = self.sb("U", [128, 128], F32)
        stb = [self.sb(f"stb{i}", [128, 128], BF16) for i in range(2)]
        NA = 4
        Asb = [self.sb(f"Asb{i}", [128, 128], BF16) for i in range(NA)]
        sq = self.sb("sq", [128, 512], BF16)
        lnv = self.sb("lnv", [128, 512], F32)
        rstd = self.sb("rstd", [128, 512], F32)
        on = self.sb("on", [128, 512], F32)
        mst = [self.sb(f"mst{i}", [128, 512], BF16) for i in range(2)]
        if not gla:
            ob = self.sb("ob", [128, 512], BF16)
            mean = self.sb("mean", [128, 512], F32)
            var = self.sb("var", [128, 512], F32)
        ps_g = self.ps("ps_g")
        ps_g2 = self.ps("ps_g2") if not gla else None
        ps_t = self.ps("ps_t", (128, 1024), BF16)
        ps_a = self.ps("ps_a")
        ps_kv = self.ps("ps_kv")
        NOB = 2 if gla else 1
        ps_o = [[self.ps(f"ps_o{e}_{i}") for i in range(NOB)] for e in range(2)]
        nms = 0
        for hp in range(2):
            rows = slice(hp * 128, (hp + 1) * 128)
            self.ld("q2", q2[:], dr["qB"][rows, :])
            self.ld("k2", k2[:], dr["kB"][rows, :])
            for e in range(2):
                h = 2 * hp + e
                mrow = (512 if gla else 0) + h * 128
                self.ld(f"sgt{e}", sgt[e][:], dr["sgT"][mrow:mrow + 128, :])
            for tb in range(NB):
                ts = slice(tb * 512, (tb + 1) * 512)
                if gla:
                    self.mm(ps_g[:], wl[0:16, rows], glr[0:16, ts], True, True, ["wl", "k_glr"], ["ps_g"])
                    self.act(sp[:, ts], ps_g[:], AF.Exp, ["ps_g", "negb"], [("sp", tb)], scale=-1.0,
                             bias=negb[:, hp:hp + 1])
                    self.act(sp[:, ts], sp[:, ts], AF.Ln, [("sp", tb), "one1"], [("sp", tb)], bias=one1[:, 0:1])
                    P.op("dve", lambda e, ts=ts: e.tensor_tensor_scan(
                        out=Bc[:, ts], data0=resetm[:], data1=sp[:, ts], initial=0.0, op0=ALU.mult, op1=ALU.add),
                        [("sp", tb), "k_resetm"], [("Bc", tb)])
                    self.act(Epos[:, ts], Bc[:, ts], AF.Exp, [("Bc", tb)], [("Epos", tb)], scale=-1.0 / 16)
                    self.act(sp[:, ts], Bc[:, ts], AF.Exp, [("Bc", tb), ("sp", tb)], [("sp", tb)], scale=1.0 / 16)
                    self.stt(qt[:, ts], q2[:, ts], 0.125, Epos[:, ts], ALU.mult, ALU.mult,
                             ["q2", ("Epos", tb)], [("qt", tb)])
                    self.tt("pool", kt2[:, ts], k2[:, ts], sp[:, ts], ALU.mult, ["k2", ("sp", tb)], [("kt2", tb)])
                    ktk = ("kt2", tb)
                else:
                    self.tt("dve", qt[:, ts], q2[:, ts], xi[:, hp * 512:(hp + 1) * 512], ALU.mult,
                            ["q2", "k_ret_xi"], [("qt", tb)])
                    ktk = "k2"
                for c in range(4):
                    n = tb * 4 + c
                    cs = slice(n * 128, (n + 1) * 128)
                    pslot = n % 4
                    self.P.op("pe", lambda e, cs=cs, pslot=pslot: e.transpose(
                        ps_t[:, pslot * 128:(pslot + 1) * 128], kt2[:, cs], ident[:]),
                        [ktk, "k_ident"], ["ps_t"])
                    if gla:
                        self.cp("act", ktok[:, n, :], ps_t[:, pslot * 128:(pslot + 1) * 128],
                                ["ps_t"], [("ktok", n)])
                    else:
                        self.tt("dve", ktok[:, n, :], ps_t[:, pslot * 128:(pslot + 1) * 128],
                                zt[:, hp * 128:(hp + 1) * 128], ALU.mult, ["ps_t", "k_ret_zt"],
                                [("ktok", n)])
            na = 0
            for n in range(32):
                tb = n // 4
                cs = slice(n * 128, (n + 1) * 128)
                ocol = slice((n % 4) * 128, (n % 4 + 1) * 128)
                for e in range(2):
                    h = 2 * hp + e
                    r = slice(64 * e, 64 * e + 64)
                    po = ps_o[e][tb % NOB]
                    pok = f"ps_o{e}_{tb % NOB}"
                    asl = slice((na % 4) * 128, (na % 4 + 1) * 128)
                    ak = "ps_a"
                    A_ = Asb[na % NA]
                    Ak = f"Asb{na % NA}"
                    na += 1
                    qk_r = [("qt", tb), ktk, "q2"]
                    if gla:
                        self.mm(ps_a[:, asl], kt2[r, cs], qt[r, cs], True, True, qk_r, [ak])
                        self.tt("dve", A_[:], ps_a[:, asl], tri[:], ALU.mult, [ak, "k_tri"], [Ak])
                    else:
                        self.mm(ps_a[:, asl], k2[r, cs], q2[r, cs], True, True, qk_r, [ak])
                        self.tt("dve", A_[:], ps_a[:, asl], dtm[:, h * 128:(h + 1) * 128], ALU.mult,
                                [ak, "k_ret_dt"], [Ak])
                    vk = f"Vg{n // 8}"
                    self.mm(po[:, ocol], Vg[:, n, h * 128:(h + 1) * 128], A_[:], True, n == 0, [Ak, vk], [pok])
                    if n > 0:
                        self.mm(po[:, ocol], stb[n % 2][r, :], qt[r, cs], False, True,
                                [(f"stb{n % 2}", e), ("qt", tb)], [pok])
                    kvsl = slice((na % 4) * 128, (na % 4 + 1) * 128)
                    kvk = "ps_kv"
                    self.mm(ps_kv[0:64, kvsl], ktok[:, n, r], Vg[:, n, h * 128:(h + 1) * 128], True, True,
                            [("ktok", n), vk], [kvk])
                    uk = ("U", e)
                    if n == 0:
                        self.cp("dve", U[r, :], ps_kv[0:64, kvsl], [kvk], [uk])
                    elif gla:
                        dcol = 128 * (n - 1) + 127
                        self.stt(U[r, :], U[r, :], Epos[r, dcol:dcol + 1], ps_kv[0:64, kvsl], ALU.mult, ALU.add,
                                 [uk, kvk, ("Epos", (n - 1) // 4)], [uk])
                    else:
                        self.stt(U[r, :], U[r, :], gcs[h], ps_kv[0:64, kvsl], ALU.mult, ALU.add, [uk, kvk], [uk])
                    if n < 31:
                        sk = (f"stb{(n + 1) % 2}", e)
                        if gla:
                            dcol = 128 * n + 127
                            self.tsc("pool", stb[(n + 1) % 2][r, :], U[r, :], Epos[r, dcol:dcol + 1], ALU.mult,
                                     [uk, ("Epos", tb)], [sk])
                        else:
                            self.cp("pool", stb[(n + 1) % 2][r, :], U[r, :], [uk], [sk])
                    if n % 4 == 3:
                        ts = slice(tb * 512, (tb + 1) * 512)
                        ms = mst[nms % 2]
                        msk = f"mst{nms % 2}"
                        nms += 1
                        dst = dr["mixT"][(512 if gla else 0) + h * 128:(512 if gla else 0) + (h + 1) * 128, ts]
                        self.act(sq[:], po[:], AF.Square, [pok], ["sq"])
                        self.mm(ps_g[:], onesd[:], sq[:], True, True, ["sq", "k_ones_d128"], ["ps_g"])
                        if gla:
                            self.act(lnv[:], ps_g[:], AF.Ln, ["ps_g", "epst"], ["lnv"], bias=epst[:, 0:1])
                            self.act(rstd[:], lnv[:], AF.Exp, ["lnv"], ["rstd"], scale=-0.5)
                            self.tt("dve", on[:], po[:], rstd[:], ALU.mult, [pok, "rstd"], ["on"])
                            self.stt(ms[:], on[:], gpar[:, j * 4 + h:j * 4 + h + 1], sgt[e][:, ts], ALU.mult, ALU.mult,
                                     ["on", "k_gla_g", f"sgt{e}"], [msk])
                        else:
                            self.cp("act", ob[:], po[:], [pok], ["ob"])
                            self.mm(ps_g2[:], onesd[:], ob[:], True, True, ["ob", "k_ones_d128"], ["ps_g2"])
                            self.cp("act", mean[:], ps_g2[:], ["ps_g2"], ["mean"])
                            self.tt("pool", var[:], mean[:], mean[:], ALU.mult, ["mean"], ["var"])
                            self.tt("dve", var[:], ps_g[:], var[:], ALU.subtract, ["ps_g", "var"], ["var"])
                            self.act(lnv[:], var[:], AF.Ln, ["var", "epst"], ["lnv"], bias=epst[:, 0:1])
                            self.act(rstd[:], lnv[:], AF.Exp, ["lnv"], ["rstd"], scale=-0.5)
                            self.tt("dve", on[:], po[:], mean[:], ALU.subtract, [pok, "mean"], ["on"])
                            self.tt("pool", on[:], on[:], rstd[:], ALU.mult, ["on", "rstd"], ["on"])
                            self.P.op("dve", lambda e_, h=h: e_.tensor_scalar(
                                out=on[:], in0=on[:], scalar1=gw[:, j * 4 + h:j * 4 + h + 1],
                                scalar2=gb[:, j * 4 + h:j * 4 + h + 1], op0=ALU.mult, op1=ALU.add),
                                ["on", "k_gn_w", "k_gn_b"], ["on"])
                            self.tt("pool", ms[:], on[:], sgt[e][:, ts], ALU.mult, ["on", f"sgt{e}"], [msk])
                        self.st(msk + "s", dst, ms[:], [msk], [("mix", kind, h, tb)])

    def phase_dil(self, L):
        P = self.P
        dr = self.dram
        ident = self.load_const("ident", [128, 128], BF16)
        nmc = self.load_const("nm_cur4", [128, 512], BF16)
        nmp = self.load_const("nm_prev4", [128, 512], BF16)
        q2 = self.sb("q2", [128, T], BF16)
        k2 = self.sb("k2", [128, T], BF16)
        V1 = self.sb("V1", [128, 32, 256], BF16)
        V4 = self.sb("V4", [128, 32, 256], BF16)
        V16 = self.sb("V16", [128, 32, 256], BF16)
        sg = [self.sb(f"sg{i}", [64, T], BF16) for i in range(2)]
        NPT = 3
        pT = [self.sb(f"pT{i}", [128, 512], BF16) for i in range(NPT)]
        self.ep_alloc()
        ps_s = [self.ps(f"ps_s{i}") for i in range(3)]
        ps_o = [self.ps(f"ps_o{i}") for i in range(4)]
        ns = 0
        for hp in range(4):
            rows = slice(hp * 128, (hp + 1) * 128)
            vc = slice(hp * 256, (hp + 1) * 256)
            self.ld("q2", q2[:], dr["qA"][rows, :])
            self.ld("k2", k2[:], dr["kA"][rows, :])
            self.ld("V1", V1[:], dr["vA"][:, vc].rearrange("(n p) c -> p n c", p=128))
            v4 = dr["vA"][:, vc].rearrange("(n i r) c -> i r n c", i=128, r=4)
            P.dma("sp", lambda e, v4=v4: [e.dma_start(out=V4[:, r * 8:(r + 1) * 8, :], in_=v4[:, r, :, :])
                                          for r in range(4)], "V4", 4, writes=["V4"])
            v16 = dr["vA"][:, vc].rearrange("(n i r) c -> i r n c", i=128, r=16)
            P.dma("sp", lambda e, v16=v16: [e.dma_start(out=V16[:, r * 2:(r + 1) * 2, :], in_=v16[:, r, :, :])
                                            for r in range(16)], "V16", 16, writes=["V16"])
            for e in range(2):
                h = 2 * hp + e
                self.ld(f"sg{e}", sg[e][:], dr["sgT"][512 + h * 64:512 + (h + 1) * 64, :])
            for e in range(2):
                h = 2 * hp + e
                r_ = slice(64 * e, 64 * e + 64)
                vh = slice(e * 128, (e + 1) * 128)
                for hf in range(2):
                    units = []
                    for gb in range(16 * hf, 16 * hf + 16):
                        bnk = (gb - 16 * hf) // 4
                        oc = slice((gb % 4) * 128, (gb % 4 + 1) * 128)
                        qc = slice(gb * 128, (gb + 1) * 128)
                        units.append((qc, qc, "cur", V1[:, gb, vh], "V1", [(bnk, oc, slice(0, 128))]))
                        if gb > 0:
                            units.append((slice((gb - 1) * 128, gb * 128), qc, "prev", V1[:, gb - 1, vh], "V1",
                                          [(bnk, oc, slice(0, 128))]))
                    for bnk in range(4):
                        n = 4 * hf + bnk
                        for r in range(4):
                            qc = slice(512 * n + r, 512 * (n + 1), 4)
                            oc = slice(r, 512, 4)
                            units.append((qc, qc, "cur", V4[:, r * 8 + n, vh], "V4", [(bnk, oc, slice(0, 128))]))
                            if n > 0:
                                units.append((slice(512 * (n - 1) + r, 512 * n, 4), qc, "prev",
                                              V4[:, r * 8 + n - 1, vh], "V4", [(bnk, oc, slice(0, 128))]))
                    for r in range(16):
                        qc = slice(2048 * hf + r, 2048 * (hf + 1), 16)
                        outs = [(bnk, slice(r, 512, 16), slice(32 * bnk, 32 * bnk + 32)) for bnk in range(4)]
                        units.append((qc, qc, "cur", V16[:, r * 2 + hf, vh], "V16", outs))
                        if hf > 0:
                            units.append((slice(r, 2048, 16), qc, "prev", V16[:, r * 2, vh], "V16", outs))
                    units = [u for u in units if u[2] == "cur"] + [u for u in units if u[2] == "prev"]
                    groups = []
                    for mk_ in ("cur", "prev"):
                        us = [u for u in units if u[2] == mk_]
                        for g0 in range(0, len(us), 4):
                            groups.append(us[g0:g0 + 4])
                    seq = []
                    for ui, u in enumerate(units):
                        for (bnk, oc, pc) in u[5]:
                            seq.append((ui, bnk))
                    first = {}
                    lastm = {}
                    for si, (ui, bnk) in enumerate(seq):
                        first.setdefault(bnk, si)
                        lastm[bnk] = si
                    si = 0
                    for grp in groups:
                        sp_ = ps_s[ns % 3]
                        sk = f"ps_s{ns % 3}"
                        pt_ = pT[ns % NPT]
                        pk = f"pT{ns % NPT}"
                        ns += 1
                        for gi, (kc, qc, mask, vt, vkey, outs) in enumerate(grp):
                            cs = slice(gi * 128, (gi + 1) * 128)
                            self.mm(sp_[:, cs], k2[r_, kc], q2[r_, qc], gi == 0, False, ["q2", "k2"], [sk])
                        w = 128 * len(grp)
                        self.mm(sp_[:, 0:w], ident[:], (nmc if grp[0][2] == "cur" else nmp)[:, 0:w], False, True,
                                ["k_ident", "k_nm_cur4", "k_nm_prev4"], [sk])
                        self.act(pt_[:, 0:w], sp_[:, 0:w], AF.Exp, [sk], [pk])
                        for gi, (kc, qc, mask, vt, vkey, outs) in enumerate(grp):
                            for (bnk, oc, pc) in outs:
                                pcs = slice(gi * 128 + pc.start, gi * 128 + pc.stop)
                                self.mm(ps_o[bnk][:, oc], vt, pt_[:, pcs], si == first[bnk], si == lastm[bnk],
                                        [pk, vkey], [f"ps_o{bnk}"])
                                si += 1
                    for bnk in range(4):
                        ts = slice(2048 * hf + 512 * bnk, 2048 * hf + 512 * (bnk + 1))
                        self.attn_epilogue(ps_o[bnk], f"ps_o{bnk}", sg[e][:, ts], f"sg{e}",
                                           dr["mixT"][512 + h * 64:512 + (h + 1) * 64, ts], ("dil", h, hf, bnk))


def make_in_maps(inputs, consts):
    x = np.asarray(inputs["x"], np.float32)
    common = {}
    for j in range(2):
        common[f"w_in_e{j}"] = np.ascontiguousarray(inputs["w_in_even"][j], np.float32)
        common[f"w_in_o{j}"] = np.ascontiguousarray(inputs["w_in_odd"][j], np.float32)
        common[f"w_out_e{j}"] = np.ascontiguousarray(inputs["w_out_even"][j], np.float32)
        common[f"w_out_o{j}"] = np.ascontiguousarray(inputs["w_out_odd"][j], np.float32)
        common[f"w_lr{j}"] = np.ascontiguousarray(inputs["w_lr_even"][j], np.float32)
    gains = [inputs["norm_even"][0], inputs["norm_odd"][0], inputs["norm_even"][1], inputs["norm_odd"][1],
             inputs["final_norm"]]
    ng = np.stack([np.asarray(g, np.float32).reshape(8, 128).T for g in gains], axis=1)
    common["normg"] = np.ascontiguousarray(ng.reshape(128, 40))
    common["b_f"] = np.ascontiguousarray(np.asarray(inputs["b_f_even"], np.float32).T)
    blr = np.asarray(inputs["b_lr_even"], np.float32).reshape(2, 2, 128)
    common["b_lr"] = np.ascontiguousarray(blr.transpose(2, 0, 1).reshape(128, 4))
    for nm, key in (("gla_g", "gla_norm_even"), ("gn_w", "ret_gn_w_odd"), ("gn_b", "ret_gn_b_odd")):
        a = np.asarray(inputs[key], np.float32).reshape(2, 4, 128)
        common[nm] = np.ascontiguousarray(a.transpose(2, 0, 1).reshape(128, 8))
    for n in CONST_NAMES:
        common["c_" + n] = consts[n]
    maps = []
    for b in range(8):
        m = dict(common)
        m["xT"] = np.ascontiguousarray(x[b].T)
        maps.append(m)
    return maps


_CACHE = {}


def kernel(**inputs):
    if "b" not in _CACHE:
        b = Builder()
        b.build()
        _CACHE["b"] = b
    b = _CACHE["b"]
    maps = make_in_maps(inputs, b.consts)
    res = run_bass_kernel_spmd(b.nc, maps, core_ids=list(range(8)))
    out = np.stack([np.ascontiguousarray(r["yT"].T) for r in res.results], axis=0)
    return out.astype(np.float32)
```

```python
import numpy as np
import ml_dtypes
from contextlib import ExitStack
import concourse.bass as bass
import concourse.mybir as mybir
from concourse.bass_utils import run_bass_kernel_spmd

F32 = mybir.dt.float32
BF16 = mybir.dt.bfloat16
AF = mybir.ActivationFunctionType
ALU = mybir.AluOpType
NPBF = ml_dtypes.bfloat16

T = 4096
D = 1024
NB = 8
EVEN_IN = 3608
ODD_IN = 3584
EPS = 1e-6
NEG = -30000.0


class Prog:
    STREAMS = ("pe", "act", "dve", "pool", "sp")
    SECT = {"pe": "tensor", "act": "scalar", "dve": "vector", "pool": "gpsimd", "sp": "sync"}
    CAP = 20000

    def __init__(self, nc, es):
        self.nc = nc
        self.es = es
        self.sems = {s: [] for s in self.STREAMS}
        self.cnt = {s: 0 for s in self.STREAMS}
        self.dsem = {}
        self.waited = {s: {} for s in self.STREAMS}
        self.reset()

    def reset(self):
        self.ops = []
        self.lw = {}
        self.rd = {}

    def _add(self, stream, fn, reads, writes, dma=None, ndma=0):
        idx = len(self.ops)
        deps = {}
        for k in reads:
            w = self.lw.get(k)
            if w is not None:
                deps[w] = True
        for k in writes:
            w = self.lw.get(k)
            if w is not None:
                deps.setdefault(w, False)
            for r in self.rd.get(k, ()):
                deps.setdefault(r, False)
        for k in reads:
            self.rd.setdefault(k, []).append(idx)
        for k in writes:
            self.lw[k] = idx
            self.rd[k] = []
        self.ops.append(dict(s=stream, fn=fn, deps=deps, dma=dma, ndma=ndma, need=False, c=0))
        return idx

    def op(self, stream, fn, reads=(), writes=()):
        return self._add(stream, fn, tuple(reads), tuple(writes))

    def dma(self, stream, fn, key, n, reads=(), writes=()):
        return self._add(stream, fn, tuple(reads), tuple(writes), dma=key, ndma=n)

    def _sem(self, stream, i):
        lst = self.sems[stream]
        while len(lst) <= i:
            lst.append(self.es.enter_context(self.nc.semaphore(f"s_{stream}_{len(lst)}")))
        return lst[i]

    def finalize_and_emit(self, block):
        ops = self.ops
        for o in ops:
            w = []
            for d, raw in o["deps"].items():
                p = ops[d]
                if p["dma"] is not None:
                    w.append(d)
                elif o["dma"] is not None:
                    w.append(d)
                elif p["s"] == o["s"]:
                    if o["s"] != "pe":
                        w.append(d)
                else:
                    w.append(d)
            o["w"] = w
            for d in w:
                ops[d]["need"] = True
        for o in ops:
            if o["dma"] is not None:
                if o["dma"] not in self.dsem:
                    self.dsem[o["dma"]] = [self.es.enter_context(self.nc.semaphore("d_" + o["dma"])), 0]
                self.dsem[o["dma"]][1] += 16 * o["ndma"]
                o["c"] = self.dsem[o["dma"]][1]
            elif o["need"]:
                self.cnt[o["s"]] += 1
                o["c"] = self.cnt[o["s"]]
        final_d = {k: v[1] for k, v in self.dsem.items()}

        def target(p):
            if p["dma"] is not None:
                return ("D" + p["dma"], self.dsem[p["dma"]][0], p["c"])
            c = p["c"]
            i = (c - 1) // self.CAP
            return (p["s"] + str(i), self._sem(p["s"], i), (c - 1) % self.CAP + 1)

        for s in self.STREAMS:
            ops_s = [o for o in ops if o["s"] == s]
            if not ops_s and s != "sp":
                continue

            def body(eng, ops_s=ops_s, s=s):
                wt = self.waited[s]
                for o in ops_s:
                    tg = {}
                    for d in o["w"]:
                        name, sem, val = target(ops[d])
                        if wt.get(name, 0) >= val:
                            continue
                        if name not in tg or tg[name][1] < val:
                            tg[name] = (sem, val)
                    for name, (sem, val) in tg.items():
                        eng.wait_ge(sem, val)
                        wt[name] = val
                    r = o["fn"](eng)
                    if o["dma"] is not None:
                        sem = self.dsem[o["dma"]][0]
                        assert len(r) == o["ndma"], (len(r), o["ndma"])
                        for ins in r:
                            ins.then_inc(sem, 16)
                    elif o["need"]:
                        c = o["c"]
                        r.then_inc(self._sem(s, (c - 1) // self.CAP), 1)
                if s == "sp":
                    for k, v in final_d.items():
                        if wt.get("D" + k, 0) < v:
                            eng.wait_ge(self.dsem[k][0], v)
                            wt["D" + k] = v

            getattr(block, self.SECT[s])(body)
        self.reset()


def _consts():
    c = {}
    p = np.arange(128)
    c["ident"] = np.eye(128, dtype=np.float32).astype(NPBF)
    c["ones"] = np.ones((128, 128), np.float32).astype(NPBF)
    c["ones_d128"] = np.full((128, 128), 1.0 / 128, np.float32).astype(NPBF)
    c["nm_cur"] = np.where(p[:, None] > p[None, :], NEG, 0.0).astype(np.float32).astype(NPBF)
    c["nm_prev"] = np.where(p[:, None] < p[None, :], NEG, 0.0).astype(np.float32).astype(NPBF)
    c["nm_cur4"] = np.tile(c["nm_cur"], (1, 4))
    c["nm_prev4"] = np.tile(c["nm_prev"], (1, 4))
    c["tri"] = np.where(p[:, None] <= p[None, :], 1.0, 0.0).astype(np.float32)
    inv = np.power(np.float32(10000.0), -np.arange(0, 64, 2, dtype=np.float32) / np.float32(64)).astype(np.float32)
    ang = (np.arange(T, dtype=np.float32)[None, :] * inv[p % 32][:, None]).astype(np.float32)
    c["cos"] = np.cos(ang.astype(np.float64)).astype(np.float32)
    c["sin"] = np.sin(ang.astype(np.float64)).astype(np.float32)
    perm = np.zeros((128, 128), np.float32)
    for m in range(128):
        if m % 64 < 32:
            perm[m + 32, m] = -1.0
        else:
            perm[m - 32, m] = 1.0
    c["perm"] = perm.astype(NPBF)
    lg = np.log(1.0 - np.power(2.0, -5.0 - np.arange(4, dtype=np.float64)))
    idx = np.arange(128, dtype=np.float64)
    dt = np.zeros((128, 4, 128), np.float32)
    for h in range(4):
        diff = idx[None, :] - idx[:, None]
        dt[:, h, :] = np.where(diff >= 0, np.exp(np.maximum(diff, 0) * lg[h]), 0.0)
    c["ret_dt"] = dt.reshape(128, 512)
    c["ret_dt4"] = np.ascontiguousarray(np.tile(dt[:, :, None, :], (1, 1, 4, 1)).reshape(128, 2048))
    c["tri4"] = np.tile(c["tri"], (1, 4))
    xi = np.zeros((128, 2, 512), np.float32)
    zt = np.zeros((128, 2, 128), np.float32)
    for hp in range(2):
        for e in range(2):
            h = 2 * hp + e
            xi[64 * e:64 * e + 64, hp, :] = np.tile(np.exp((idx + 1.0) * lg[h]), 4)[None, :]
            zt[:, hp, 64 * e:64 * e + 64] = np.exp((127.0 - idx) * lg[h])[:, None]
    c["ret_xi"] = xi.reshape(128, 1024)
    c["ret_zt"] = zt.reshape(128, 256)
    c["ret_zt4"] = np.ascontiguousarray(np.tile(zt[:, :, None, :], (1, 1, 4, 1)).reshape(128, 1024))
    c["ret_gc"] = [float(np.exp(128.0 * lg[h])) for h in range(4)]
    rm = np.ones((128, 512), np.float32)
    rm[:, ::128] = 0.0
    c["resetm"] = rm
    return c


CONST_NAMES = ["ident", "ones", "ones_d128", "nm_cur", "nm_prev", "nm_cur4", "nm_prev4", "tri", "cos", "sin", "perm",
               "ret_dt", "ret_xi", "ret_zt", "resetm", "ret_dt4", "ret_zt4", "tri4"]


class Builder:
    def __init__(self, n_layers=4, debug_out=()):
        self.n_layers = n_layers
        self.debug_out = tuple(debug_out)
        self.consts = _consts()
        self.nc = bass.Bass("TRN2", target_bir_lowering=False)
        self.es = ExitStack()
        self.P = Prog(self.nc, self.es)
        self.dram = {}

    def din(self, name, shape, dt):
        self.dram[name] = self.nc.dram_tensor(name, list(shape), dt, kind="ExternalInput").ap()
        return self.dram[name]

    def dscr(self, name, shape, dt):
        kind = "ExternalOutput" if name in self.debug_out else "Internal"
        self.dram[name] = self.nc.dram_tensor(name, list(shape), dt, kind=kind).ap()
        return self.dram[name]

    def declare(self):
        nc = self.nc
        self.din("xT", [D, T], F32)
        for j in range(2):
            self.din(f"w_in_e{j}", [D, EVEN_IN], F32)
            self.din(f"w_in_o{j}", [D, ODD_IN], F32)
            self.din(f"w_out_e{j}", [D, D], F32)
            self.din(f"w_out_o{j}", [D, D], F32)
            self.din(f"w_lr{j}", [16, 256], F32)
        self.din("normg", [128, 5 * 8], F32)
        self.din("b_f", [8, 2], F32)
        self.din("b_lr", [128, 4], F32)
        self.din("gla_g", [128, 8], F32)
        self.din("gn_w", [128, 8], F32)
        self.din("gn_b", [128, 8], F32)
        for n in CONST_NAMES:
            a = self.consts[n]
            self.din("c_" + n, a.shape, BF16 if a.dtype == NPBF else F32)
        self.dram["yT"] = nc.dram_tensor("yT", [D, T], F32, kind="ExternalOutput").ap()
        self.dscr("h0", [D, T], F32)
        self.dscr("h1", [D, T], F32)
        self.dscr("qA", [512, T], BF16)
        self.dscr("kA", [512, T], BF16)
        self.dscr("qB", [256, T], BF16)
        self.dscr("kB", [256, T], BF16)
        self.dscr("ffT", [8, T], F32)
        self.dscr("glrT", [16, T], BF16)
        self.dscr("sgT", [D, T], BF16)
        self.dscr("vA", [T, 1024], BF16)
        self.dscr("vB", [T, 512], BF16)
        self.dscr("mixT", [D, T], BF16)
        self.dscr("caq", [8, 6, T], BF16)
        self.dscr("cak", [8, 6, T], BF16)

    def phase(self, fn):
        nc = self.nc
        self.phase_no = getattr(self, "phase_no", -1) + 1
        with ExitStack() as pes:
            self.pes = pes
            self.tiles = {}
            fn()
            with nc.Block() as block:
                self.P.finalize_and_emit(block)

    def sb(self, name, shape, dt):
        t = self.pes.enter_context(self.nc.sbuf_tensor(f"p{self.phase_no}_{name}", list(shape), dt))
        return t

    def ps(self, name, shape=(128, 512), dt=F32):
        return self.pes.enter_context(self.nc.psum_tensor(f"p{self.phase_no}_{name}", list(shape), dt))

    def load_const(self, name, shape, dt, src=None):
        t = self.sb("k_" + name, shape, dt)
        src = self.dram["c_" + name] if src is None else src
        self.P.dma("sp", lambda e, t=t, src=src: [e.dma_start(out=t[:], in_=src)], "k_" + name, 1,
                   writes=["k_" + name])
        return t

    def phase_ca(self, L):
        P = self.P
        dr = self.dram
        last = (L == self.n_layers)
        odd = (L % 2 == 1)
        j = L // 2
        hsrc = dr["xT"] if L <= 1 else dr[f"h{(L - 1) % 2}"]
        hdst = dr[f"h{L % 2}"]
        NIN = ODD_IN if odd else EVEN_IN

        ones = self.load_const("ones", [128, 128], BF16)
        normg = self.load_const("normg", [128, 40], F32, src=dr["normg"])
        epst = self.sb("epst", [128, 1], F32)
        P.op("dve", lambda e: e.memset(epst[:], EPS), writes=["epst"])
        if odd and not last:
            cosb = [self.sb(f"cosb{i}", [128, 512], F32) for i in range(2)]
            sinb = [self.sb(f"sinb{i}", [128, 512], F32) for i in range(2)]
            perm = self.load_const("perm", [128, 128], BF16)

        wst = [self.sb(f"wst{i}", [128, 1024], F32) for i in range(2)]
        nst = [0]

        def load_w(dst, src, ncols, nm):
            for c0 in range(0, ncols, 1024):
                for ic in range(8):
                    cw = min(1024, ncols - c0)
                    i = nst[0] % 2
                    nst[0] += 1
                    st = wst[i]
                    P.dma("sp", lambda e, st=st, ic=ic, c0=c0, cw=cw: [e.dma_start(
                        out=st[:, 0:cw], in_=src[ic * 128:(ic + 1) * 128, c0:c0 + cw])],
                        f"wst{i}", 1, writes=[f"wst{i}"])
                    eng = "pool" if (nst[0] % 2) else "dve"
                    P.op(eng, lambda e, st=st, ic=ic, c0=c0, cw=cw: e.tensor_copy(
                        out=dst[:, ic, c0:c0 + cw], in_=st[:, 0:cw]),
                        reads=[f"wst{i}"], writes=[(nm, ic, c0 // 1024)])

        if L > 0:
            wout = self.sb("wout", [128, 8, D], BF16)
            load_w(wout, dr[f"w_out_{'e' if (L - 1) % 2 == 0 else 'o'}{(L - 1) // 2}"], D, "wout")
        if not last:
            win = self.sb("win", [128, 8, NIN], BF16)
            load_w(win, dr[f"w_in_{'o' if odd else 'e'}{j}"], NIN, "win")

        hT = [self.sb(f"hT{i}", [128, 8, 512], F32) for i in range(2)]
        mg = [self.sb(f"mg{i}", [128, 8, 512], BF16) for i in range(1)] * 2 if L > 0 else None
        sq = self.sb("sq", [128, 8, 512], BF16)
        lnt = self.sb("lnt", [128, 512], F32)
        rstd = self.sb("rstd", [128, 512], F32)
        uT = [self.sb(f"uT{i}", [128, 8, 512], BF16) for i in range(2)] if not last else None
        NST = 4
        stg = [self.sb(f"stg{i}", [128, 512], BF16) for i in range(NST)]
        stf = self.sb("stf", [128, 512], F32)
        if odd and not last:
            zb = [self.sb(f"zb{i}", [128, 512], BF16) for i in range(2)]
            t1 = [self.sb(f"t1_{i}", [128, 512], F32) for i in range(2)]
            t2 = [self.sb(f"t2_{i}", [128, 512], F32) for i in range(2)]
        if last:
            yst = [self.sb(f"yst{i}", [128, 512], F32) for i in range(2)]
        else:
            stv = [self.sb(f"stv{i}", [128, 8, 128], BF16) for i in range(2)]
            for i in range(2):
                P.op("pool", lambda e, i=i: e.memset(stv[i][:], 1.0), writes=[f"stv{i}"])
        ps_ss = self.ps("ps_ss")
        ps_o = [self.ps(f"ps_o{i}") for i in range(2)]
        ps_z = [self.ps(f"ps_z{i}") for i in range(3)]
        ps_sw = [self.ps(f"ps_sw{i}") for i in range(2)] if (odd and not last) else None

        hv = hsrc.rearrange("(c p) t -> p c t", p=128)
        hdv = hdst.rearrange("(c p) t -> p c t", p=128)
        mv = dr["mixT"].rearrange("(c p) t -> p c t", p=128)
        yv = dr["yT"].rearrange("(c p) t -> p c t", p=128)

        if not last:
            if not odd:
                fm = []
                for c in range(4):
                    fm.append((c * 128, 128, "plain", 0.125, dr["qA"][c * 128:(c + 1) * 128, :]))
                for c in range(4):
                    fm.append((512 + c * 128, 128, "plain", 1.0, dr["kA"][c * 128:(c + 1) * 128, :]))
                fm.append((1536, 8, "f32", 1.0, dr["ffT"][0:8, :]))
                for c in range(2):
                    fm.append((1544 + c * 128, 128, "plain", 1.0, dr["qB"][c * 128:(c + 1) * 128, :]))
                for c in range(2):
                    fm.append((1800 + c * 128, 128, "plain", 1.0, dr["kB"][c * 128:(c + 1) * 128, :]))
                fm.append((2568, 16, "plain", 1.0, dr["glrT"][0:16, :]))
                for c in range(8):
                    fm.append((2584 + c * 128, 128, "silu", 1.0, dr["sgT"][c * 128:(c + 1) * 128, :]))
                tm = [(1024, dr["vA"]), (2056, dr["vB"])]
            else:
                fm = []
                for c in range(2):
                    fm.append((c * 128, 128, "rope", 0.125, dr["qB"][c * 128:(c + 1) * 128, :]))
                for c in range(2):
                    fm.append((256 + c * 128, 128, "rope", 1.0, dr["kB"][c * 128:(c + 1) * 128, :]))
                for c in range(4):
                    fm.append((1024 + c * 128, 128, "rope", 0.125, dr["qA"][c * 128:(c + 1) * 128, :]))
                for c in range(4):
                    fm.append((1536 + c * 128, 128, "rope", 1.0, dr["kA"][c * 128:(c + 1) * 128, :]))
                for c in range(8):
                    fm.append((2560 + c * 128, 128, "silu", 1.0, dr["sgT"][c * 128:(c + 1) * 128, :]))
                tm = [(512, dr["vB"]), (2048, dr["vA"])]

        cnt = dict(z=0, st=0, o=0, rp=0, sv=0)
        for tb in range(NB):
            ts = slice(tb * 512, (tb + 1) * 512)
            h = hT[tb % 2]
            hk = f"hT{tb % 2}"
            P.dma("sp", lambda e, h=h, ts=ts: [e.dma_start(out=h[:, 0:4, :], in_=hv[:, 0:4, ts]),
                                               e.dma_start(out=h[:, 4:8, :], in_=hv[:, 4:8, ts])],
                  hk, 2, writes=[hk])
            if L > 0:
                m = mg[tb % 2]
                mk = "mg0"
                P.dma("sp", lambda e, m=m, ts=ts: [e.dma_start(out=m[:], in_=mv[:, :, ts])], mk, 1,
                      writes=[mk])
                for oc in range(8):
                    po = ps_o[cnt["o"] % 2]
                    pk = f"ps_o{cnt['o'] % 2}"
                    cnt["o"] += 1
                    for mc in range(8):
                        P.op("pe", lambda e, po=po, mc=mc, oc=oc, m=m: e.matmul(
                            po[:], lhsT=wout[:, mc, oc * 128:(oc + 1) * 128], rhs=m[:, mc, :],
                            start=(mc == 0), stop=(mc == 7)),
                            reads=[mk, ("wout", mc, 0)], writes=[pk])
                    P.op("dve", lambda e, po=po, oc=oc, h=h: e.tensor_tensor(
                        out=h[:, oc, :], in0=h[:, oc, :], in1=po[:], op=ALU.add),
                        reads=[pk, hk], writes=[hk])
                if not last:
                    P.dma("pool", lambda e, h=h, ts=ts: [e.dma_start(out=hdv[:, :, ts], in_=h[:])],
                          hk + "s", 1, reads=[hk], writes=[("hdst", tb)])
            P.op("act", lambda e, h=h: e.activation(out=sq[:], in_=h[:], func=AF.Square),
                 reads=[hk], writes=["sq"])
            for c in range(8):
                P.op("pe", lambda e, c=c: e.matmul(ps_ss[:], lhsT=ones[:], rhs=sq[:, c, :],
                                                   start=(c == 0), stop=(c == 7)),
                     reads=["sq", "k_ones"], writes=["ps_ss"])
            P.op("act", lambda e: e.activation(out=lnt[:], in_=ps_ss[:], func=AF.Ln,
                                               scale=1.0 / D, bias=epst[:, 0:1]),
                 reads=["ps_ss", "epst"], writes=["lnt"])
            P.op("act", lambda e: e.activation(out=rstd[:], in_=lnt[:], func=AF.Exp, scale=-0.5),
                 reads=["lnt"], writes=["rstd"])
            if last:
                for c in range(8):
                    y = yst[c % 2]
                    yk = f"yst{c % 2}"
                    P.op("dve", lambda e, y=y, c=c, h=h: e.scalar_tensor_tensor(
                        out=y[:], in0=h[:, c, :], scalar=normg[:, L * 8 + c:L * 8 + c + 1], in1=rstd[:],
                        op0=ALU.mult, op1=ALU.mult),
                        reads=[hk, "rstd", "k_normg"], writes=[yk])
                    P.dma("pool", lambda e, y=y, c=c, ts=ts: [e.dma_start(out=yv[:, c, ts], in_=y[:])],
                          yk + "s", 1, reads=[yk], writes=[("y", tb, c)])
                continue
            u = uT[tb % 2]
            uk = f"uT{tb % 2}"
            for c in range(8):
                P.op("dve", lambda e, u=u, c=c, h=h: e.scalar_tensor_tensor(
                    out=u[:, c, :], in0=h[:, c, :], scalar=normg[:, L * 8 + c:L * 8 + c + 1], in1=rstd[:],
                    op0=ALU.mult, op1=ALU.mult),
                    reads=[hk, "rstd", "k_normg"], writes=[(uk, c)])
            ukeys = [(uk, c) for c in range(8)]
            if odd:
                cb = tb % 2
                self.ld(f"cosb{cb}", cosb[cb][:], dr["c_cos"][:, ts])
                self.ld(f"sinb{cb}", sinb[cb][:], dr["c_sin"][:, ts])
            pending = []
            for (c0, M, kind, scale, dst) in fm:
                pz = ps_z[cnt["z"] % 3]
                zk = f"ps_z{cnt['z'] % 3}"
                cnt["z"] += 1
                for ic in range(8):
                    P.op("pe", lambda e, pz=pz, ic=ic, c0=c0, M=M, u=u: e.matmul(
                        pz[0:M, :], lhsT=win[:, ic, c0:c0 + M], rhs=u[:, ic, :],
                        start=(ic == 0), stop=(ic == 7)),
                        reads=[(uk, ic)] + [("win", ic, g) for g in range(c0 // 1024, (c0 + M - 1) // 1024 + 1)],
                        writes=[zk])
                while pending:
                    pending.pop(0)()
                if kind == "f32":
                    P.op("act", lambda e, pz=pz, M=M: e.activation(out=stf[0:M, :], in_=pz[0:M, :], func=AF.Copy),
                         reads=[zk], writes=["stf"])
                    P.dma("pool", lambda e, M=M, dst=dst, ts=ts: [e.dma_start(out=dst[:, ts], in_=stf[0:M, :])],
                          "stfs", 1, reads=["stf"], writes=[("fm", c0, tb)])
                    continue
                if kind in ("plain", "silu"):
                    s = stg[cnt["st"] % NST]
                    sk = f"stg{cnt['st'] % NST}"
                    cnt["st"] += 1
                    if kind == "plain":
                        P.op("act", lambda e, pz=pz, M=M, s=s, scale=scale: e.activation(
                            out=s[0:M, :], in_=pz[0:M, :], func=AF.Copy, scale=scale),
                            reads=[zk], writes=[sk])
                    else:
                        P.op("act", lambda e, pz=pz, M=M, s=s: e.activation(
                            out=s[0:M, :], in_=pz[0:M, :], func=AF.Silu),
                            reads=[zk], writes=[sk])
                    P.dma("pool", lambda e, M=M, dst=dst, ts=ts, s=s: [e.dma_start(out=dst[:, ts], in_=s[0:M, :])],
                          sk + "s", 1, reads=[sk], writes=[("fm", c0, tb)])
                    continue
                r = cnt["rp"] % 2
                cnt["rp"] += 1
                P.op("act", lambda e, pz=pz, r=r, scale=scale: e.activation(
                    out=zb[r][:], in_=pz[:], func=AF.Copy, scale=scale),
                    reads=[zk], writes=[f"zb{r}"])

                def tail(r=r, dst=dst, c0=c0, cb=cb, ts=ts, tb=tb):
                    P.op("pe", lambda e: e.matmul(ps_sw[r][:], lhsT=perm[:], rhs=zb[r][:], start=True, stop=True),
                         reads=[f"zb{r}", "k_perm"], writes=[f"ps_sw{r}"])
                    P.op("pool", lambda e: e.tensor_tensor(out=t1[r][:], in0=zb[r][:], in1=cosb[cb][:], op=ALU.mult),
                         reads=[f"zb{r}", f"cosb{cb}"], writes=[f"t1_{r}"])
                    P.op("dve", lambda e: e.tensor_tensor(out=t2[r][:], in0=ps_sw[r][:], in1=sinb[cb][:], op=ALU.mult),
                         reads=[f"ps_sw{r}", f"sinb{cb}"], writes=[f"t2_{r}"])
                    s_ = stg[cnt["st"] % NST]
                    sk_ = f"stg{cnt['st'] % NST}"
                    cnt["st"] += 1
                    P.op("dve", lambda e: e.tensor_tensor(out=s_[:], in0=t1[r][:], in1=t2[r][:], op=ALU.add),
                         reads=[f"t1_{r}", f"t2_{r}"], writes=[sk_])
                    P.dma("pool", lambda e: [e.dma_start(out=dst[:, ts], in_=s_[:])],
                          sk_ + "s", 1, reads=[sk_], writes=[("fm", c0, tb)])
                pending.append(tail)
            while pending:
                pending.pop(0)()
            for jt in range(4):
                tile = tb * 4 + jt
                for (c0, dst) in tm:
                    pz = ps_z[cnt["z"] % 3]
                    zk = f"ps_z{cnt['z'] % 3}"
                    cnt["z"] += 1
                    for ic in range(8):
                        P.op("pe", lambda e, pz=pz, ic=ic, c0=c0, u=u, jt=jt: e.matmul(
                            pz[:], lhsT=u[:, ic, jt * 128:(jt + 1) * 128], rhs=win[:, ic, c0:c0 + 512],
                            start=(ic == 0), stop=(ic == 7)),
                            reads=[(uk, ic)] + [("win", ic, g) for g in range(c0 // 1024, (c0 + 511) // 1024 + 1)],
                            writes=[zk])
                    if dst is dr["vA"]:
                        vi = cnt["sv"] % 2
                        cnt["sv"] += 1
                        P.op("dve", lambda e, pz=pz, vi=vi: e.tensor_copy(
                            out=stv[vi][:, :, 0:64], in_=pz[:].rearrange("p (h d) -> p h d", d=64)),
                            reads=[zk], writes=[f"stv{vi}"])
                        P.dma("pool", lambda e, dst=dst, tile=tile, vi=vi: [e.dma_start(
                            out=dst[tile * 128:(tile + 1) * 128, :],
                            in_=stv[vi][:].rearrange("p h d -> p (h d)"))],
                            f"stv{vi}s", 1, reads=[f"stv{vi}"], writes=[("tm", c0, tile)])
                        continue
                    s = stg[cnt["st"] % NST]
                    sk = f"stg{cnt['st'] % NST}"
                    cnt["st"] += 1
                    P.op("dve", lambda e, pz=pz, s=s: e.tensor_copy(out=s[:], in_=pz[:]),
                         reads=[zk], writes=[sk])
                    P.dma("pool", lambda e, dst=dst, tile=tile, s=s: [e.dma_start(
                        out=dst[tile * 128:(tile + 1) * 128, :], in_=s[:])],
                        sk + "s", 1, reads=[sk], writes=[("tm", c0, tile)])

    def build(self, nph=None):
        self.declare()
        phases = []
        for L in range(self.n_layers + 1):
            phases.append(lambda L=L: self.phase_ca(L))
            if L == self.n_layers:
                break
            if L % 2 == 0:
                phases.append(lambda L=L: self.phase_even_c(L))
                phases.append(lambda L=L: self.phase_fox(L))
                phases.append(lambda L=L: self.phase_lin(L, "gla"))
            else:
                phases.append(lambda L=L: self.phase_lin(L, "ret"))
                phases.append(lambda L=L: self.phase_dil(L))
        sel = phases[:nph] if not isinstance(nph, (list, tuple)) else [phases[i] for i in nph]
        for ph in sel:
            self.phase(ph)
        return self.nc


    def mm(self, out, lhsT, rhs, start, stop, reads, writes):
        self.P.op("pe", lambda e: e.matmul(out, lhsT=lhsT, rhs=rhs, start=start, stop=stop), reads, writes)

    def act(self, out, in_, func, reads, writes, scale=None, bias=None):
        kw = {}
        if scale is not None:
            kw["scale"] = scale
        if bias is not None:
            kw["bias"] = bias
        self.P.op("act", lambda e: e.activation(out=out, in_=in_, func=func, **kw), reads, writes)

    def tt(self, eng, out, in0, in1, op, reads, writes):
        self.P.op(eng, lambda e: e.tensor_tensor(out=out, in0=in0, in1=in1, op=op), reads, writes)

    def tsc(self, eng, out, in0, scalar, op, reads, writes):
        self.P.op(eng, lambda e: e.tensor_scalar(out=out, in0=in0, scalar1=scalar, scalar2=None, op0=op), reads, writes)

    def stt(self, out, in0, scalar, in1, op0, op1, reads, writes):
        self.P.op("dve", lambda e: e.scalar_tensor_tensor(out=out, in0=in0, scalar=scalar, in1=in1, op0=op0, op1=op1),
                  reads, writes)

    def cp(self, eng, out, in_, reads, writes):
        if eng == "act":
            self.P.op(eng, lambda e: e.copy(out=out, in_=in_), reads, writes)
        else:
            self.P.op(eng, lambda e: e.tensor_copy(out=out, in_=in_), reads, writes)

    def ld(self, key, out, in_, writes=None, reads=(), eng="sp"):
        self.P.dma(eng, lambda e: [e.dma_start(out=out, in_=in_)], key, 1, reads=reads,
                   writes=[key] if writes is None else writes)

    def st(self, key, out, in_, reads, writes, eng="pool"):
        self.P.dma(eng, lambda e: [e.dma_start(out=out, in_=in_)], key, 1, reads=reads, writes=writes)

    def phase_even_c(self, L):
        P = self.P
        dr = self.dram
        j = L // 2
        A = self.sb("cA", [8, T], F32)
        o1 = self.sb("c1", [8, T], F32)
        Pc = self.sb("cP", [8, T], F32)
        r1 = self.sb("cr", [8, T], F32)
        cq = self.sb("cq", [8, 6, T], BF16)
        ck = self.sb("ck", [8, 6, T], BF16)
        bf = self.sb("bf", [8, 2], F32)
        negb = self.sb("negb", [8, 1], F32)
        one1 = self.sb("one1", [8, 1], F32)
        self.ld("cA", A[:], dr["ffT"][:, :])
        self.ld("bf", bf[:], dr["b_f"][:, :])
        self.tsc("dve", negb[:], bf[:, j:j + 1], -1.0, ALU.mult, ["bf"], ["negb"])
        P.op("dve", lambda e: e.memset(one1[:], 1.0), writes=["one1"])
        P.op("pool", lambda e: e.memset(o1[:], 1.0), writes=["c1"])
        P.op("pool", lambda e: e.memset(cq[:, 3:6, :], 1.0), writes=["cq1"])
        P.op("pool", lambda e: e.memset(ck[:, 0:3, :], 1.0), writes=["ck1"])
        self.act(A[:], A[:], AF.Exp, ["cA", "negb"], ["cA"], scale=-1.0, bias=negb[:, 0:1])
        self.act(A[:], A[:], AF.Ln, ["cA", "one1"], ["cA"], bias=one1[:, 0:1])
        P.op("dve", lambda e: e.tensor_tensor_scan(out=Pc[:], data0=o1[:], data1=A[:], initial=0.0,
                                                   op0=ALU.mult, op1=ALU.add), ["c1", "cA"], ["cP"])
        self.cp("dve", ck[:, 3, :], Pc[:], ["cP"], ["ck3"])
        self.tt("dve", r1[:], Pc[:], ck[:, 3, :], ALU.subtract, ["cP", "ck3"], ["cr"])
        self.cp("dve", ck[:, 4, :], r1[:], ["cr"], ["ck4"])
        self.tt("dve", A[:], r1[:], ck[:, 4, :], ALU.subtract, ["cr", "ck4", "cA"], ["cA"])
        self.cp("dve", ck[:, 5, :], A[:], ["cA"], ["ck5"])
        self.tsc("dve", cq[:, 0:3, :], ck[:, 3:6, :], -1.0, ALU.mult, ["ck3", "ck4", "ck5"], ["cq0"])
        self.st("cqs", dr["caq"][:, :, :], cq[:], ["cq0", "cq1"], ["caq"])
        self.st("cks", dr["cak"][:, :, :], ck[:], ["ck1", "ck3", "ck4", "ck5"], ["cak"])

    def attn_epilogue(self, oacc, ok, sg_ap, sgk, dst, uid):
        i = self._ep % 2
        self._ep += 1
        rl, tn, ms = self.ep_rl[i], self.ep_tn[i], self.ep_ms[i]
        self.P.op("dve", lambda e: e.reciprocal(out=rl[0:64, :], in_=oacc[64:128, :]), [ok], [f"ep_rl{i}"])
        self.tt("dve", tn[0:64, :], oacc[0:64, :], rl[0:64, :], ALU.mult, [ok, f"ep_rl{i}"], [f"ep_tn{i}"])
        self.tt("pool", ms[0:64, :], tn[0:64, :], sg_ap, ALU.mult, [f"ep_tn{i}", sgk], [f"ep_ms{i}"])
        self.st(f"ep_ms{i}s", dst, ms[0:64, :], [f"ep_ms{i}"], [("mix", uid)])

    def ep_alloc(self):
        self._ep = 0
        self.ep_rl = [self.sb(f"ep_rl{i}", [64, 512], F32) for i in range(2)]
        self.ep_tn = [self.sb(f"ep_tn{i}", [64, 512], F32) for i in range(2)]
        self.ep_ms = [self.sb(f"ep_ms{i}", [64, 512], BF16) for i in range(2)]

    def phase_fox(self, L):
        P = self.P
        dr = self.dram
        ident = self.load_const("ident", [128, 128], BF16)
        nmc = self.load_const("nm_cur", [128, 128], BF16)
        V = self.sb("V", [128, 32, 1024], BF16)
        for q4 in range(4):
            self.ld(f"V{q4}", V[:, q4 * 8:(q4 + 1) * 8, :],
                    dr["vA"][q4 * 1024:(q4 + 1) * 1024, :].rearrange("(n p) c -> p n c", p=128))
        vkeys = [f"V{q4}" for q4 in range(4)]
        qa = [self.sb(f"qa{i}", [128, T], BF16) for i in range(2)]
        ka = [self.sb(f"ka{i}", [128, T], BF16) for i in range(2)]
        sg = [self.sb(f"sg{i}", [64, T], BF16) for i in range(2)]
        NPT = 4
        pT = [self.sb(f"pT{i}", [128, 512], BF16) for i in range(NPT)]
        self.ep_alloc()
        ps_s = [self.ps(f"ps_s{i}") for i in range(4)]
        ps_o = [self.ps(f"ps_o{i}") for i in range(2)]
        ns = 0
        no = 0
        for h in range(8):
            b = h % 2
            self.ld(f"qa{b}", qa[b][0:64, :], dr["qA"][h * 64:(h + 1) * 64, :], writes=[f"qa{b}", f"qa{b}x"])
            self.ld(f"qa{b}c", qa[b][64:70, :], dr["caq"][h, :, :], writes=[f"qa{b}x"], reads=["caq"])
            self.ld(f"ka{b}", ka[b][0:64, :], dr["kA"][h * 64:(h + 1) * 64, :], writes=[f"ka{b}", f"ka{b}x"])
            self.ld(f"ka{b}c", ka[b][64:70, :], dr["cak"][h, :, :], writes=[f"ka{b}x"], reads=["cak"])
            self.ld(f"sg{b}", sg[b][:], dr["sgT"][h * 64:(h + 1) * 64, :])
            qk_reads = [f"qa{b}", f"qa{b}x", f"ka{b}", f"ka{b}x"]
            for qb in range(8):
                oacc = ps_o[no % 2]
                ok = f"ps_o{no % 2}"
                no += 1
                nkt = 4 * qb + 4
                units = []
                for kt in range(nkt):
                    jd = kt - 4 * qb
                    c0 = 128 * jd if jd > 0 else 0
                    units.append((kt, jd, c0))

                def s_stage(u, ns):
                    kt, jd, c0 = u
                    sp_ = ps_s[ns % 4]
                    sk = f"ps_s{ns % 4}"
                    pt_ = pT[ns % NPT]
                    pk = f"pT{ns % NPT}"
                    self.mm(sp_[:, c0:512], ka[b][0:70, kt * 128:(kt + 1) * 128],
                            qa[b][0:70, qb * 512 + c0:(qb + 1) * 512], True, jd < 0, qk_reads, [sk])
                    if jd >= 0:
                        self.mm(sp_[:, c0:c0 + 128], ident[:], nmc[:], False, True,
                                ["k_ident", "k_nm_cur"], [sk])
                    self.act(pt_[:, c0:512], sp_[:, c0:512], AF.Exp, [sk], [pk])
                    return pt_, pk

                LA = 2
                staged = []
                for ui, u in enumerate(units):
                    staged.append(s_stage(u, ns))
                    ns += 1
                    if ui >= LA:
                        kt, jd, c0 = units[ui - LA]
                        pt_, pk = staged[ui - LA]
                        self.mm(oacc[:, c0:512], V[:, kt, h * 128:(h + 1) * 128], pt_[:, c0:512],
                                kt == 0, kt == nkt - 1, [pk, vkeys[kt // 8]], [ok])
                for ui in range(max(0, len(units) - LA), len(units)):
                    kt, jd, c0 = units[ui]
                    pt_, pk = staged[ui]
                    self.mm(oacc[:, c0:512], V[:, kt, h * 128:(h + 1) * 128], pt_[:, c0:512],
                            kt == 0, kt == nkt - 1, [pk, vkeys[kt // 8]], [ok])
                self.attn_epilogue(oacc, ok, sg[b][:, qb * 512:(qb + 1) * 512], f"sg{b}",
                                   dr["mixT"][h * 64:(h + 1) * 64, qb * 512:(qb + 1) * 512], ("fox", h, qb))

    def phase_lin(self, L, kind):
        P = self.P
        dr = self.dram
        j = L // 2
        gla = kind == "gla"
        ident = self.load_const("ident", [128, 128], BF16)
        onesd = self.load_const("ones_d128", [128, 128], BF16)
        epst = self.sb("epst", [128, 1], F32)
        P.op("dve", lambda e: e.memset(epst[:], EPS), writes=["epst"])
        if gla:
            tri4 = self.load_const("tri4", [128, 512], F32)
            resetm = self.load_const("resetm", [128, 512], F32)
            blr = self.load_const("b_lr", [128, 4], F32, src=dr["b_lr"])
            gpar = self.load_const("gla_g", [128, 8], F32, src=dr["gla_g"])
            wl32 = self.load_const("wl32", [16, 256], F32, src=dr[f"w_lr{j}"])
            wl = self.sb("wl", [16, 256], BF16)
            self.cp("dve", wl[:], wl32[:], ["k_wl32"], ["wl"])
            glr = self.load_const("glr", [16, T], BF16, src=dr["glrT"])
            one1 = self.sb("one1", [128, 1], F32)
            P.op("dve", lambda e: e.memset(one1[:], 1.0), writes=["one1"])
            negb = self.sb("negb", [128, 2], F32)
            self.tsc("dve", negb[:], blr[:, j * 2:j * 2 + 2], -1.0, ALU.mult, ["k_b_lr"], ["negb"])
            sp = self.sb("sp", [128, T], F32)
            Bc = self.sb("Bc", [128, T], F32)
            Epos = self.sb("Epos", [128, T], F32)
        else:
            dtm4 = self.load_const("ret_dt4", [128, 2048], F32)
            xi = self.load_const("ret_xi", [128, 1024], F32)
            zt4 = self.load_const("ret_zt4", [128, 1024], F32)
            gw = self.load_const("gn_w", [128, 8], F32, src=dr["gn_w"])
            gb = self.load_const("gn_b", [128, 8], F32, src=dr["gn_b"])
            gcs = self.consts["ret_gc"]
        q2 = self.sb("q2", [128, T], BF16)
        k2 = self.sb("k2", [128, T], BF16)
        qt = self.sb("qt", [128, T], BF16)
        kt2 = self.sb("kt2", [128, T], BF16) if gla else k2
        ktok = self.sb("ktok", [128, 32, 128], BF16)
        Vg = self.sb("Vg", [128, 32, 512], BF16)
        for q4 in range(4):
            self.ld(f"Vg{q4}", Vg[:, q4 * 8:(q4 + 1) * 8, :],
                    dr["vB"][q4 * 1024:(q4 + 1) * 1024, :].rearrange("(n p) c -> p n c", p=128))
        sgt = [self.sb(f"sgt{e}", [128, T], BF16) for e in range(2)]
        U = self.sb("U", [128, 128], F32)
        stb = [self.sb(f"stb{i}", [128, 128], BF16) for i in range(2)]
        As = [self.sb(f"As{i}", [128, 512], BF16) for i in range(2)]
        sq = self.sb("sq", [128, 512], BF16)
        lnv = self.sb("lnv", [128, 512], F32)
        rstd = self.sb("rstd", [128, 512], F32)
        on = self.sb("on", [128, 512], F32)
        mst = [self.sb(f"mst{i}", [128, 512], BF16) for i in range(2)]
        if not gla:
            ob = self.sb("ob", [128, 512], BF16)
            mean = self.sb("mean", [128, 512], F32)
            var = self.sb("var", [128, 512], F32)
        ps_g = self.ps("ps_g")
        ps_g2 = self.ps("ps_g2") if not gla else None
        ps_t = self.ps("ps_t", (128, 1024), BF16)
        ps_a = [self.ps(f"ps_a{e}") for e in range(2 if gla else 1)]
        ps_kv = [self.ps(f"ps_kv{e}") for e in range(2)]
        NOB = 1
        ps_o = [[self.ps(f"ps_o{e}_{i}") for i in range(NOB)] for e in range(2)]
        nms = 0
        for hp in range(2):
            rows = slice(hp * 128, (hp + 1) * 128)
            self.ld("q2", q2[:], dr["qB"][rows, :])
            self.ld("k2", k2[:], dr["kB"][rows, :])
            for e in range(2):
                h = 2 * hp + e
                mrow = (512 if gla else 0) + h * 128
                self.ld(f"sgt{e}", sgt[e][:], dr["sgT"][mrow:mrow + 128, :])
            for tb in range(NB):
                ts = slice(tb * 512, (tb + 1) * 512)
                if gla:
                    self.mm(ps_g[:], wl[0:16, rows], glr[0:16, ts], True, True, ["wl", "k_glr"], ["ps_g"])
                    self.act(sp[:, ts], ps_g[:], AF.Exp, ["ps_g", "negb"], [("sp", tb)], scale=-1.0,
                             bias=negb[:, hp:hp + 1])
                    self.act(sp[:, ts], sp[:, ts], AF.Ln, [("sp", tb), "one1"], [("sp", tb)], bias=one1[:, 0:1])
                    P.op("dve", lambda e, ts=ts: e.tensor_tensor_scan(
                        out=Bc[:, ts], data0=resetm[:], data1=sp[:, ts], initial=0.0, op0=ALU.mult, op1=ALU.add),
                        [("sp", tb), "k_resetm"], [("Bc", tb)])
                    self.act(Epos[:, ts], Bc[:, ts], AF.Exp, [("Bc", tb)], [("Epos", tb)], scale=-1.0 / 16)
                    self.act(sp[:, ts], Bc[:, ts], AF.Exp, [("Bc", tb), ("sp", tb)], [("sp", tb)], scale=1.0 / 16)
                    self.stt(qt[:, ts], q2[:, ts], 0.125, Epos[:, ts], ALU.mult, ALU.mult,
                             ["q2", ("Epos", tb)], [("qt", tb)])
                    self.tt("pool", kt2[:, ts], k2[:, ts], sp[:, ts], ALU.mult, ["k2", ("sp", tb)], [("kt2", tb)])
                    ktk = ("kt2", tb)
                else:
                    self.tt("dve", qt[:, ts], q2[:, ts], xi[:, hp * 512:(hp + 1) * 512], ALU.mult,
                            ["q2", "k_ret_xi"], [("qt", tb)])
                    ktk = "k2"
                for c in range(4):
                    n = tb * 4 + c
                    cs = slice(n * 128, (n + 1) * 128)
                    self.P.op("pe", lambda e, cs=cs, c=c: e.transpose(
                        ps_t[:, c * 128:(c + 1) * 128], kt2[:, cs], ident[:]),
                        [ktk, "k_ident"], ["ps_t"])
                kdst = ktok[:, tb * 4:(tb + 1) * 4, :].rearrange("p n d -> p (n d)")
                if gla:
                    self.cp("act", kdst, ps_t[:, 0:512], ["ps_t"], [("ktok", tb)])
                else:
                    self.tt("dve", kdst, ps_t[:, 0:512], zt4[:, hp * 512:(hp + 1) * 512], ALU.mult,
                            ["ps_t", "k_ret_zt4"], [("ktok", tb)])
            for tb in range(NB):
                ts = slice(tb * 512, (tb + 1) * 512)
                for e in range(2):
                    h = 2 * hp + e
                    r = slice(64 * e, 64 * e + 64)
                    hc = slice(h * 128, (h + 1) * 128)
                    pa, pak = (ps_a[e], f"ps_a{e}") if gla else (ps_a[0], "ps_a0")
                    pkv, pkvk = ps_kv[e], f"ps_kv{e}"
                    po, pok = ps_o[e][0], f"ps_o{e}_0"
                    qk_r = [("qt", tb), ktk, "q2"]
                    for c in range(4):
                        n = tb * 4 + c
                        cs = slice(n * 128, (n + 1) * 128)
                        col = slice(c * 128, (c + 1) * 128)
                        if gla:
                            self.mm(pa[:, col], kt2[r, cs], qt[r, cs], c == 0, c == 3, qk_r, [pak])
                        else:
                            self.mm(pa[:, col], k2[r, cs], q2[r, cs], c == 0, c == 3, qk_r, [pak])
                    if gla:
                        self.tt("dve", As[e][:], pa[:], tri4[:], ALU.mult, [pak, "k_tri4"], [f"As{e}"])
                    else:
                        self.tt("dve", As[e][:], pa[:], dtm4[:, h * 512:(h + 1) * 512], ALU.mult,
                                [pak, "k_ret_dt4"], [f"As{e}"])
                    for c in range(4):
                        n = tb * 4 + c
                        col = slice(c * 128, (c + 1) * 128)
                        self.mm(pkv[0:64, col], ktok[:, n, r], Vg[:, n, hc], c == 0, c == 3,
                                [("ktok", tb), f"Vg{n // 8}"], [pkvk])
                    for c in range(4):
                        n = tb * 4 + c
                        col = slice(c * 128, (c + 1) * 128)
                        self.mm(po[:, col], Vg[:, n, hc], As[e][:, col], c == 0, (tb == 0 and c == 3 and False),
                                [f"As{e}", f"Vg{n // 8}"], [pok])
                for c in range(4):
                    n = tb * 4 + c
                    cs = slice(n * 128, (n + 1) * 128)
                    col = slice(c * 128, (c + 1) * 128)
                    for e in range(2):
                        h = 2 * hp + e
                        r = slice(64 * e, 64 * e + 64)
                        pkv, pkvk = ps_kv[e], f"ps_kv{e}"
                        po, pok = ps_o[e][0], f"ps_o{e}_0"
                        uk = ("U", e)
                        if n == 0:
                            self.cp("dve", U[r, :], pkv[0:64, col], [pkvk], [uk])
                        elif gla:
                            dcol = 128 * (n - 1) + 127
                            self.stt(U[r, :], U[r, :], Epos[r, dcol:dcol + 1], pkv[0:64, col], ALU.mult, ALU.add,
                                     [uk, pkvk, ("Epos", (n - 1) // 4)], [uk])
                        else:
                            self.stt(U[r, :], U[r, :], gcs[h], pkv[0:64, col], ALU.mult, ALU.add, [uk, pkvk], [uk])
                        if n < 31:
                            sk = (f"stb{(n + 1) % 2}", e)
                            if gla:
                                dcol = 128 * n + 127
                                self.tsc("dve", stb[(n + 1) % 2][r, :], U[r, :], Epos[r, dcol:dcol + 1], ALU.mult,
                                         [uk, ("Epos", tb)], [sk])
                            else:
                                self.cp("act", stb[(n + 1) % 2][r, :], U[r, :], [uk], [sk])
                        if n > 0:
                            self.mm(po[:, col], stb[n % 2][r, :], qt[r, cs], False, c == 3,
                                    [(f"stb{n % 2}", e), ("qt", tb)], [pok])
                for e in range(2):
                    h = 2 * hp + e
                    po, pok = ps_o[e][0], f"ps_o{e}_0"
                    if True:
                        n = tb * 4 + 3
                    if n % 4 == 3:
                        ts = slice(tb * 512, (tb + 1) * 512)
                        ms = mst[nms % 2]
                        msk = f"mst{nms % 2}"
                        nms += 1
                        dst = dr["mixT"][(512 if gla else 0) + h * 128:(512 if gla else 0) + (h + 1) * 128, ts]
                        self.act(sq[:], po[:], AF.Square, [pok], ["sq"])
                        self.mm(ps_g[:], onesd[:], sq[:], True, True, ["sq", "k_ones_d128"], ["ps_g"])
                        if gla:
                            self.act(lnv[:], ps_g[:], AF.Ln, ["ps_g", "epst"], ["lnv"], bias=epst[:, 0:1])
                            self.act(rstd[:], lnv[:], AF.Exp, ["lnv"], ["rstd"], scale=-0.5)
                            self.tt("dve", on[:], po[:], rstd[:], ALU.mult, [pok, "rstd"], ["on"])
                            self.stt(ms[:], on[:], gpar[:, j * 4 + h:j * 4 + h + 1], sgt[e][:, ts], ALU.mult, ALU.mult,
                                     ["on", "k_gla_g", f"sgt{e}"], [msk])
                        else:
                            self.cp("act", ob[:], po[:], [pok], ["ob"])
                            self.mm(ps_g2[:], onesd[:], ob[:], True, True, ["ob", "k_ones_d128"], ["ps_g2"])
                            self.cp("act", mean[:], ps_g2[:], ["ps_g2"], ["mean"])
                            self.tt("pool", var[:], mean[:], mean[:], ALU.mult, ["mean"], ["var"])
                            self.tt("dve", var[:], ps_g[:], var[:], ALU.subtract, ["ps_g", "var"], ["var"])
                            self.act(lnv[:], var[:], AF.Ln, ["var", "epst"], ["lnv"], bias=epst[:, 0:1])
                            self.act(rstd[:], lnv[:], AF.Exp, ["lnv"], ["rstd"], scale=-0.5)
                            self.tt("dve", on[:], po[:], mean[:], ALU.subtract, [pok, "mean"], ["on"])
                            self.tt("pool", on[:], on[:], rstd[:], ALU.mult, ["on", "rstd"], ["on"])
                            self.P.op("dve", lambda e_, h=h: e_.tensor_scalar(
                                out=on[:], in0=on[:], scalar1=gw[:, j * 4 + h:j * 4 + h + 1],
                                scalar2=gb[:, j * 4 + h:j * 4 + h + 1], op0=ALU.mult, op1=ALU.add),
                                ["on", "k_gn_w", "k_gn_b"], ["on"])
                            self.tt("pool", ms[:], on[:], sgt[e][:, ts], ALU.mult, ["on", f"sgt{e}"], [msk])
                        self.st(msk + "s", dst, ms[:], [msk], [("mix", kind, h, tb)])

    def phase_dil(self, L):
        P = self.P
        dr = self.dram
        ident = self.load_const("ident", [128, 128], BF16)
        nmc = self.load_const("nm_cur4", [128, 512], BF16)
        nmp = self.load_const("nm_prev4", [128, 512], BF16)
        q2 = self.sb("q2", [128, T], BF16)
        k2 = self.sb("k2", [128, T], BF16)
        V1 = self.sb("V1", [128, 32, 256], BF16)
        V4 = self.sb("V4", [128, 32, 256], BF16)
        V16 = self.sb("V16", [128, 32, 256], BF16)
        sg = [self.sb(f"sg{i}", [64, T], BF16) for i in range(2)]
        NPT = 3
        pT = [self.sb(f"pT{i}", [128, 512], BF16) for i in range(NPT)]
        self.ep_alloc()
        ps_s = [self.ps(f"ps_s{i}") for i in range(3)]
        ps_o = [self.ps(f"ps_o{i}") for i in range(4)]
        ns = 0
        for hp in range(4):
            rows = slice(hp * 128, (hp + 1) * 128)
            vc = slice(hp * 256, (hp + 1) * 256)
            self.ld("q2", q2[:], dr["qA"][rows, :])
            self.ld("k2", k2[:], dr["kA"][rows, :])
            self.ld("V1", V1[:], dr["vA"][:, vc].rearrange("(n p) c -> p n c", p=128))
            v4 = dr["vA"][:, vc].rearrange("(n i r) c -> i r n c", i=128, r=4)
            P.dma("sp", lambda e, v4=v4: [e.dma_start(out=V4[:, r * 8:(r + 1) * 8, :], in_=v4[:, r, :, :])
                                          for r in range(4)], "V4", 4, writes=["V4"])
            v16 = dr["vA"][:, vc].rearrange("(n i r) c -> i r n c", i=128, r=16)
            P.dma("sp", lambda e, v16=v16: [e.dma_start(out=V16[:, r * 2:(r + 1) * 2, :], in_=v16[:, r, :, :])
                                            for r in range(16)], "V16", 16, writes=["V16"])
            for e in range(2):
                h = 2 * hp + e
                self.ld(f"sg{e}", sg[e][:], dr["sgT"][512 + h * 64:512 + (h + 1) * 64, :])
            for e in range(2):
                h = 2 * hp + e
                r_ = slice(64 * e, 64 * e + 64)
                vh = slice(e * 128, (e + 1) * 128)
                for hf in range(2):
                    units = []
                    for gb in range(16 * hf, 16 * hf + 16):
                        bnk = (gb - 16 * hf) // 4
                        oc = slice((gb % 4) * 128, (gb % 4 + 1) * 128)
                        qc = slice(gb * 128, (gb + 1) * 128)
                        units.append((qc, qc, "cur", V1[:, gb, vh], "V1", [(bnk, oc, slice(0, 128))]))
                        if gb > 0:
                            units.append((slice((gb - 1) * 128, gb * 128), qc, "prev", V1[:, gb - 1, vh], "V1",
                                          [(bnk, oc, slice(0, 128))]))
                    for bnk in range(4):
                        n = 4 * hf + bnk
                        for r in range(4):
                            qc = slice(512 * n + r, 512 * (n + 1), 4)
                            oc = slice(r, 512, 4)
                            units.append((qc, qc, "cur", V4[:, r * 8 + n, vh], "V4", [(bnk, oc, slice(0, 128))]))
                            if n > 0:
                                units.append((slice(512 * (n - 1) + r, 512 * n, 4), qc, "prev",
                                              V4[:, r * 8 + n - 1, vh], "V4", [(bnk, oc, slice(0, 128))]))
                    for r in range(16):
                        qc = slice(2048 * hf + r, 2048 * (hf + 1), 16)
                        outs = [(bnk, slice(r, 512, 16), slice(32 * bnk, 32 * bnk + 32)) for bnk in range(4)]
                        units.append((qc, qc, "cur", V16[:, r * 2 + hf, vh], "V16", outs))
                        if hf > 0:
                            units.append((slice(r, 2048, 16), qc, "prev", V16[:, r * 2, vh], "V16", outs))
                    units = [u for u in units if u[2] == "cur"] + [u for u in units if u[2] == "prev"]
                    groups = []
                    for mk_ in ("cur", "prev"):
                        us = [u for u in units if u[2] == mk_]
                        for g0 in range(0, len(us), 4):
                            groups.append(us[g0:g0 + 4])
                    seq = []
                    for ui, u in enumerate(units):
                        for (bnk, oc, pc) in u[5]:
                            seq.append((ui, bnk))
                    first = {}
                    lastm = {}
                    for si, (ui, bnk) in enumerate(seq):
                        first.setdefault(bnk, si)
                        lastm[bnk] = si
                    si = 0
                    for grp in groups:
                        sp_ = ps_s[ns % 3]
                        sk = f"ps_s{ns % 3}"
                        pt_ = pT[ns % NPT]
                        pk = f"pT{ns % NPT}"
                        ns += 1
                        for gi, (kc, qc, mask, vt, vkey, outs) in enumerate(grp):
                            cs = slice(gi * 128, (gi + 1) * 128)
                            self.mm(sp_[:, cs], k2[r_, kc], q2[r_, qc], gi == 0, False, ["q2", "k2"], [sk])
                        w = 128 * len(grp)
                        self.mm(sp_[:, 0:w], ident[:], (nmc if grp[0][2] == "cur" else nmp)[:, 0:w], False, True,
                                ["k_ident", "k_nm_cur4", "k_nm_prev4"], [sk])
                        self.act(pt_[:, 0:w], sp_[:, 0:w], AF.Exp, [sk], [pk])
                        for gi, (kc, qc, mask, vt, vkey, outs) in enumerate(grp):
                            for (bnk, oc, pc) in outs:
                                pcs = slice(gi * 128 + pc.start, gi * 128 + pc.stop)
                                self.mm(ps_o[bnk][:, oc], vt, pt_[:, pcs], si == first[bnk], si == lastm[bnk],
                                        [pk, vkey], [f"ps_o{bnk}"])
                                si += 1
                    for bnk in range(4):
                        ts = slice(2048 * hf + 512 * bnk, 2048 * hf + 512 * (bnk + 1))
                        self.attn_epilogue(ps_o[bnk], f"ps_o{bnk}", sg[e][:, ts], f"sg{e}",
                                           dr["mixT"][512 + h * 64:512 + (h + 1) * 64, ts], ("dil", h, hf, bnk))


def make_in_maps(inputs, consts):
    x = np.asarray(inputs["x"], np.float32)
    common = {}
    for j in range(2):
        common[f"w_in_e{j}"] = np.ascontiguousarray(inputs["w_in_even"][j], np.float32)
        common[f"w_in_o{j}"] = np.ascontiguousarray(inputs["w_in_odd"][j], np.float32)
        common[f"w_out_e{j}"] = np.ascontiguousarray(inputs["w_out_even"][j], np.float32)
        common[f"w_out_o{j}"] = np.ascontiguousarray(inputs["w_out_odd"][j], np.float32)
        common[f"w_lr{j}"] = np.ascontiguousarray(inputs["w_lr_even"][j], np.float32)
    gains = [inputs["norm_even"][0], inputs["norm_odd"][0], inputs["norm_even"][1], inputs["norm_odd"][1],
             inputs["final_norm"]]
    ng = np.stack([np.asarray(g, np.float32).reshape(8, 128).T for g in gains], axis=1)
    common["normg"] = np.ascontiguousarray(ng.reshape(128, 40))
    common["b_f"] = np.ascontiguousarray(np.asarray(inputs["b_f_even"], np.float32).T)
    blr = np.asarray(inputs["b_lr_even"], np.float32).reshape(2, 2, 128)
    common["b_lr"] = np.ascontiguousarray(blr.transpose(2, 0, 1).reshape(128, 4))
    for nm, key in (("gla_g", "gla_norm_even"), ("gn_w", "ret_gn_w_odd"), ("gn_b", "ret_gn_b_odd")):
        a = np.asarray(inputs[key], np.float32).reshape(2, 4, 128)
        common[nm] = np.ascontiguousarray(a.transpose(2, 0, 1).reshape(128, 8))
    for n in CONST_NAMES:
        common["c_" + n] = consts[n]
    maps = []
    for b in range(8):
        m = dict(common)
        m["xT"] = np.ascontiguousarray(x[b].T)
        maps.append(m)
    return maps


_CACHE = {}


def kernel(**inputs):
    if "b" not in _CACHE:
        b = Builder()
        b.build()
        _CACHE["b"] = b
    b = _CACHE["b"]
    maps = make_in_maps(inputs, b.consts)
    res = run_bass_kernel_spmd(b.nc, maps, core_ids=list(range(8)))
    out = np.stack([np.ascontiguousarray(r["yT"].T) for r in res.results], axis=0)
    return out.astype(np.float32)
```

```python
import numpy as np
import ml_dtypes
from contextlib import ExitStack
import concourse.bass as bass
import concourse.mybir as mybir
from concourse.bass_utils import run_bass_kernel_spmd

F32 = mybir.dt.float32
BF16 = mybir.dt.bfloat16
AF = mybir.ActivationFunctionType
ALU = mybir.AluOpType
NPBF = ml_dtypes.bfloat16

T = 4096
D = 1024
NB = 8
EVEN_IN = 3608
ODD_IN = 3584
EPS = 1e-6
NEG = -30000.0


class Prog:
    STREAMS = ("pe", "act", "dve", "pool", "sp")
    SECT = {"pe": "tensor", "act": "scalar", "dve": "vector", "pool": "gpsimd", "sp": "sync"}
    CAP = 20000

    def __init__(self, nc, es):
        self.nc = nc
        self.es = es
        self.sems = {s: [] for s in self.STREAMS}
        self.cnt = {s: 0 for s in self.STREAMS}
        self.dsem = {}
        self.waited = {s: {} for s in self.STREAMS}
        self.reset()

    def reset(self):
        self.ops = []
        self.lw = {}
        self.rd = {}

    def _add(self, stream, fn, reads, writes, dma=None, ndma=0):
        idx = len(self.ops)
        deps = {}
        for k in reads:
            w = self.lw.get(k)
            if w is not None:
                deps[w] = True
        for k in writes:
            w = self.lw.get(k)
            if w is not None:
                deps.setdefault(w, False)
            for r in self.rd.get(k, ()):
                deps.setdefault(r, False)
        for k in reads:
            self.rd.setdefault(k, []).append(idx)
        for k in writes:
            self.lw[k] = idx
            self.rd[k] = []
        self.ops.append(dict(s=stream, fn=fn, deps=deps, dma=dma, ndma=ndma, need=False, c=0))
        return idx

    def op(self, stream, fn, reads=(), writes=()):
        return self._add(stream, fn, tuple(reads), tuple(writes))

    def dma(self, stream, fn, key, n, reads=(), writes=()):
        return self._add(stream, fn, tuple(reads), tuple(writes), dma=key, ndma=n)

    def _sem(self, stream, i):
        lst = self.sems[stream]
        while len(lst) <= i:
            lst.append(self.es.enter_context(self.nc.semaphore(f"s_{stream}_{len(lst)}")))
        return lst[i]

    def finalize_and_emit(self, block):
        ops = self.ops
        for o in ops:
            w = []
            for d, raw in o["deps"].items():
                p = ops[d]
                if p["dma"] is not None:
                    w.append(d)
                elif o["dma"] is not None:
                    w.append(d)
                elif p["s"] == o["s"]:
                    if o["s"] != "pe":
                        w.append(d)
                else:
                    w.append(d)
            o["w"] = w
            for d in w:
                ops[d]["need"] = True
        for o in ops:
            if o["dma"] is not None:
                if o["dma"] not in self.dsem:
                    self.dsem[o["dma"]] = [self.es.enter_context(self.nc.semaphore("d_" + o["dma"])), 0]
                self.dsem[o["dma"]][1] += 16 * o["ndma"]
                o["c"] = self.dsem[o["dma"]][1]
            elif o["need"]:
                self.cnt[o["s"]] += 1
                o["c"] = self.cnt[o["s"]]
        final_d = {k: v[1] for k, v in self.dsem.items()}

        def target(p):
            if p["dma"] is not None:
                return ("D" + p["dma"], self.dsem[p["dma"]][0], p["c"])
            c = p["c"]
            i = (c - 1) // self.CAP
            return (p["s"] + str(i), self._sem(p["s"], i), (c - 1) % self.CAP + 1)

        for s in self.STREAMS:
            ops_s = [o for o in ops if o["s"] == s]
            if not ops_s and s != "sp":
                continue

            def body(eng, ops_s=ops_s, s=s):
                wt = self.waited[s]
                for o in ops_s:
                    tg = {}
                    for d in o["w"]:
                        name, sem, val = target(ops[d])
                        if wt.get(name, 0) >= val:
                            continue
                        if name not in tg or tg[name][1] < val:
                            tg[name] = (sem, val)
                    for name, (sem, val) in tg.items():
                        eng.wait_ge(sem, val)
                        wt[name] = val
                    r = o["fn"](eng)
                    if o["dma"] is not None:
                        sem = self.dsem[o["dma"]][0]
                        assert len(r) == o["ndma"], (len(r), o["ndma"])
                        for ins in r:
                            ins.then_inc(sem, 16)
                    elif o["need"]:
                        c = o["c"]
                        r.then_inc(self._sem(s, (c - 1) // self.CAP), 1)
                if s == "sp":
                    for k, v in final_d.items():
                        if wt.get("D" + k, 0) < v:
                            eng.wait_ge(self.dsem[k][0], v)
                            wt["D" + k] = v

            getattr(block, self.SECT[s])(body)
        self.reset()


def _consts():
    c = {}
    p = np.arange(128)
    c["ident"] = np.eye(128, dtype=np.float32).astype(NPBF)
    c["ones"] = np.ones((128, 128), np.float32).astype(NPBF)
    c["ones_d128"] = np.full((128, 128), 1.0 / 128, np.float32).astype(NPBF)
    c["nm_cur"] = np.where(p[:, None] > p[None, :], NEG, 0.0).astype(np.float32).astype(NPBF)
    c["nm_prev"] = np.where(p[:, None] < p[None, :], NEG, 0.0).astype(np.float32).astype(NPBF)
    c["nm_cur4"] = np.tile(c["nm_cur"], (1, 4))
    c["nm_prev4"] = np.tile(c["nm_prev"], (1, 4))
    c["tri"] = np.where(p[:, None] <= p[None, :], 1.0, 0.0).astype(np.float32)
    inv = np.power(np.float32(10000.0), -np.arange(0, 64, 2, dtype=np.float32) / np.float32(64)).astype(np.float32)
    ang = (np.arange(T, dtype=np.float32)[None, :] * inv[p % 32][:, None]).astype(np.float32)
    c["cos"] = np.cos(ang.astype(np.float64)).astype(np.float32)
    c["sin"] = np.sin(ang.astype(np.float64)).astype(np.float32)
    perm = np.zeros((128, 128), np.float32)
    for m in range(128):
        if m % 64 < 32:
            perm[m + 32, m] = -1.0
        else:
            perm[m - 32, m] = 1.0
    c["perm"] = perm.astype(NPBF)
    lg = np.log(1.0 - np.power(2.0, -5.0 - np.arange(4, dtype=np.float64)))
    idx = np.arange(128, dtype=np.float64)
    dt = np.zeros((128, 4, 128), np.float32)
    for h in range(4):
        diff = idx[None, :] - idx[:, None]
        dt[:, h, :] = np.where(diff >= 0, np.exp(np.maximum(diff, 0) * lg[h]), 0.0)
    c["ret_dt"] = dt.reshape(128, 512)
    c["ret_dt4"] = np.ascontiguousarray(np.tile(dt[:, :, None, :], (1, 1, 4, 1)).reshape(128, 2048))
    c["tri4"] = np.tile(c["tri"], (1, 4))
    xi = np.zeros((128, 2, 512), np.float32)
    zt = np.zeros((128, 2, 128), np.float32)
    for hp in range(2):
        for e in range(2):
            h = 2 * hp + e
            xi[64 * e:64 * e + 64, hp, :] = np.tile(np.exp((idx + 1.0) * lg[h]), 4)[None, :]
            zt[:, hp, 64 * e:64 * e + 64] = np.exp((127.0 - idx) * lg[h])[:, None]
    c["ret_xi"] = xi.reshape(128, 1024)
    c["ret_zt"] = zt.reshape(128, 256)
    c["ret_zt4"] = np.ascontiguousarray(np.tile(zt[:, :, None, :], (1, 1, 4, 1)).reshape(128, 1024))
    c["ret_gc"] = [float(np.exp(128.0 * lg[h])) for h in range(4)]
    rm = np.ones((128, 512), np.float32)
    rm[:, ::128] = 0.0
    c["resetm"] = rm
    return c


CONST_NAMES = ["ident", "ones", "ones_d128", "nm_cur", "nm_prev", "nm_cur4", "nm_prev4", "tri", "cos", "sin", "perm",
               "ret_dt", "ret_xi", "ret_zt", "resetm", "ret_dt4", "ret_zt4", "tri4"]


class Builder:
    def __init__(self, n_layers=4, debug_out=()):
        self.n_layers = n_layers
        self.debug_out = tuple(debug_out)
        self.consts = _consts()
        self.nc = bass.Bass("TRN2", target_bir_lowering=False)
        self.es = ExitStack()
        self.P = Prog(self.nc, self.es)
        self.dram = {}

    def din(self, name, shape, dt):
        self.dram[name] = self.nc.dram_tensor(name, list(shape), dt, kind="ExternalInput").ap()
        return self.dram[name]

    def dscr(self, name, shape, dt):
        kind = "ExternalOutput" if name in self.debug_out else "Internal"
        self.dram[name] = self.nc.dram_tensor(name, list(shape), dt, kind=kind).ap()
        return self.dram[name]

    def declare(self):
        nc = self.nc
        self.din("xT", [D, T], F32)
        for j in range(2):
            self.din(f"w_in_e{j}", [D, EVEN_IN], F32)
            self.din(f"w_in_o{j}", [D, ODD_IN], F32)
            self.din(f"w_out_e{j}", [D, D], F32)
            self.din(f"w_out_o{j}", [D, D], F32)
            self.din(f"w_lr{j}", [16, 256], F32)
        self.din("normg", [128, 5 * 8], F32)
        self.din("b_f", [8, 2], F32)
        self.din("b_lr", [128, 4], F32)
        self.din("gla_g", [128, 8], F32)
        self.din("gn_w", [128, 8], F32)
        self.din("gn_b", [128, 8], F32)
        for n in CONST_NAMES:
            a = self.consts[n]
            self.din("c_" + n, a.shape, BF16 if a.dtype == NPBF else F32)
        self.dram["yT"] = nc.dram_tensor("yT", [D, T], F32, kind="ExternalOutput").ap()
        self.dscr("h0", [D, T], F32)
        self.dscr("h1", [D, T], F32)
        self.dscr("qA", [512, T], BF16)
        self.dscr("kA", [512, T], BF16)
        self.dscr("qB", [256, T], BF16)
        self.dscr("kB", [256, T], BF16)
        self.dscr("ffT", [8, T], F32)
        self.dscr("glrT", [16, T], BF16)
        self.dscr("sgT", [D, T], BF16)
        self.dscr("vA", [T, 1024], BF16)
        self.dscr("vB", [T, 512], BF16)
        self.dscr("mixT", [D, T], BF16)
        self.dscr("caq", [8, 6, T], BF16)
        self.dscr("cak", [8, 6, T], BF16)

    def phase(self, fn):
        nc = self.nc
        self.phase_no = getattr(self, "phase_no", -1) + 1
        with ExitStack() as pes:
            self.pes = pes
            self.tiles = {}
            fn()
            with nc.Block() as block:
                self.P.finalize_and_emit(block)

    def sb(self, name, shape, dt):
        t = self.pes.enter_context(self.nc.sbuf_tensor(f"p{self.phase_no}_{name}", list(shape), dt))
        return t

    def ps(self, name, shape=(128, 512), dt=F32):
        return self.pes.enter_context(self.nc.psum_tensor(f"p{self.phase_no}_{name}", list(shape), dt))

    def load_const(self, name, shape, dt, src=None):
        t = self.sb("k_" + name, shape, dt)
        src = self.dram["c_" + name] if src is None else src
        self.P.dma("sp", lambda e, t=t, src=src: [e.dma_start(out=t[:], in_=src)], "k_" + name, 1,
                   writes=["k_" + name])
        return t

    def phase_ca(self, L):
        P = self.P
        dr = self.dram
        last = (L == self.n_layers)
        odd = (L % 2 == 1)
        j = L // 2
        hsrc = dr["xT"] if L <= 1 else dr[f"h{(L - 1) % 2}"]
        hdst = dr[f"h{L % 2}"]
        NIN = ODD_IN if odd else EVEN_IN

        ones = self.load_const("ones", [128, 128], BF16)
        normg = self.load_const("normg", [128, 40], F32, src=dr["normg"])
        epst = self.sb("epst", [128, 1], F32)
        P.op("dve", lambda e: e.memset(epst[:], EPS), writes=["epst"])
        if odd and not last:
            cosb = [self.sb(f"cosb{i}", [128, 512], F32) for i in range(2)]
            sinb = [self.sb(f"sinb{i}", [128, 512], F32) for i in range(2)]
            perm = self.load_const("perm", [128, 128], BF16)

        wst = [self.sb(f"wst{i}", [128, 1024], F32) for i in range(2)]
        nst = [0]

        def load_w(dst, src, ncols, nm):
            for c0 in range(0, ncols, 1024):
                for ic in range(8):
                    cw = min(1024, ncols - c0)
                    i = nst[0] % 2
                    nst[0] += 1
                    st = wst[i]
                    P.dma("sp", lambda e, st=st, ic=ic, c0=c0, cw=cw: [e.dma_start(
                        out=st[:, 0:cw], in_=src[ic * 128:(ic + 1) * 128, c0:c0 + cw])],
                        f"wst{i}", 1, writes=[f"wst{i}"])
                    eng = "pool" if (nst[0] % 2) else "dve"
                    P.op(eng, lambda e, st=st, ic=ic, c0=c0, cw=cw: e.tensor_copy(
                        out=dst[:, ic, c0:c0 + cw], in_=st[:, 0:cw]),
                        reads=[f"wst{i}"], writes=[(nm, ic, c0 // 1024)])

        if L > 0:
            wout = self.sb("wout", [128, 8, D], BF16)
            load_w(wout, dr[f"w_out_{'e' if (L - 1) % 2 == 0 else 'o'}{(L - 1) // 2}"], D, "wout")
        if not last:
            win = self.sb("win", [128, 8, NIN], BF16)
            load_w(win, dr[f"w_in_{'o' if odd else 'e'}{j}"], NIN, "win")

        hT = [self.sb(f"hT{i}", [128, 8, 512], F32) for i in range(2)]
        mg = [self.sb(f"mg{i}", [128, 8, 512], BF16) for i in range(1)] * 2 if L > 0 else None
        sq = self.sb("sq", [128, 8, 512], BF16)
        lnt = self.sb("lnt", [128, 512], F32)
        rstd = self.sb("rstd", [128, 512], F32)
        uT = [self.sb(f"uT{i}", [128, 8, 512], BF16) for i in range(2)] if not last else None
        NST = 4
        stg = [self.sb(f"stg{i}", [128, 512], BF16) for i in range(NST)]
        stf = self.sb("stf", [128, 512], F32)
        if odd and not last:
            zb = [self.sb(f"zb{i}", [128, 512], BF16) for i in range(2)]
            t1 = [self.sb(f"t1_{i}", [128, 512], F32) for i in range(2)]
            t2 = [self.sb(f"t2_{i}", [128, 512], F32) for i in range(2)]
        if last:
            yst = [self.sb(f"yst{i}", [128, 512], F32) for i in range(2)]
        else:
            stv = [self.sb(f"stv{i}", [128, 8, 128], BF16) for i in range(2)]
            for i in range(2):
                P.op("pool", lambda e, i=i: e.memset(stv[i][:], 1.0), writes=[f"stv{i}"])
        ps_ss = self.ps("ps_ss")
        ps_o = [self.ps(f"ps_o{i}") for i in range(2)]
        ps_z = [self.ps(f"ps_z{i}") for i in range(3)]
        ps_sw = [self.ps(f"ps_sw{i}") for i in range(2)] if (odd and not last) else None

        hv = hsrc.rearrange("(c p) t -> p c t", p=128)
        hdv = hdst.rearrange("(c p) t -> p c t", p=128)
        mv = dr["mixT"].rearrange("(c p) t -> p c t", p=128)
        yv = dr["yT"].rearrange("(c p) t -> p c t", p=128)

        if not last:
            if not odd:
                fm = []
                for c in range(4):
                    fm.append((c * 128, 128, "plain", 0.125, dr["qA"][c * 128:(c + 1) * 128, :]))
                for c in range(4):
                    fm.append((512 + c * 128, 128, "plain", 1.0, dr["kA"][c * 128:(c + 1) * 128, :]))
                fm.append((1536, 8, "f32", 1.0, dr["ffT"][0:8, :]))
                for c in range(2):
                    fm.append((1544 + c * 128, 128, "plain", 1.0, dr["qB"][c * 128:(c + 1) * 128, :]))
                for c in range(2):
                    fm.append((1800 + c * 128, 128, "plain", 1.0, dr["kB"][c * 128:(c + 1) * 128, :]))
                fm.append((2568, 16, "plain", 1.0, dr["glrT"][0:16, :]))
                for c in range(8):
                    fm.append((2584 + c * 128, 128, "silu", 1.0, dr["sgT"][c * 128:(c + 1) * 128, :]))
                tm = [(1024, dr["vA"]), (2056, dr["vB"])]
            else:
                fm = []
                for c in range(2):
                    fm.append((c * 128, 128, "rope", 0.125, dr["qB"][c * 128:(c + 1) * 128, :]))
                for c in range(2):
                    fm.append((256 + c * 128, 128, "rope", 1.0, dr["kB"][c * 128:(c + 1) * 128, :]))
                for c in range(4):
                    fm.append((1024 + c * 128, 128, "rope", 0.125, dr["qA"][c * 128:(c + 1) * 128, :]))
                for c in range(4):
                    fm.append((1536 + c * 128, 128, "rope", 1.0, dr["kA"][c * 128:(c + 1) * 128, :]))
                for c in range(8):
                    fm.append((2560 + c * 128, 128, "silu", 1.0, dr["sgT"][c * 128:(c + 1) * 128, :]))
                tm = [(512, dr["vB"]), (2048, dr["vA"])]

        cnt = dict(z=0, st=0, o=0, rp=0, sv=0)
        for tb in range(NB):
            ts = slice(tb * 512, (tb + 1) * 512)
            h = hT[tb % 2]
            hk = f"hT{tb % 2}"
            P.dma("sp", lambda e, h=h, ts=ts: [e.dma_start(out=h[:, 0:4, :], in_=hv[:, 0:4, ts]),
                                               e.dma_start(out=h[:, 4:8, :], in_=hv[:, 4:8, ts])],
                  hk, 2, writes=[hk])
            if L > 0:
                m = mg[tb % 2]
                mk = "mg0"
                P.dma("sp", lambda e, m=m, ts=ts: [e.dma_start(out=m[:], in_=mv[:, :, ts])], mk, 1,
                      writes=[mk])
                for oc in range(8):
                    po = ps_o[cnt["o"] % 2]
                    pk = f"ps_o{cnt['o'] % 2}"
                    cnt["o"] += 1
                    for mc in range(8):
                        P.op("pe", lambda e, po=po, mc=mc, oc=oc, m=m: e.matmul(
                            po[:], lhsT=wout[:, mc, oc * 128:(oc + 1) * 128], rhs=m[:, mc, :],
                            start=(mc == 0), stop=(mc == 7)),
                            reads=[mk, ("wout", mc, 0)], writes=[pk])
                    P.op("dve", lambda e, po=po, oc=oc, h=h: e.tensor_tensor(
                        out=h[:, oc, :], in0=h[:, oc, :], in1=po[:], op=ALU.add),
                        reads=[pk, hk], writes=[hk])
                if not last:
                    P.dma("pool", lambda e, h=h, ts=ts: [e.dma_start(out=hdv[:, :, ts], in_=h[:])],
                          hk + "s", 1, reads=[hk], writes=[("hdst", tb)])
            P.op("act", lambda e, h=h: e.activation(out=sq[:], in_=h[:], func=AF.Square),
                 reads=[hk], writes=["sq"])
            for c in range(8):
                P.op("pe", lambda e, c=c: e.matmul(ps_ss[:], lhsT=ones[:], rhs=sq[:, c, :],
                                                   start=(c == 0), stop=(c == 7)),
                     reads=["sq", "k_ones"], writes=["ps_ss"])
            P.op("act", lambda e: e.activation(out=lnt[:], in_=ps_ss[:], func=AF.Ln,
                                               scale=1.0 / D, bias=epst[:, 0:1]),
                 reads=["ps_ss", "epst"], writes=["lnt"])
            P.op("act", lambda e: e.activation(out=rstd[:], in_=lnt[:], func=AF.Exp, scale=-0.5),
                 reads=["lnt"], writes=["rstd"])
            if last:
                for c in range(8):
                    y = yst[c % 2]
                    yk = f"yst{c % 2}"
                    P.op("dve", lambda e, y=y, c=c, h=h: e.scalar_tensor_tensor(
                        out=y[:], in0=h[:, c, :], scalar=normg[:, L * 8 + c:L * 8 + c + 1], in1=rstd[:],
                        op0=ALU.mult, op1=ALU.mult),
                        reads=[hk, "rstd", "k_normg"], writes=[yk])
                    P.dma("pool", lambda e, y=y, c=c, ts=ts: [e.dma_start(out=yv[:, c, ts], in_=y[:])],
                          yk + "s", 1, reads=[yk], writes=[("y", tb, c)])
                continue
            u = uT[tb % 2]
            uk = f"uT{tb % 2}"
            for c in range(8):
                P.op("dve", lambda e, u=u, c=c, h=h: e.scalar_tensor_tensor(
                    out=u[:, c, :], in0=h[:, c, :], scalar=normg[:, L * 8 + c:L * 8 + c + 1], in1=rstd[:],
                    op0=ALU.mult, op1=ALU.mult),
                    reads=[hk, "rstd", "k_normg"], writes=[(uk, c)])
            ukeys = [(uk, c) for c in range(8)]
            if odd:
                cb = tb % 2
                self.ld(f"cosb{cb}", cosb[cb][:], dr["c_cos"][:, ts])
                self.ld(f"sinb{cb}", sinb[cb][:], dr["c_sin"][:, ts])
            pending = []
            for (c0, M, kind, scale, dst) in fm:
                pz = ps_z[cnt["z"] % 3]
                zk = f"ps_z{cnt['z'] % 3}"
                cnt["z"] += 1
                for ic in range(8):
                    P.op("pe", lambda e, pz=pz, ic=ic, c0=c0, M=M, u=u: e.matmul(
                        pz[0:M, :], lhsT=win[:, ic, c0:c0 + M], rhs=u[:, ic, :],
                        start=(ic == 0), stop=(ic == 7)),
                        reads=[(uk, ic)] + [("win", ic, g) for g in range(c0 // 1024, (c0 + M - 1) // 1024 + 1)],
                        writes=[zk])
                while pending:
                    pending.pop(0)()
                if kind == "f32":
                    P.op("act", lambda e, pz=pz, M=M: e.activation(out=stf[0:M, :], in_=pz[0:M, :], func=AF.Copy),
                         reads=[zk], writes=["stf"])
                    P.dma("pool", lambda e, M=M, dst=dst, ts=ts: [e.dma_start(out=dst[:, ts], in_=stf[0:M, :])],
                          "stfs", 1, reads=["stf"], writes=[("fm", c0, tb)])
                    continue
                if kind in ("plain", "silu"):
                    s = stg[cnt["st"] % NST]
                    sk = f"stg{cnt['st'] % NST}"
                    cnt["st"] += 1
                    if kind == "plain":
                        P.op("act", lambda e, pz=pz, M=M, s=s, scale=scale: e.activation(
                            out=s[0:M, :], in_=pz[0:M, :], func=AF.Copy, scale=scale),
                            reads=[zk], writes=[sk])
                    else:
                        P.op("act", lambda e, pz=pz, M=M, s=s: e.activation(
                            out=s[0:M, :], in_=pz[0:M, :], func=AF.Silu),
                            reads=[zk], writes=[sk])
                    P.dma("pool", lambda e, M=M, dst=dst, ts=ts, s=s: [e.dma_start(out=dst[:, ts], in_=s[0:M, :])],
                          sk + "s", 1, reads=[sk], writes=[("fm", c0, tb)])
                    continue
                r = cnt["rp"] % 2
                cnt["rp"] += 1
                P.op("act", lambda e, pz=pz, r=r, scale=scale: e.activation(
                    out=zb[r][:], in_=pz[:], func=AF.Copy, scale=scale),
                    reads=[zk], writes=[f"zb{r}"])

                def tail(r=r, dst=dst, c0=c0, cb=cb, ts=ts, tb=tb):
                    P.op("pe", lambda e: e.matmul(ps_sw[r][:], lhsT=perm[:], rhs=zb[r][:], start=True, stop=True),
                         reads=[f"zb{r}", "k_perm"], writes=[f"ps_sw{r}"])
                    P.op("dve", lambda e: e.tensor_tensor(out=t1[r][:], in0=zb[r][:], in1=cosb[cb][:], op=ALU.mult),
                         reads=[f"zb{r}", f"cosb{cb}"], writes=[f"t1_{r}"])
                    P.op("dve", lambda e: e.tensor_tensor(out=t2[r][:], in0=ps_sw[r][:], in1=sinb[cb][:], op=ALU.mult),
                         reads=[f"ps_sw{r}", f"sinb{cb}"], writes=[f"t2_{r}"])
                    s_ = stg[cnt["st"] % NST]
                    sk_ = f"stg{cnt['st'] % NST}"
                    cnt["st"] += 1
                    P.op("dve", lambda e: e.tensor_tensor(out=s_[:], in0=t1[r][:], in1=t2[r][:], op=ALU.add),
                         reads=[f"t1_{r}", f"t2_{r}"], writes=[sk_])
                    P.dma("pool", lambda e: [e.dma_start(out=dst[:, ts], in_=s_[:])],
                          sk_ + "s", 1, reads=[sk_], writes=[("fm", c0, tb)])
                pending.append(tail)
            while pending:
                pending.pop(0)()
            for jt in range(4):
                tile = tb * 4 + jt
                for (c0, dst) in tm:
                    pz = ps_z[cnt["z"] % 3]
                    zk = f"ps_z{cnt['z'] % 3}"
                    cnt["z"] += 1
                    for ic in range(8):
                        P.op("pe", lambda e, pz=pz, ic=ic, c0=c0, u=u, jt=jt: e.matmul(
                            pz[:], lhsT=u[:, ic, jt * 128:(jt + 1) * 128], rhs=win[:, ic, c0:c0 + 512],
                            start=(ic == 0), stop=(ic == 7)),
                            reads=[(uk, ic)] + [("win", ic, g) for g in range(c0 // 1024, (c0 + 511) // 1024 + 1)],
                            writes=[zk])
                    if dst is dr["vA"]:
                        vi = cnt["sv"] % 2
                        cnt["sv"] += 1
                        P.op("dve", lambda e, pz=pz, vi=vi: e.tensor_copy(
                            out=stv[vi][:, :, 0:64], in_=pz[:].rearrange("p (h d) -> p h d", d=64)),
                            reads=[zk], writes=[f"stv{vi}"])
                        P.dma("pool", lambda e, dst=dst, tile=tile, vi=vi: [e.dma_start(
                            out=dst[tile * 128:(tile + 1) * 128, :],
                            in_=stv[vi][:].rearrange("p h d -> p (h d)"))],
                            f"stv{vi}s", 1, reads=[f"stv{vi}"], writes=[("tm", c0, tile)])
                        continue
                    s = stg[cnt["st"] % NST]
                    sk = f"stg{cnt['st'] % NST}"
                    cnt["st"] += 1
                    P.op("dve", lambda e, pz=pz, s=s: e.tensor_copy(out=s[:], in_=pz[:]),
                         reads=[zk], writes=[sk])
                    P.dma("pool", lambda e, dst=dst, tile=tile, s=s: [e.dma_start(
                        out=dst[tile * 128:(tile + 1) * 128, :], in_=s[:])],
                        sk + "s", 1, reads=[sk], writes=[("tm", c0, tile)])

    def build(self, nph=None):
        self.declare()
        phases = []
        for L in range(self.n_layers + 1):
            phases.append(lambda L=L: self.phase_ca(L))
            if L == self.n_layers:
                break
            if L % 2 == 0:
                phases.append(lambda L=L: self.phase_even_c(L))
                phases.append(lambda L=L: self.phase_fox(L))
                phases.append(lambda L=L: self.phase_lin(L, "gla"))
            else:
                phases.append(lambda L=L: self.phase_lin(L, "ret"))
                phases.append(lambda L=L: self.phase_dil(L))
        sel = phases[:nph] if not isinstance(nph, (list, tuple)) else [phases[i] for i in nph]
        for ph in sel:
            self.phase(ph)
        return self.nc


    def mm(self, out, lhsT, rhs, start, stop, reads, writes):
        self.P.op("pe", lambda e: e.matmul(out, lhsT=lhsT, rhs=rhs, start=start, stop=stop), reads, writes)

    def act(self, out, in_, func, reads, writes, scale=None, bias=None):
        kw = {}
        if scale is not None:
            kw["scale"] = scale
        if bias is not None:
            kw["bias"] = bias
        self.P.op("act", lambda e: e.activation(out=out, in_=in_, func=func, **kw), reads, writes)

    def tt(self, eng, out, in0, in1, op, reads, writes):
        self.P.op(eng, lambda e: e.tensor_tensor(out=out, in0=in0, in1=in1, op=op), reads, writes)

    def tsc(self, eng, out, in0, scalar, op, reads, writes):
        self.P.op(eng, lambda e: e.tensor_scalar(out=out, in0=in0, scalar1=scalar, scalar2=None, op0=op), reads, writes)

    def stt(self, out, in0, scalar, in1, op0, op1, reads, writes):
        self.P.op("dve", lambda e: e.scalar_tensor_tensor(out=out, in0=in0, scalar=scalar, in1=in1, op0=op0, op1=op1),
                  reads, writes)

    def cp(self, eng, out, in_, reads, writes):
        if eng == "act":
            self.P.op(eng, lambda e: e.copy(out=out, in_=in_), reads, writes)
        else:
            self.P.op(eng, lambda e: e.tensor_copy(out=out, in_=in_), reads, writes)

    def ld(self, key, out, in_, writes=None, reads=(), eng="sp"):
        self.P.dma(eng, lambda e: [e.dma_start(out=out, in_=in_)], key, 1, reads=reads,
                   writes=[key] if writes is None else writes)

    def st(self, key, out, in_, reads, writes, eng="pool"):
        self.P.dma(eng, lambda e: [e.dma_start(out=out, in_=in_)], key, 1, reads=reads, writes=writes)

    def phase_even_c(self, L):
        P = self.P
        dr = self.dram
        j = L // 2
        A = self.sb("cA", [8, T], F32)
        o1 = self.sb("c1", [8, T], F32)
        Pc = self.sb("cP", [8, T], F32)
        r1 = self.sb("cr", [8, T], F32)
        cq = self.sb("cq", [8, 6, T], BF16)
        ck = self.sb("ck", [8, 6, T], BF16)
        bf = self.sb("bf", [8, 2], F32)
        negb = self.sb("negb", [8, 1], F32)
        one1 = self.sb("one1", [8, 1], F32)
        self.ld("cA", A[:], dr["ffT"][:, :])
        self.ld("bf", bf[:], dr["b_f"][:, :])
        self.tsc("dve", negb[:], bf[:, j:j + 1], -1.0, ALU.mult, ["bf"], ["negb"])
        P.op("dve", lambda e: e.memset(one1[:], 1.0), writes=["one1"])
        P.op("pool", lambda e: e.memset(o1[:], 1.0), writes=["c1"])
        P.op("pool", lambda e: e.memset(cq[:, 3:6, :], 1.0), writes=["cq1"])
        P.op("pool", lambda e: e.memset(ck[:, 0:3, :], 1.0), writes=["ck1"])
        self.act(A[:], A[:], AF.Exp, ["cA", "negb"], ["cA"], scale=-1.0, bias=negb[:, 0:1])
        self.act(A[:], A[:], AF.Ln, ["cA", "one1"], ["cA"], bias=one1[:, 0:1])
        P.op("dve", lambda e: e.tensor_tensor_scan(out=Pc[:], data0=o1[:], data1=A[:], initial=0.0,
                                                   op0=ALU.mult, op1=ALU.add), ["c1", "cA"], ["cP"])
        self.cp("dve", ck[:, 3, :], Pc[:], ["cP"], ["ck3"])
        self.tt("dve", r1[:], Pc[:], ck[:, 3, :], ALU.subtract, ["cP", "ck3"], ["cr"])
        self.cp("dve", ck[:, 4, :], r1[:], ["cr"], ["ck4"])
        self.tt("dve", A[:], r1[:], ck[:, 4, :], ALU.subtract, ["cr", "ck4", "cA"], ["cA"])
        self.cp("dve", ck[:, 5, :], A[:], ["cA"], ["ck5"])
        self.tsc("dve", cq[:, 0:3, :], ck[:, 3:6, :], -1.0, ALU.mult, ["ck3", "ck4", "ck5"], ["cq0"])
        self.st("cqs", dr["caq"][:, :, :], cq[:], ["cq0", "cq1"], ["caq"])
        self.st("cks", dr["cak"][:, :, :], ck[:], ["ck1", "ck3", "ck4", "ck5"], ["cak"])

    def attn_epilogue(self, oacc, ok, sg_ap, sgk, dst, uid):
        i = self._ep % 2
        self._ep += 1
        rl, tn, ms = self.ep_rl[i], self.ep_tn[i], self.ep_ms[i]
        self.P.op("dve", lambda e: e.reciprocal(out=rl[0:64, :], in_=oacc[64:128, :]), [ok], [f"ep_rl{i}"])
        self.tt("dve", tn[0:64, :], oacc[0:64, :], rl[0:64, :], ALU.mult, [ok, f"ep_rl{i}"], [f"ep_tn{i}"])
        self.tt("pool", ms[0:64, :], tn[0:64, :], sg_ap, ALU.mult, [f"ep_tn{i}", sgk], [f"ep_ms{i}"])
        self.st(f"ep_ms{i}s", dst, ms[0:64, :], [f"ep_ms{i}"], [("mix", uid)])

    def ep_alloc(self):
        self._ep = 0
        self.ep_rl = [self.sb(f"ep_rl{i}", [64, 512], F32) for i in range(2)]
        self.ep_tn = [self.sb(f"ep_tn{i}", [64, 512], F32) for i in range(2)]
        self.ep_ms = [self.sb(f"ep_ms{i}", [64, 512], BF16) for i in range(2)]

    def phase_fox(self, L):
        P = self.P
        dr = self.dram
        ident = self.load_const("ident", [128, 128], BF16)
        nmc = self.load_const("nm_cur", [128, 128], BF16)
        V = self.sb("V", [128, 32, 1024], BF16)
        for q4 in range(4):
            self.ld(f"V{q4}", V[:, q4 * 8:(q4 + 1) * 8, :],
                    dr["vA"][q4 * 1024:(q4 + 1) * 1024, :].rearrange("(n p) c -> p n c", p=128))
        vkeys = [f"V{q4}" for q4 in range(4)]
        qa = [self.sb(f"qa{i}", [128, T], BF16) for i in range(2)]
        ka = [self.sb(f"ka{i}", [128, T], BF16) for i in range(2)]
        sg = [self.sb(f"sg{i}", [64, T], BF16) for i in range(2)]
        NPT = 4
        pT = [self.sb(f"pT{i}", [128, 512], BF16) for i in range(NPT)]
        self.ep_alloc()
        ps_s = [self.ps(f"ps_s{i}") for i in range(4)]
        ps_o = [self.ps(f"ps_o{i}") for i in range(2)]
        ns = 0
        no = 0
        for h in range(8):
            b = h % 2
            self.ld(f"qa{b}", qa[b][0:64, :], dr["qA"][h * 64:(h + 1) * 64, :], writes=[f"qa{b}", f"qa{b}x"])
            self.ld(f"qa{b}c", qa[b][64:70, :], dr["caq"][h, :, :], writes=[f"qa{b}x"], reads=["caq"])
            self.ld(f"ka{b}", ka[b][0:64, :], dr["kA"][h * 64:(h + 1) * 64, :], writes=[f"ka{b}", f"ka{b}x"])
            self.ld(f"ka{b}c", ka[b][64:70, :], dr["cak"][h, :, :], writes=[f"ka{b}x"], reads=["cak"])
            self.ld(f"sg{b}", sg[b][:], dr["sgT"][h * 64:(h + 1) * 64, :])
            qk_reads = [f"qa{b}", f"qa{b}x", f"ka{b}", f"ka{b}x"]
            for qb in range(8):
                oacc = ps_o[no % 2]
                ok = f"ps_o{no % 2}"
                no += 1
                nkt = 4 * qb + 4
                units = []
                for kt in range(nkt):
                    jd = kt - 4 * qb
                    c0 = 128 * jd if jd > 0 else 0
                    units.append((kt, jd, c0))

                def s_stage(u, ns):
                    kt, jd, c0 = u
                    sp_ = ps_s[ns % 4]
                    sk = f"ps_s{ns % 4}"
                    pt_ = pT[ns % NPT]
                    pk = f"pT{ns % NPT}"
                    self.mm(sp_[:, c0:512], ka[b][0:70, kt * 128:(kt + 1) * 128],
                            qa[b][0:70, qb * 512 + c0:(qb + 1) * 512], True, jd < 0, qk_reads, [sk])
                    if jd >= 0:
                        self.mm(sp_[:, c0:c0 + 128], ident[:], nmc[:], False, True,
                                ["k_ident", "k_nm_cur"], [sk])
                    self.act(pt_[:, c0:512], sp_[:, c0:512], AF.Exp, [sk], [pk])
                    return pt_, pk

                LA = 2
                staged = []
                for ui, u in enumerate(units):
                    staged.append(s_stage(u, ns))
                    ns += 1
                    if ui >= LA:
                        kt, jd, c0 = units[ui - LA]
                        pt_, pk = staged[ui - LA]
                        self.mm(oacc[:, c0:512], V[:, kt, h * 128:(h + 1) * 128], pt_[:, c0:512],
                                kt == 0, kt == nkt - 1, [pk, vkeys[kt // 8]], [ok])
                for ui in range(max(0, len(units) - LA), len(units)):
                    kt, jd, c0 = units[ui]
                    pt_, pk = staged[ui]
                    self.mm(oacc[:, c0:512], V[:, kt, h * 128:(h + 1) * 128], pt_[:, c0:512],
                            kt == 0, kt == nkt - 1, [pk, vkeys[kt // 8]], [ok])
                self.attn_epilogue(oacc, ok, sg[b][:, qb * 512:(qb + 1) * 512], f"sg{b}",
                                   dr["mixT"][h * 64:(h + 1) * 64, qb * 512:(qb + 1) * 512], ("fox", h, qb))

    def phase_lin(self, L, kind):
        P = self.P
        dr = self.dram
        j = L // 2
        gla = kind == "gla"
        ident = self.load_const("ident", [128, 128], BF16)
        onesd = self.load_const("ones_d128", [128, 128], BF16)
        epst = self.sb("epst", [128, 1], F32)
        P.op("dve", lambda e: e.memset(epst[:], EPS), writes=["epst"])
        if gla:
            tri4 = self.load_const("tri4", [128, 512], F32)
            resetm = self.load_const("resetm", [128, 512], F32)
            blr = self.load_const("b_lr", [128, 4], F32, src=dr["b_lr"])
            gpar = self.load_const("gla_g", [128, 8], F32, src=dr["gla_g"])
            wl32 = self.load_const("wl32", [16, 256], F32, src=dr[f"w_lr{j}"])
            wl = self.sb("wl", [16, 256], BF16)
            self.cp("dve", wl[:], wl32[:], ["k_wl32"], ["wl"])
            glr = self.load_const("glr", [16, T], BF16, src=dr["glrT"])
            one1 = self.sb("one1", [128, 1], F32)
            P.op("dve", lambda e: e.memset(one1[:], 1.0), writes=["one1"])
            negb = self.sb("negb", [128, 2], F32)
            self.tsc("dve", negb[:], blr[:, j * 2:j * 2 + 2], -1.0, ALU.mult, ["k_b_lr"], ["negb"])
            sp = self.sb("sp", [128, T], F32)
            Bc = self.sb("Bc", [128, T], F32)
            Epos = self.sb("Epos", [128, T], F32)
        else:
            dtm4 = self.load_const("ret_dt4", [128, 2048], F32)
            xi = self.load_const("ret_xi", [128, 1024], F32)
            zt4 = self.load_const("ret_zt4", [128, 1024], F32)
            gw = self.load_const("gn_w", [128, 8], F32, src=dr["gn_w"])
            gb = self.load_const("gn_b", [128, 8], F32, src=dr["gn_b"])
            gcs = self.consts["ret_gc"]
        q2 = self.sb("q2", [128, T], BF16)
        k2 = self.sb("k2", [128, T], BF16)
        qt = self.sb("qt", [128, T], BF16)
        kt2 = self.sb("kt2", [128, T], BF16) if gla else k2
        ktok = self.sb("ktok", [128, 32, 128], BF16)
        Vg = self.sb("Vg", [128, 32, 512], BF16)
        for q4 in range(4):
            self.ld(f"Vg{q4}", Vg[:, q4 * 8:(q4 + 1) * 8, :],
                    dr["vB"][q4 * 1024:(q4 + 1) * 1024, :].rearrange("(n p) c -> p n c", p=128))
        sgt = [self.sb(f"sgt{e}", [128, T], BF16) for e in range(2)]
        U = self.sb("U", [128, 128], F32)
        stb = [self.sb(f"stb{i}", [128, 128], BF16) for i in range(2)]
        As = [self.sb(f"As{i}", [128, 512], BF16) for i in range(2)]
        sq = self.sb("sq", [128, 512], BF16)
        lnv = self.sb("lnv", [128, 512], F32)
        rstd = self.sb("rstd", [128, 512], F32)
        on = self.sb("on", [128, 512], F32)
        mst = [self.sb(f"mst{i}", [128, 512], BF16) for i in range(2)]
        if not gla:
            ob = self.sb("ob", [128, 512], BF16)
            mean = self.sb("mean", [128, 512], F32)
            var = self.sb("var", [128, 512], F32)
        ps_g = self.ps("ps_g")
        ps_g2 = self.ps("ps_g2") if not gla else None
        ps_t = self.ps("ps_t", (128, 1024), BF16)
        ps_a = [self.ps(f"ps_a{e}") for e in range(2 if gla else 1)]
        ps_kv = [self.ps(f"ps_kv{e}") for e in range(2)]
        NOB = 1
        ps_o = [[self.ps(f"ps_o{e}_{i}") for i in range(NOB)] for e in range(2)]
        nms = 0
        for hp in range(2):
            rows = slice(hp * 128, (hp + 1) * 128)
            self.ld("q2", q2[:], dr["qB"][rows, :])
            self.ld("k2", k2[:], dr["kB"][rows, :])
            for e in range(2):
                h = 2 * hp + e
                mrow = (512 if gla else 0) + h * 128
                self.ld(f"sgt{e}", sgt[e][:], dr["sgT"][mrow:mrow + 128, :])
            for tb in range(NB):
                ts = slice(tb * 512, (tb + 1) * 512)
                if gla:
                    self.mm(ps_g[:], wl[0:16, rows], glr[0:16, ts], True, True, ["wl", "k_glr"], ["ps_g"])
                    self.act(sp[:, ts], ps_g[:], AF.Exp, ["ps_g", "negb"], [("sp", tb)], scale=-1.0,
                             bias=negb[:, hp:hp + 1])
                    self.act(sp[:, ts], sp[:, ts], AF.Ln, [("sp", tb), "one1"], [("sp", tb)], bias=one1[:, 0:1])
                    P.op("dve", lambda e, ts=ts: e.tensor_tensor_scan(
                        out=Bc[:, ts], data0=resetm[:], data1=sp[:, ts], initial=0.0, op0=ALU.mult, op1=ALU.add),
                        [("sp", tb), "k_resetm"], [("Bc", tb)])
                    self.act(Epos[:, ts], Bc[:, ts], AF.Exp, [("Bc", tb)], [("Epos", tb)], scale=-1.0 / 16)
                    self.act(sp[:, ts], Bc[:, ts], AF.Exp, [("Bc", tb), ("sp", tb)], [("sp", tb)], scale=1.0 / 16)
                    self.stt(qt[:, ts], q2[:, ts], 0.125, Epos[:, ts], ALU.mult, ALU.mult,
                             ["q2", ("Epos", tb)], [("qt", tb)])
                    self.tt("dve", kt2[:, ts], k2[:, ts], sp[:, ts], ALU.mult, ["k2", ("sp", tb)], [("kt2", tb)])
                    ktk = ("kt2", tb)
                else:
                    self.tt("dve", qt[:, ts], q2[:, ts], xi[:, hp * 512:(hp + 1) * 512], ALU.mult,
                            ["q2", "k_ret_xi"], [("qt", tb)])
                    ktk = "k2"
                for c in range(4):
                    n = tb * 4 + c
                    cs = slice(n * 128, (n + 1) * 128)
                    self.P.op("pe", lambda e, cs=cs, c=c: e.transpose(
                        ps_t[:, c * 128:(c + 1) * 128], kt2[:, cs], ident[:]),
                        [ktk, "k_ident"], ["ps_t"])
                kdst = ktok[:, tb * 4:(tb + 1) * 4, :].rearrange("p n d -> p (n d)")
                if gla:
                    self.cp("act", kdst, ps_t[:, 0:512], ["ps_t"], [("ktok", tb)])
                else:
                    self.tt("dve", kdst, ps_t[:, 0:512], zt4[:, hp * 512:(hp + 1) * 512], ALU.mult,
                            ["ps_t", "k_ret_zt4"], [("ktok", tb)])
            for tb in range(NB):
                ts = slice(tb * 512, (tb + 1) * 512)
                for e in range(2):
                    h = 2 * hp + e
                    r = slice(64 * e, 64 * e + 64)
                    hc = slice(h * 128, (h + 1) * 128)
                    pa, pak = (ps_a[e], f"ps_a{e}") if gla else (ps_a[0], "ps_a0")
                    pkv, pkvk = ps_kv[e], f"ps_kv{e}"
                    po, pok = ps_o[e][0], f"ps_o{e}_0"
                    qk_r = [("qt", tb), ktk, "q2"]
                    for c in range(4):
                        n = tb * 4 + c
                        cs = slice(n * 128, (n + 1) * 128)
                        col = slice(c * 128, (c + 1) * 128)
                        if gla:
                            self.mm(pa[:, col], kt2[r, cs], qt[r, cs], c == 0, c == 3, qk_r, [pak])
                        else:
                            self.mm(pa[:, col], k2[r, cs], q2[r, cs], c == 0, c == 3, qk_r, [pak])
                    if gla:
                        self.tt("dve", As[e][:], pa[:], tri4[:], ALU.mult, [pak, "k_tri4"], [f"As{e}"])
                    else:
                        self.tt("dve", As[e][:], pa[:], dtm4[:, h * 512:(h + 1) * 512], ALU.mult,
                                [pak, "k_ret_dt4"], [f"As{e}"])
                    for c in range(4):
                        n = tb * 4 + c
                        col = slice(c * 128, (c + 1) * 128)
                        self.mm(pkv[0:64, col], ktok[:, n, r], Vg[:, n, hc], c == 0, c == 3,
                                [("ktok", tb), f"Vg{n // 8}"], [pkvk])
                    for c in range(4):
                        n = tb * 4 + c
                        col = slice(c * 128, (c + 1) * 128)
                        self.mm(po[:, col], Vg[:, n, hc], As[e][:, col], c == 0, (tb == 0 and c == 3 and False),
                                [f"As{e}", f"Vg{n // 8}"], [pok])
                for c in range(4):
                    n = tb * 4 + c
                    cs = slice(n * 128, (n + 1) * 128)
                    col = slice(c * 128, (c + 1) * 128)
                    for e in range(2):
                        h = 2 * hp + e
                        r = slice(64 * e, 64 * e + 64)
                        pkv, pkvk = ps_kv[e], f"ps_kv{e}"
                        po, pok = ps_o[e][0], f"ps_o{e}_0"
                        uk = ("U", e)
                        if n == 0:
                            self.cp("dve", U[r, :], pkv[0:64, col], [pkvk], [uk])
                        elif gla:
                            dcol = 128 * (n - 1) + 127
                            self.stt(U[r, :], U[r, :], Epos[r, dcol:dcol + 1], pkv[0:64, col], ALU.mult, ALU.add,
                                     [uk, pkvk, ("Epos", (n - 1) // 4)], [uk])
                        else:
                            self.stt(U[r, :], U[r, :], gcs[h], pkv[0:64, col], ALU.mult, ALU.add, [uk, pkvk], [uk])
                        if n < 31:
                            sk = (f"stb{(n + 1) % 2}", e)
                            if gla:
                                dcol = 128 * n + 127
                                self.tsc("dve", stb[(n + 1) % 2][r, :], U[r, :], Epos[r, dcol:dcol + 1], ALU.mult,
                                         [uk, ("Epos", tb)], [sk])
                            else:
                                self.cp("act", stb[(n + 1) % 2][r, :], U[r, :], [uk], [sk])
                        if n > 0:
                            self.mm(po[:, col], stb[n % 2][r, :], qt[r, cs], False, c == 3,
                                    [(f"stb{n % 2}", e), ("qt", tb)], [pok])
                for e in range(2):
                    h = 2 * hp + e
                    po, pok = ps_o[e][0], f"ps_o{e}_0"
                    if True:
                        n = tb * 4 + 3
                    if n % 4 == 3:
                        ts = slice(tb * 512, (tb + 1) * 512)
                        ms = mst[nms % 2]
                        msk = f"mst{nms % 2}"
                        nms += 1
                        dst = dr["mixT"][(512 if gla else 0) + h * 128:(512 if gla else 0) + (h + 1) * 128, ts]
                        self.act(sq[:], po[:], AF.Square, [pok], ["sq"])
                        self.mm(ps_g[:], onesd[:], sq[:], True, True, ["sq", "k_ones_d128"], ["ps_g"])
                        if gla:
                            self.act(lnv[:], ps_g[:], AF.Ln, ["ps_g", "epst"], ["lnv"], bias=epst[:, 0:1])
                            self.act(rstd[:], lnv[:], AF.Exp, ["lnv"], ["rstd"], scale=-0.5)
                            self.tt("dve", on[:], po[:], rstd[:], ALU.mult, [pok, "rstd"], ["on"])
                            self.stt(ms[:], on[:], gpar[:, j * 4 + h:j * 4 + h + 1], sgt[e][:, ts], ALU.mult, ALU.mult,
                                     ["on", "k_gla_g", f"sgt{e}"], [msk])
                        else:
                            self.cp("act", ob[:], po[:], [pok], ["ob"])
                            self.mm(ps_g2[:], onesd[:], ob[:], True, True, ["ob", "k_ones_d128"], ["ps_g2"])
                            self.cp("act", mean[:], ps_g2[:], ["ps_g2"], ["mean"])
                            self.tt("dve", var[:], mean[:], mean[:], ALU.mult, ["mean"], ["var"])
                            self.tt("dve", var[:], ps_g[:], var[:], ALU.subtract, ["ps_g", "var"], ["var"])
                            self.act(lnv[:], var[:], AF.Ln, ["var", "epst"], ["lnv"], bias=epst[:, 0:1])
                            self.act(rstd[:], lnv[:], AF.Exp, ["lnv"], ["rstd"], scale=-0.5)
                            self.tt("dve", on[:], po[:], mean[:], ALU.subtract, [pok, "mean"], ["on"])
                            self.tt("dve", on[:], on[:], rstd[:], ALU.mult, ["on", "rstd"], ["on"])
                            self.P.op("dve", lambda e_, h=h: e_.tensor_scalar(
                                out=on[:], in0=on[:], scalar1=gw[:, j * 4 + h:j * 4 + h + 1],
                                scalar2=gb[:, j * 4 + h:j * 4 + h + 1], op0=ALU.mult, op1=ALU.add),
                                ["on", "k_gn_w", "k_gn_b"], ["on"])
                            self.tt("dve", ms[:], on[:], sgt[e][:, ts], ALU.mult, ["on", f"sgt{e}"], [msk])
                        self.st(msk + "s", dst, ms[:], [msk], [("mix", kind, h, tb)])

    def phase_dil(self, L):
        P = self.P
        dr = self.dram
        ident = self.load_const("ident", [128, 128], BF16)
        nmc = self.load_const("nm_cur4", [128, 512], BF16)
        nmp = self.load_const("nm_prev4", [128, 512], BF16)
        q2 = self.sb("q2", [128, T], BF16)
        k2 = self.sb("k2", [128, T], BF16)
        V1 = self.sb("V1", [128, 32, 256], BF16)
        V4 = self.sb("V4", [128, 32, 256], BF16)
        V16 = self.sb("V16", [128, 32, 256], BF16)
        sg = [self.sb(f"sg{i}", [64, T], BF16) for i in range(2)]
        NPT = 3
        pT = [self.sb(f"pT{i}", [128, 512], BF16) for i in range(NPT)]
        self.ep_alloc()
        ps_s = [self.ps(f"ps_s{i}") for i in range(3)]
        ps_o = [self.ps(f"ps_o{i}") for i in range(4)]
        ns = 0
        for hp in range(4):
            rows = slice(hp * 128, (hp + 1) * 128)
            vc = slice(hp * 256, (hp + 1) * 256)
            self.ld("q2", q2[:], dr["qA"][rows, :])
            self.ld("k2", k2[:], dr["kA"][rows, :])
            self.ld("V1", V1[:], dr["vA"][:, vc].rearrange("(n p) c -> p n c", p=128))
            v4 = dr["vA"][:, vc].rearrange("(n i r) c -> i r n c", i=128, r=4)
            P.dma("sp", lambda e, v4=v4: [e.dma_start(out=V4[:, r * 8:(r + 1) * 8, :], in_=v4[:, r, :, :])
                                          for r in range(4)], "V4", 4, writes=["V4"])
            v16 = dr["vA"][:, vc].rearrange("(n i r) c -> i r n c", i=128, r=16)
            P.dma("sp", lambda e, v16=v16: [e.dma_start(out=V16[:, r * 2:(r + 1) * 2, :], in_=v16[:, r, :, :])
                                            for r in range(16)], "V16", 16, writes=["V16"])
            for e in range(2):
                h = 2 * hp + e
                self.ld(f"sg{e}", sg[e][:], dr["sgT"][512 + h * 64:512 + (h + 1) * 64, :])
            for e in range(2):
                h = 2 * hp + e
                r_ = slice(64 * e, 64 * e + 64)
                vh = slice(e * 128, (e + 1) * 128)
                for hf in range(2):
                    units = []
                    for gb in range(16 * hf, 16 * hf + 16):
                        bnk = (gb - 16 * hf) // 4
                        oc = slice((gb % 4) * 128, (gb % 4 + 1) * 128)
                        qc = slice(gb * 128, (gb + 1) * 128)
                        units.append((qc, qc, "cur", V1[:, gb, vh], "V1", [(bnk, oc, slice(0, 128))]))
                        if gb > 0:
                            units.append((slice((gb - 1) * 128, gb * 128), qc, "prev", V1[:, gb - 1, vh], "V1",
                                          [(bnk, oc, slice(0, 128))]))
                    for bnk in range(4):
                        n = 4 * hf + bnk
                        for r in range(4):
                            qc = slice(512 * n + r, 512 * (n + 1), 4)
                            oc = slice(r, 512, 4)
                            units.append((qc, qc, "cur", V4[:, r * 8 + n, vh], "V4", [(bnk, oc, slice(0, 128))]))
                            if n > 0:
                                units.append((slice(512 * (n - 1) + r, 512 * n, 4), qc, "prev",
                                              V4[:, r * 8 + n - 1, vh], "V4", [(bnk, oc, slice(0, 128))]))
                    for r in range(16):
                        qc = slice(2048 * hf + r, 2048 * (hf + 1), 16)
                        outs = [(bnk, slice(r, 512, 16), slice(32 * bnk, 32 * bnk + 32)) for bnk in range(4)]
                        units.append((qc, qc, "cur", V16[:, r * 2 + hf, vh], "V16", outs))
                        if hf > 0:
                            units.append((slice(r, 2048, 16), qc, "prev", V16[:, r * 2, vh], "V16", outs))
                    units = [u for u in units if u[2] == "cur"] + [u for u in units if u[2] == "prev"]
                    groups = []
                    for mk_ in ("cur", "prev"):
                        us = [u for u in units if u[2] == mk_]
                        for g0 in range(0, len(us), 4):
                            groups.append(us[g0:g0 + 4])
                    seq = []
                    for ui, u in enumerate(units):
                        for (bnk, oc, pc) in u[5]:
                            seq.append((ui, bnk))
                    first = {}
                    lastm = {}
                    for si, (ui, bnk) in enumerate(seq):
                        first.setdefault(bnk, si)
                        lastm[bnk] = si
                    sic = [0]

                    def stage_a(grp, ns):
                        sp_ = ps_s[ns % 3]
                        sk = f"ps_s{ns % 3}"
                        pt_ = pT[ns % NPT]
                        pk = f"pT{ns % NPT}"
                        for gi, (kc, qc, mask, vt, vkey, outs) in enumerate(grp):
                            cs = slice(gi * 128, (gi + 1) * 128)
                            self.mm(sp_[:, cs], k2[r_, kc], q2[r_, qc], gi == 0, False, ["q2", "k2"], [sk])
                        w = 128 * len(grp)
                        self.mm(sp_[:, 0:w], ident[:], (nmc if grp[0][2] == "cur" else nmp)[:, 0:w], False, True,
                                ["k_ident", "k_nm_cur4", "k_nm_prev4"], [sk])
                        self.act(pt_[:, 0:w], sp_[:, 0:w], AF.Exp, [sk], [pk])
                        return (grp, pt_, pk)

                    def stage_b(item):
                        grp, pt_, pk = item
                        for gi, (kc, qc, mask, vt, vkey, outs) in enumerate(grp):
                            for (bnk, oc, pc) in outs:
                                pcs = slice(gi * 128 + pc.start, gi * 128 + pc.stop)
                                si = sic[0]
                                self.mm(ps_o[bnk][:, oc], vt, pt_[:, pcs], si == first[bnk], si == lastm[bnk],
                                        [pk, vkey], [f"ps_o{bnk}"])
                                sic[0] += 1

                    LA = 2
                    staged = []
                    for gidx, grp in enumerate(groups):
                        staged.append(stage_a(grp, ns))
                        ns += 1
                        if gidx >= LA:
                            stage_b(staged[gidx - LA])
                    for gidx in range(max(0, len(groups) - LA), len(groups)):
                        stage_b(staged[gidx])
                    for bnk in range(4):
                        ts = slice(2048 * hf + 512 * bnk, 2048 * hf + 512 * (bnk + 1))
                        self.attn_epilogue(ps_o[bnk], f"ps_o{bnk}", sg[e][:, ts], f"sg{e}",
                                           dr["mixT"][512 + h * 64:512 + (h + 1) * 64, ts], ("dil", h, hf, bnk))


def make_in_maps(inputs, consts):
    x = np.asarray(inputs["x"], np.float32)
    common = {}
    for j in range(2):
        common[f"w_in_e{j}"] = np.ascontiguousarray(inputs["w_in_even"][j], np.float32)
        common[f"w_in_o{j}"] = np.ascontiguousarray(inputs["w_in_odd"][j], np.float32)
        common[f"w_out_e{j}"] = np.ascontiguousarray(inputs["w_out_even"][j], np.float32)
        common[f"w_out_o{j}"] = np.ascontiguousarray(inputs["w_out_odd"][j], np.float32)
        common[f"w_lr{j}"] = np.ascontiguousarray(inputs["w_lr_even"][j], np.float32)
    gains = [inputs["norm_even"][0], inputs["norm_odd"][0], inputs["norm_even"][1], inputs["norm_odd"][1],
             inputs["final_norm"]]
    ng = np.stack([np.asarray(g, np.float32).reshape(8, 128).T for g in gains], axis=1)
    common["normg"] = np.ascontiguousarray(ng.reshape(128, 40))
    common["b_f"] = np.ascontiguousarray(np.asarray(inputs["b_f_even"], np.float32).T)
    blr = np.asarray(inputs["b_lr_even"], np.float32).reshape(2, 2, 128)
    common["b_lr"] = np.ascontiguousarray(blr.transpose(2, 0, 1).reshape(128, 4))
    for nm, key in (("gla_g", "gla_norm_even"), ("gn_w", "ret_gn_w_odd"), ("gn_b", "ret_gn_b_odd")):
        a = np.asarray(inputs[key], np.float32).reshape(2, 4, 128)
        common[nm] = np.ascontiguousarray(a.transpose(2, 0, 1).reshape(128, 8))
    for n in CONST_NAMES:
        common["c_" + n] = consts[n]
    maps = []
    for b in range(8):
        m = dict(common)
        m["xT"] = np.ascontiguousarray(x[b].T)
        maps.append(m)
    return maps


_CACHE = {}


def kernel(**inputs):
    if "b" not in _CACHE:
        b = Builder()
        b.build()
        _CACHE["b"] = b
    b = _CACHE["b"]
    maps = make_in_maps(inputs, b.consts)
    res = run_bass_kernel_spmd(b.nc, maps, core_ids=list(range(8)))
    out = np.stack([np.ascontiguousarray(r["yT"].T) for r in res.results], axis=0)
    return out.astype(np.float32)
```
